# Optimizing a Trainium2 kernel written in Bass

```python
import math
import jax, jax.numpy as jnp
from jax import lax
import numpy as np

D_MODEL = 2048
BATCH = 8
SEQ = 4096
DEPTH = 4

GRID_W = 64
CTX_LEN = 256
NORM_EPS = 1e-6
N_MOD = 6
ATT_HEAD_DIM = 128
ATT_HEADS = D_MODEL // 2 // ATT_HEAD_DIM
ATT_KV_HEADS = ATT_HEADS // 4
ATT_WINDOW = 128
ATT_BLOCK = 128
ROPE_THETA = 10000.0
SSD_HEAD_DIM = 64
SSD_W = D_MODEL // 4
SSD_HEADS = SSD_W // SSD_HEAD_DIM
SSD_GROUPS = 2
SSD_STATE = 128
SSD_CONV = 3
SSD_CHUNK = 128
SC_W = D_MODEL // 4
SC_CONV = 3
ATT_W = ATT_HEADS * ATT_HEAD_DIM
ATT_KV_W = ATT_KV_HEADS * ATT_HEAD_DIM
SSD_BC_W = SSD_GROUPS * SSD_STATE
SSD_XBC_W = SSD_W + 2 * SSD_BC_W
SSD_DT_W = 2 * SSD_HEADS
MIX_W = ATT_W + SSD_W + SC_W
IN_SPLITS = (ATT_W, ATT_KV_W, ATT_KV_W, SSD_W, SSD_XBC_W, SSD_DT_W, SC_W, SC_W, SC_W)
IN_W = sum(IN_SPLITS)
N_EXPERTS = 16
EC_FACTOR = 2
EXPERT_FF = D_MODEL // 2

kernel_name = 'hybrid_ssd_swa_shortconv_ec_moe_dit'

f32 = jnp.float32


def _rmsnorm(x, g):
    xf = x.astype(f32)
    y = xf * lax.rsqrt(jnp.mean(xf * xf, axis=-1, keepdims=True) + NORM_EPS)
    return (y * g.astype(f32)).astype(x.dtype)


def _modulate(x, g, shift, scale):
    return _rmsnorm(x, g) * (1 + scale) + shift


def _dwconv(x, w):
    k = w.shape[0]
    return lax.conv_general_dilated(x, w[:, None, :].astype(x.dtype), window_strides=(1,),
                                    padding=[(k // 2, k // 2)],
                                    dimension_numbers=('NWC', 'WIO', 'NWC'),
                                    feature_group_count=x.shape[-1])


def _rope_tables(n_tokens):
    rows = n_tokens // GRID_W
    row = jnp.repeat(jnp.arange(rows), GRID_W).astype(f32)
    col = jnp.tile(jnp.arange(GRID_W), rows).astype(f32)
    n_freq = ATT_HEAD_DIM // 4
    inv = ROPE_THETA ** (-jnp.arange(n_freq, dtype=f32) / n_freq)
    ang_r = (row[:, None] * inv)[:, None, :]
    ang_c = (col[:, None] * inv)[:, None, :]
    return (jnp.cos(ang_r), jnp.sin(ang_r), jnp.cos(ang_c), jnp.sin(ang_c))


def _rot_half(x, cos, sin):
    x1, x2 = jnp.split(x, 2, axis=-1)
    return jnp.concatenate([x1 * cos - x2 * sin, x2 * cos + x1 * sin], axis=-1)


def _rope_2d(x, tabs):
    cos_r, sin_r, cos_c, sin_c = (t.astype(x.dtype) for t in tabs)
    xr, xc = jnp.split(x, 2, axis=-1)
    return jnp.concatenate([_rot_half(xr, cos_r, sin_r), _rot_half(xc, cos_c, sin_c)], axis=-1)


def _attn_latent(q, k, v, kc, vc, sink):
    b, s, _, dh = q.shape
    l = kc.shape[1]
    nb = s // ATT_BLOCK
    grp = ATT_HEADS // ATT_KV_HEADS
    scale = dh ** -0.5
    qb = q.reshape(b, nb, ATT_BLOCK, ATT_KV_HEADS, grp, dh)

    def band(t):
        tp = jnp.pad(t, ((0, 0), (ATT_BLOCK, ATT_BLOCK), (0, 0), (0, 0)))
        tp = tp.reshape(b, nb + 2, ATT_BLOCK, ATT_KV_HEADS, dh)
        return jnp.concatenate([tp[:, :-2], tp[:, 1:-1], tp[:, 2:]], axis=2)

    kb, vb = band(k), band(v)
    qpos = jnp.arange(s).reshape(nb, ATT_BLOCK)
    kpos = (jnp.arange(nb)[:, None] - 1) * ATT_BLOCK + jnp.arange(3 * ATT_BLOCK)[None, :]
    mask = ((jnp.abs(qpos[:, :, None] - kpos[:, None, :]) <= ATT_WINDOW)
            & (kpos[:, None, :] >= 0) & (kpos[:, None, :] < s))
    s_win = jnp.einsum('bnqhgd,bnkhd->bnhgqk', qb, kb).astype(f32) * scale
    s_win = jnp.where(mask[None, :, None, None], s_win, -jnp.inf)
    s_ctx = jnp.einsum('bnqhgd,bkhd->bnhgqk', qb, kc).astype(f32) * scale
    s_sink = jnp.broadcast_to(sink.astype(f32).reshape(ATT_KV_HEADS, grp, 1, 1), s_win.shape[:-1] + (1,))
    p = jax.nn.softmax(jnp.concatenate([s_win, s_ctx, s_sink], axis=-1), axis=-1).astype(q.dtype)
    w = 3 * ATT_BLOCK
    o = (jnp.einsum('bnhgqk,bnkhd->bnqhgd', p[..., :w], vb)
         + jnp.einsum('bnhgqk,bkhd->bnqhgd', p[..., w:w + l], vc))
    return o.reshape(b, s, ATT_HEADS * dh)


def _attn_context(qc, kc, vc, sink):
    b, l, _, dh = qc.shape
    grp = ATT_HEADS // ATT_KV_HEADS
    qg = qc.reshape(b, l, ATT_KV_HEADS, grp, dh)
    sc = jnp.einsum('bqhgd,bkhd->bhgqk', qg, kc).astype(f32) * (dh ** -0.5)
    s_sink = jnp.broadcast_to(sink.astype(f32).reshape(ATT_KV_HEADS, grp, 1, 1), sc.shape[:-1] + (1,))
    p = jax.nn.softmax(jnp.concatenate([sc, s_sink], axis=-1), axis=-1)[..., :l].astype(qc.dtype)
    o = jnp.einsum('bhgqk,bkhd->bqhgd', p, vc)
    return o.reshape(b, l, ATT_HEADS * dh)


def _ssd_scan(xs, dt, a, bm, cm, h0):
    b, t = xs.shape[:2]
    nc = t // SSD_CHUNK
    ch = lambda u: u.reshape((b, nc, SSD_CHUNK) + u.shape[2:])
    xs, dt, bm, cm = ch(xs), ch(dt), ch(bm), ch(cm)
    cum = jnp.cumsum(dt * a, axis=2)
    xdt = xs * dt[..., None]
    tril = jnp.tril(jnp.ones((SSD_CHUNK, SSD_CHUNK), bool))
    seg = cum[:, :, :, None] - cum[:, :, None, :]
    decay = jnp.exp(jnp.where(tril[:, :, None, None], seg, -jnp.inf))
    cb = jnp.einsum('bcqgn,bcsgn->bcqsg', cm, bm)
    y_diag = jnp.einsum('bcqsgk,bcsgkp->bcqgkp', cb[..., None] * decay, xdt)
    decay_end = jnp.exp(cum[:, :, -1:] - cum)
    st = jnp.einsum('bcsgn,bcsgkp->bcgkpn', bm, xdt * decay_end[..., None])
    chunk_decay = jnp.exp(cum[:, :, -1])

    def step(h, inp):
        s_c, d_c = inp
        return h * d_c[..., None, None] + s_c, h

    h_last, h_prev = lax.scan(step, h0, (jnp.moveaxis(st, 1, 0), jnp.moveaxis(chunk_decay, 1, 0)))
    h_prev = jnp.moveaxis(h_prev, 0, 1)
    y_off = jnp.einsum('bcqgn,bcgkpn->bcqgkp', cm, h_prev) * jnp.exp(cum)[..., None]
    return (y_diag + y_off).reshape((b, t) + xs.shape[3:]), h_last


def _ssd_bidir(xbc_raw, dt_raw, conv_w, conv_b, a_log, dt_bias, d_skip, h0):
    b, t, _ = xbc_raw.shape
    kh = SSD_HEADS // SSD_GROUPS
    xbc = jax.nn.silu(_dwconv(xbc_raw, conv_w) + conv_b).astype(f32)
    xs, bm, cm = jnp.split(xbc, [SSD_W, SSD_W + SSD_BC_W], axis=-1)
    xs = xs.reshape(b, t, SSD_GROUPS, kh, SSD_HEAD_DIM)
    bm = bm.reshape(b, t, SSD_GROUPS, SSD_STATE)
    cm = cm.reshape(b, t, SSD_GROUPS, SSD_STATE)
    dt = jax.nn.softplus(dt_raw.astype(f32).reshape(b, t, 2, SSD_GROUPS, kh)
                         + dt_bias.astype(f32).reshape(2, SSD_GROUPS, kh))
    a = -jnp.exp(a_log.astype(f32).reshape(2, SSD_GROUPS, kh))
    flip = lambda u: jnp.flip(u, axis=1)
    y_f, h_f = _ssd_scan(xs, dt[:, :, 0], a[0], bm, cm, h0[0])
    y_b, h_b = _ssd_scan(flip(xs), flip(dt[:, :, 1]), a[1], flip(bm), flip(cm), h0[1])
    y = y_f + flip(y_b) + d_skip.astype(f32).reshape(SSD_GROUPS, kh, 1) * xs
    return y.reshape(b, t, SSD_W), jnp.stack([h_f, h_b])


def _ssd_out(y, z, g):
    b, t, _ = y.shape
    u = (y * jax.nn.silu(z.astype(f32))).reshape(b, t, SSD_GROUPS, SSD_W // SSD_GROUPS)
    u = u * lax.rsqrt(jnp.mean(u * u, axis=-1, keepdims=True) + NORM_EPS)
    return (u.reshape(b, t, SSD_W) * g.astype(f32)).astype(z.dtype)


def _ec_moe(h, w_r, w_g, w_u, w_d):
    b, t, d = h.shape
    cap = EC_FACTOR * t // N_EXPERTS
    aff = jax.nn.softmax((h @ w_r).astype(f32), axis=-1)
    gval, idx = lax.top_k(jnp.swapaxes(aff, 1, 2), cap)
    xg = jax.vmap(lambda hb, ib: hb[ib])(h, idx)
    a = jnp.einsum('becd,edf->becf', xg, w_g)
    u = jnp.einsum('becd,edf->becf', xg, w_u)
    y = jnp.einsum('becf,efd->becd', jax.nn.silu(a) * u, w_d) * gval[..., None].astype(h.dtype)
    return jax.vmap(lambda yb, ib: jnp.zeros((t, d), h.dtype).at[ib.reshape(-1)].add(yb.reshape(-1, d)))(y, idx)


def _layer(x, xc, c, c_ctx, rope, with_ctx_out, norm1_g, norm2_g, w_mod, b_mod, w_in, q_norm_g, k_norm_g,
           attn_sink, ssd_conv_w, ssd_conv_b, ssd_dt_bias, ssd_a_log, ssd_d, ssd_norm_g, sc_conv_w, w_out,
           w_router, w_eg, w_eu, w_ed):
    b = x.shape[0]
    sh1, sc1, g1, sh2, sc2, g2 = [m[:, None, :] for m in jnp.split(jax.nn.silu(c) @ w_mod + b_mod, N_MOD, axis=-1)]
    sh1c, sc1c, g1c, sh2c, sc2c, g2c = jnp.split(jax.nn.silu(c_ctx) @ w_mod + b_mod, N_MOD, axis=-1)
    cuts = [int(i) for i in np.cumsum(IN_SPLITS)[:-1]]
    h = _modulate(x, norm1_g, sh1, sc1)
    hc = _modulate(xc, norm1_g, sh1c, sc1c)
    q, k, v, z, xbc, dtr, scb, scc, sch = jnp.split(h @ w_in, cuts, axis=-1)
    qc, kc, vc, zc, xbcc, dtrc, scbc, sccc, schc = jnp.split(hc @ w_in, cuts, axis=-1)
    heads = lambda u, n: u.reshape(u.shape[:2] + (n, ATT_HEAD_DIM))

    kc_h = _rmsnorm(heads(kc, ATT_KV_HEADS), k_norm_g)
    vc_h = heads(vc, ATT_KV_HEADS)
    q_h = _rope_2d(_rmsnorm(heads(q, ATT_HEADS), q_norm_g), rope)
    k_h = _rope_2d(_rmsnorm(heads(k, ATT_KV_HEADS), k_norm_g), rope)
    att = _attn_latent(q_h, k_h, heads(v, ATT_KV_HEADS), kc_h, vc_h, attn_sink)

    h0 = jnp.zeros((2, b, SSD_GROUPS, SSD_HEADS // SSD_GROUPS, SSD_HEAD_DIM, SSD_STATE), f32)
    yc_raw, h_ctx = _ssd_bidir(xbcc, dtrc, ssd_conv_w, ssd_conv_b, ssd_a_log, ssd_dt_bias, ssd_d, h0)
    y_raw, _ = _ssd_bidir(xbc, dtr, ssd_conv_w, ssd_conv_b, ssd_a_log, ssd_dt_bias, ssd_d, h_ctx)
    ssd = _ssd_out(y_raw, z, ssd_norm_g)

    sconv = scb * _dwconv(scc * sch, sc_conv_w)

    x = x + g1 * (jnp.concatenate([att, ssd, sconv], axis=-1) @ w_out)
    x = x + g2 * _ec_moe(_modulate(x, norm2_g, sh2, sc2), w_router, w_eg, w_eu, w_ed)

    if with_ctx_out:
        attc = _attn_context(_rmsnorm(heads(qc, ATT_HEADS), q_norm_g), kc_h, vc_h, attn_sink)
        ssdc = _ssd_out(yc_raw, zc, ssd_norm_g)
        sconvc = scbc * _dwconv(sccc * schc, sc_conv_w)
        xc = xc + g1c * (jnp.concatenate([attc, ssdc, sconvc], axis=-1) @ w_out)
        xc = xc + g2c * _ec_moe(_modulate(xc, norm2_g, sh2c, sc2c), w_router, w_eg, w_eu, w_ed)
    return x, xc


def setup_inputs(seed: int = 0) -> dict:
    key = jax.random.key(seed)
    ks = iter(jax.random.split(key, 32))
    nrm = lambda shape, scale: jax.random.normal(next(ks), shape, f32) * scale
    L, D = DEPTH, D_MODEL
    dt0 = jnp.exp(jax.random.uniform(next(ks), (L, 2, SSD_HEADS), f32, math.log(1e-3), math.log(1e-1)))
    dt_bias = dt0 + jnp.log(-jnp.expm1(-dt0))
    a_log = jnp.log(jax.random.uniform(next(ks), (L, 2, SSD_HEADS), f32, 1.0, 16.0))
    return {
        'x': nrm((BATCH, SEQ, D), 1.0),
        'c': nrm((BATCH, D), 1.0),
        'ctx': nrm((BATCH, CTX_LEN, D), 1.0),
        'c_ctx': nrm((D,), 1.0),
        'norm1_g': 1.0 + nrm((L, D), 0.02),
        'norm2_g': 1.0 + nrm((L, D), 0.02),
        'w_mod': nrm((L, D, N_MOD * D), 0.5 * D ** -0.5),
        'b_mod': nrm((L, N_MOD * D), 0.02),
        'w_in': nrm((L, D, IN_W), D ** -0.5),
        'q_norm_g': 1.0 + nrm((L, ATT_HEAD_DIM), 0.02),
        'k_norm_g': 1.0 + nrm((L, ATT_HEAD_DIM), 0.02),
        'attn_sink': nrm((L, ATT_HEADS), 0.5),
        'ssd_conv_w': nrm((L, SSD_CONV, SSD_XBC_W), SSD_CONV ** -0.5),
        'ssd_conv_b': nrm((L, SSD_XBC_W), 0.02),
        'ssd_dt_bias': dt_bias,
        'ssd_a_log': a_log,
        'ssd_d': 1.0 + nrm((L, SSD_HEADS), 0.1),
        'ssd_norm_g': 1.0 + nrm((L, SSD_W), 0.02),
        'sc_conv_w': nrm((L, SC_CONV, SC_W), SC_CONV ** -0.5),
        'w_out': nrm((L, MIX_W, D), MIX_W ** -0.5),
        'w_router': nrm((L, D, N_EXPERTS), D ** -0.5),
        'w_expert_gate': nrm((L, N_EXPERTS, D, EXPERT_FF), D ** -0.5),
        'w_expert_up': nrm((L, N_EXPERTS, D, EXPERT_FF), D ** -0.5),
        'w_expert_down': nrm((L, N_EXPERTS, EXPERT_FF, D), EXPERT_FF ** -0.5),
    }


def reference(x, c, ctx, c_ctx, norm1_g, norm2_g, w_mod, b_mod, w_in, q_norm_g, k_norm_g, attn_sink,
              ssd_conv_w, ssd_conv_b, ssd_dt_bias, ssd_a_log, ssd_d, ssd_norm_g, sc_conv_w, w_out,
              w_router, w_expert_gate, w_expert_up, w_expert_down):
    rope = _rope_tables(x.shape[1])
    xc = ctx
    for i in range(DEPTH):
        x, xc = _layer(x, xc, c, c_ctx, rope, i < DEPTH - 1, norm1_g[i], norm2_g[i], w_mod[i], b_mod[i],
                       w_in[i], q_norm_g[i], k_norm_g[i], attn_sink[i], ssd_conv_w[i], ssd_conv_b[i],
                       ssd_dt_bias[i], ssd_a_log[i], ssd_d[i], ssd_norm_g[i], sc_conv_w[i], w_out[i],
                       w_router[i], w_expert_gate[i], w_expert_up[i], w_expert_down[i])
    return x
```

```python
import math
from contextlib import ExitStack

import numpy as np
import concourse.bass as bass
import concourse.mybir as mybir
from concourse.bass_utils import run_bass_kernel_spmd

F32 = mybir.dt.float32
BF = mybir.dt.bfloat16
U32 = mybir.dt.uint32
AF = mybir.ActivationFunctionType
ALU = mybir.AluOpType
AX = mybir.AxisListType

D = 2048
KC = 16
CTX = 256
NE = 16
FF = 1024
EPS = 1e-6
C_Q, C_K, C_V, C_Z, C_XBC, C_DT, C_SB, C_SC, C_SH, C_END = 0, 1024, 1280, 1536, 2048, 3072, 3088, 3600, 4112, 4624

SAME_ENGINE_SYNC = True
EPOCH = 30000


class Op:
    __slots__ = ("eng", "fn", "slot", "deps", "needs_inc", "val", "idx", "ep")

    def __init__(self, eng, fn, slot):
        self.eng, self.fn, self.slot = eng, fn, slot
        self.deps = set()
        self.needs_inc = False
        self.val = 0


class Sch:
    ENGS = ("pe", "act", "dve", "pool", "sp")

    def __init__(self):
        self.ops = []
        self.last_w = {}
        self.readers = {}
        self.slot_last = {}

    def barrier(self):
        last = {}
        for o in self.ops:
            if o.fn is None:
                continue
            last[o.slot if o.slot is not None else ("eng", o.eng)] = o
        for e in self.ENGS:
            b = Op(e, None, None)
            b.idx = len(self.ops)
            b.deps = set(last.values())
            self.ops.append(b)

    def op(self, eng, fn, r=(), w=(), slot=None):
        o = Op(eng, fn, slot)
        o.idx = len(self.ops)
        deps = o.deps
        for k in r:
            lw = self.last_w.get(k)
            if lw is not None:
                deps.add(lw)
        for k in w:
            lw = self.last_w.get(k)
            if lw is not None:
                deps.add(lw)
            for rd in self.readers.get(k, ()):
                deps.add(rd)
        if slot is not None:
            p = self.slot_last.get(slot)
            if p is not None:
                deps.add(p)
            self.slot_last[slot] = o
        deps.discard(o)
        for k in r:
            self.readers.setdefault(k, []).append(o)
        for k in w:
            self.last_w[k] = o
            self.readers[k] = []
        self.ops.append(o)
        return o

    def emit(self, nc):
        for o in self.ops:
            keep = set()
            for d in o.deps:
                if o.fn is not None and d.slot is None and d.eng == o.eng and (o.eng == "pe" or not SAME_ENGINE_SYNC):
                    continue
                keep.add(d)
            o.deps = keep
            for d in keep:
                d.needs_inc = True
        eng_cnt = {e: 0 for e in self.ENGS}
        slot_cnt = {}
        slots = []
        for o in self.ops:
            if o.slot is not None:
                if o.slot not in slot_cnt:
                    slot_cnt[o.slot] = 0
                    slots.append(o.slot)
                slot_cnt[o.slot] += 16
                o.val = slot_cnt[o.slot]
                o.needs_inc = True
            elif o.needs_inc:
                o.ep = eng_cnt[o.eng] // EPOCH
                o.val = eng_cnt[o.eng] % EPOCH + 1
                eng_cnt[o.eng] += 1
        with ExitStack() as es:
            esem = {(e, k): es.enter_context(nc.semaphore("e_%s%d" % (e, k)))
                    for e in self.ENGS if e != "sp" for k in range(eng_cnt[e] // EPOCH + 1)}
            ssem = {s: es.enter_context(nc.semaphore("s_%d" % i)) for i, s in enumerate(slots)}
            block = es.enter_context(nc.Block())
            engobj = {"pe": "tensor", "act": "scalar", "dve": "vector", "pool": "gpsimd", "sp": "sync"}

            def semof(o):
                return ssem[o.slot] if o.slot is not None else esem[(o.eng, o.ep)]

            def run(e):
                def body(eng):
                    waited = {}
                    for o in self.ops:
                        if o.eng != e:
                            continue
                        need = {}
                        for d in o.deps:
                            s = semof(d)
                            key = id(s)
                            if need.get(key, (0, None))[0] < d.val:
                                need[key] = (d.val, s)
                        for key, (val, s) in need.items():
                            if waited.get(key, 0) >= val:
                                continue
                            eng.wait_ge(s, val)
                            waited[key] = val
                        if o.fn is None:
                            continue
                        ins = o.fn(eng)
                        if o.needs_inc:
                            ins.then_inc(semof(o), 16 if o.slot is not None else 1)
                    if e == "sp":
                        for s in slots:
                            eng.wait_ge(ssem[s], slot_cnt[s])
                return body

            for e in self.ENGS:
                getattr(block, engobj[e])(run(e))
        return eng_cnt, slot_cnt


def build(T, L, debug=False, upto=99):
    TA = CTX + T
    NTT = TA // 128
    NCH = NTT
    cap_l = 2 * T // NE
    cap_c = 2 * CTX // NE
    assert cap_l % 128 == 0 or cap_l <= 128
    nc = bass.Bass("TRN2", target_bir_lowering=False)
    S = Sch()
    es = ExitStack()

    def din(name, shape, dt=F32):
        return nc.dram_tensor(name, list(shape), dt, kind="ExternalInput").ap()

    def dscr(name, shape, dt=F32):
        return nc.dram_tensor(name, list(shape), dt, kind="ExternalOutput" if debug else "Internal").ap()

    ARENA_N = 52224
    arena = es.enter_context(nc.sbuf_tensor("arena", [128, ARENA_N], F32))
    ar = {"off": 0}

    def sb(name, shape, dt=F32):
        shape = list(shape)
        nelem = 1
        for x in shape[1:]:
            nelem *= x
        size = 4 if dt in (F32, U32) else 2
        n32 = (nelem * size + 3) // 4
        n32 = (n32 + 7) // 8 * 8
        off = ar["off"]
        assert off + n32 <= ARENA_N, ("arena overflow", name, off, n32)
        ar["off"] = off + n32
        ap = arena[0:shape[0], off:off + n32]
        if dt != F32:
            ap = ap.bitcast(dt)
        ap = ap[:, 0:nelem]
        if len(shape) == 3:
            ap = ap.rearrange("p (a b) -> p a b", b=shape[2])
        elif len(shape) == 4:
            ap = ap.rearrange("p (a b c) -> p a b c", b=shape[2], c=shape[3])
        return ap

    def phase_begin():
        S.barrier()
        ar["off"] = ar["perm"]

    xall = din("xall", [TA, D])
    cT_d = din("cT", [128, KC, 2])
    ropec_d = din("ropec", [T, 1024])
    ropes_d = din("ropes", [T, 1024])
    consts_d = din("consts", [128, 5, 128])
    w_mod = din("w_mod", [L, D, 6 * D])
    b_mod = din("b_mod", [L, 6 * D])
    w_in = din("w_in", [L, D, C_END])
    w_out = din("w_out", [L, D, D])
    w_r = din("w_router", [L, D, NE])
    w_eg = din("w_expert_gate", [L, NE, D, FF])
    w_eu = din("w_expert_up", [L, NE, D, FF])
    w_ed = din("w_expert_down", [L, NE, FF, D])
    norm1_g = din("norm1_g", [L, D])
    norm2_g = din("norm2_g", [L, D])
    qng = din("q_norm_g", [L, 128])
    kng = din("k_norm_g", [L, 128])
    sink_d = din("attn_sink", [L, 8])
    convw_d = din("convw", [L, 128, 8, 3])
    convb_d = din("convb", [L, 128, 8])
    dtb_d = din("ssd_dt_bias", [L, 16])
    alog_d = din("ssd_a_log", [L, 16])
    dsk_d = din("ssd_d", [L, 8])
    sng_d = din("ssd_norm_g", [L, 512])
    scw_d = din("scw", [L, 128, 4, 3])
    out_d = nc.dram_tensor("out", [T, D], F32, kind="ExternalOutput").ap()

    xres = dscr("xres", [TA, D])
    modd = dscr("modd", [L, 2, 6 * D])
    gsd = dscr("gsd", [L, 2, 6, D])
    q_raw = dscr("q_raw", [TA, 1024])
    k_raw = dscr("k_raw", [TA, 256])
    v_tok = dscr("v_tok", [TA, 256], BF)
    z_tok = dscr("z_tok", [TA, 512])
    dt_raw = dscr("dt_raw", [TA, 16])
    xbcT = dscr("xbcT", [1024, TA])
    scT = dscr("scT", [1536, TA])
    qT = dscr("qT", [8, 128, TA], BF)
    kT = dscr("kT", [2, 128, TA], BF)
    mixT = dscr("mixT", [D, TA], BF)
    convT = dscr("convT", [1024, TA], BF)
    xbc_tok = dscr("xbc_tok", [TA, 768], BF)
    y_f = dscr("y_f", [TA, 512])
    h2_d = dscr("h2", [TA, D], BF)
    moe_d = dscr("moe_out", [TA, D])

    cst = sb("cst", [128, 5, 128])
    cstb = sb("cstb", [128, 5, 128], BF)
    onesb = sb("onesb", [128, 128], BF)
    ones32 = sb("ones32", [128, 128])
    zero32 = sb("zero32", [128, 2048])
    IDN, LE, GE, GT, LT = 0, 1, 2, 3, 4
    S.op("sp", lambda e: e.dma_start(out=cst[:], in_=consts_d), w=["cst"], slot="ld0")
    S.op("dve", lambda e: e.tensor_copy(out=cstb[:], in_=cst[:]), r=["cst"], w=["cstb"])
    S.op("dve", lambda e: e.memset(onesb[:], 1.0), w=["onesb"])
    S.op("dve", lambda e: e.memset(ones32[:], 1.0), w=["ones32"])
    S.op("pool", lambda e: e.memset(zero32[:], 0.0), w=["zero32"])

    psA = [es.enter_context(nc.psum_tensor("psA%d" % i, [128, 512], F32)) for i in range(6)]
    psT = [es.enter_context(nc.psum_tensor("psT%d" % i, [128, 1024], BF)) for i in range(2)]
    PA = ["psA%d" % i for i in range(6)]
    PT = ["psT0", "psT1"]

    cnt = {"i": 0}

    def rr(n):
        cnt["i"] += 1
        return cnt["i"] % n

    for tt in range(NTT):
        rows = slice(tt * 128, (tt + 1) * 128)
        S.op("sp", lambda e, rows=rows: e.dma_start(out=xres[rows, :], in_=xall[rows, :]),
             w=[("xres", tt)], slot="cp%d" % (tt % 4))

    def mm(out, lhsT, rhs, start, stop, r, w):
        import os
        if os.environ.get("NOMM"):
            return
        S.op("pe", lambda e: e.matmul(out, lhsT=lhsT, rhs=rhs, start=start, stop=stop), r=r, w=w)

    def tr(out, in_, ident, r, w):
        S.op("pe", lambda e: e.transpose(out=out, in_=in_, identity=ident), r=r, w=w)

    def act(out, in_, func, r, w, **kw):
        S.op("act", lambda e: e.activation(out=out, in_=in_, func=func, **kw), r=r, w=w)

    def tt(eng, out, in0, in1, op, r, w):
        S.op(eng, lambda e: e.tensor_tensor(out=out, in0=in0, in1=in1, op=op), r=r, w=w)

    def ts(eng, out, in0, s1, s2, op0, op1, r, w):
        if s2 is None:
            S.op(eng, lambda e: e.tensor_scalar(out=out, in0=in0, scalar1=s1, scalar2=None, op0=op0), r=r, w=w)
        else:
            S.op(eng, lambda e: e.tensor_scalar(out=out, in0=in0, scalar1=s1, scalar2=s2, op0=op0, op1=op1), r=r, w=w)

    def stt_(eng, out, in0, scalar, in1, op0, op1, r, w):
        S.op(eng, lambda e: e.scalar_tensor_tensor(out=out, in0=in0, scalar=scalar, in1=in1, op0=op0, op1=op1), r=r, w=w)

    def cp(eng, out, in_, r, w):
        if eng == "act":
            S.op(eng, lambda e: e.copy(out=out, in_=in_), r=r, w=w)
        else:
            S.op(eng, lambda e: e.tensor_copy(out=out, in_=in_), r=r, w=w)

    def dma(eng, out, in_, r, w, slot, **kw):
        import os
        if os.environ.get("NOST") and slot.startswith("ev"):
            return
        if eng == "st":
            eng = "act"
        S.op(eng, lambda e: e.dma_start(out=out, in_=in_, **kw), r=r, w=w, slot=slot)

    def bc_rows(ap, P=128):
        pat = [list(x) for x in ap.ap]
        while len(pat) > 1 and pat[0][1] == 1:
            pat = pat[1:]
        return bass.AP(ap.tensor, ap.offset, [[0, P]] + pat)

    def bc_last(ap, n):
        pat = [list(x) for x in ap.ap]
        return bass.AP(ap.tensor, ap.offset, pat + [[0, n]])

    def bc_mid(ap, n):
        pat = [list(x) for x in ap.ap]
        return bass.AP(ap.tensor, ap.offset, [pat[0], [0, n]] + pat[1:])

    wslot = {"i": 0}
    NWS = 8

    def load_w(dst, src, nk, key, slot):
        for k0 in range(0, nk, 4):
            m = min(4, nk - k0)
            wslot["i"] += 1
            dma("pool", dst[:, k0:k0 + m, :], src[k0 * 128:(k0 + m) * 128, :].rearrange("(k p) n -> p k n", p=128),
                r=[], w=[key], slot="W%d" % (wslot["i"] % NWS), max_dma_last_dim=8192)

    def evac_eng():
        return "act" if rr(2) else "dve"

    def K(name, rng):
        return [(name, i) for i in rng]

    def rstd_from_ss(ssap, outap, n, tmpap, keys):
        ts("dve", tmpap, ssap, 1.0 / n, EPS, ALU.mult, ALU.add, r=keys, w=keys)
        act(tmpap, tmpap, AF.Sqrt, r=keys, w=keys)
        S.op("dve", lambda e: e.reciprocal(out=outap, in_=tmpap), r=keys, w=keys)

    ar["perm"] = ar["off"]

    def phase0():
        phase_begin()
        cTs = sb("cTs", [128, KC, 2])
        cTb = sb("cTb", [128, KC, 2], BF)
        mblk = sb("mblk", [2, 2048])
        bblk = sb("bblk", [2, 2048])
        gblk = sb("gblk", [2, 2048])
        oblk = sb("oblk", [2, 2048])
        wm = [sb("wm%d" % i, [128, 2048], BF) for i in range(4)]
        dma("sp", cTs[:], cT_d, r=[], w=["cTs"], slot="ld0")
        act(cTb[:], cTs[:], AF.Silu, r=["cTs"], w=["cTb"])
        idxmap = {0: 1, 1: 0, 2: 2, 3: 4, 4: 3, 5: 5}
        for l in range(L):
            for cb in range(6):
                for kc in range(KC):
                    b = rr(4)
                    dma("pool", wm[b][:], w_mod[l, kc * 128:(kc + 1) * 128, cb * 2048:(cb + 1) * 2048],
                        r=[], w=[("wm", b)], slot="W%d" % (rr(NWS)), max_dma_last_dim=8192)
                    for j in range(4):
                        mm(psA[j][0:2, :], cTb[:, kc, :], wm[b][:, j * 512:(j + 1) * 512], kc == 0, kc == KC - 1,
                           r=["cTb", ("wm", b)], w=[PA[j]])
                for p in range(2):
                    dma("sp", bblk[p:p + 1, :], b_mod[l:l + 1, cb * 2048:(cb + 1) * 2048], r=[], w=["bblk"], slot="ld%d" % p)
                    if cb in (1, 4):
                        ng = norm1_g if cb == 1 else norm2_g
                        dma("sp", gblk[p:p + 1, :], ng[l:l + 1, :], r=[], w=["gblk"], slot="ld%d" % (2 + p))
                for j in range(4):
                    tt("dve", mblk[:, j * 512:(j + 1) * 512], psA[j][0:2, :], bblk[:, j * 512:(j + 1) * 512], ALU.add,
                       r=[PA[j], "bblk"], w=["mblk"])
                if cb in (1, 4):
                    stt_("dve", oblk[:], mblk[:], 1.0, gblk[:], ALU.add, ALU.mult, r=["mblk", "gblk"], w=["oblk"])
                else:
                    cp("dve", oblk[:], mblk[:], r=["mblk"], w=["oblk"])
                dma("sp", gsd[l, :, idxmap[cb], :], oblk[:], r=["oblk"], w=[("gsd", l)], slot="st0")

    blocks = [(0, CTX)]
    BLK = min(2048, T)
    for i in range(T // BLK):
        blocks.append((CTX + i * BLK, BLK))

    xt = h32 = junk = GS = stt = gate = None

    def load_GS(l, which, with_gate):
        for st in range(2):
            for j in range(2):
                dma("sp", GS[:, st, j, :], bc_rows(gsd[l, st, 3 * which + j:3 * which + j + 1, :]),
                    r=[("gsd", l)], w=["GS"], slot="ld%d" % (st * 2 + j))
            if with_gate:
                dma("sp", gate[:, st, :], bc_rows(gsd[l, st, 3 * which + 2:3 * which + 3, :]),
                    r=[("gsd", l)], w=["gate"], slot="ld%d" % (4 + st))

    def norm_tile(tt_i, xsrc_key, out_ap, out_key):
        b = tt_i % 2
        st = 1 if tt_i < CTX // 128 else 0
        sq_, h_, s_ = junk[b], h32[b], stt[b]
        S.op("pool", lambda e, a=s_: e.memset(a[:], 0.0), w=[("stt", b)])
        act(sq_[:], xt[b][:], AF.Square, r=[("xt", b)], w=[("junk", b), ("stt", b)], accum_out=s_[:, 0:1])
        rstd_from_ss(s_[:, 0:1], s_[:, 2:3], D, s_[:, 1:2], [("stt", b)])
        stt_("dve", h_[:], xt[b][:], s_[:, 2:3], GS[:, st, 0, :], ALU.mult, ALU.mult, r=[("xt", b), ("stt", b), "GS"], w=[("h32", b)])
        tt("pool", out_ap, h_[:], GS[:, st, 1, :], ALU.add, r=[("h32", b), "GS"], w=[out_key])

    def load_x(tt_i):
        b = tt_i % 2
        dma("sp", xt[b][:], xres[tt_i * 128:(tt_i + 1) * 128, :], r=[("xres", tt_i)], w=[("xt", b)], slot="xt%d" % b)

    def alloc_norm():
        g = {}
        g["xt"] = [sb("xt%d" % i, [128, 2048]) for i in range(2)]
        g["h32"] = [sb("h32_%d" % i, [128, 2048]) for i in range(2)]
        g["junk"] = [sb("junk%d" % i, [128, 2048], BF) for i in range(2)]
        g["GS"] = sb("GS", [128, 2, 2, 2048])
        g["stt"] = [sb("stt%d" % i, [128, 16]) for i in range(2)]
        return g

    def phase_AB(l):
        phase_begin()
        nonlocal xt, h32, junk, GS, stt, gate
        g = alloc_norm()
        xt, h32, junk, GS, stt = g["xt"], g["h32"], g["junk"], g["GS"], g["stt"]
        BLK2 = min(1024, T)
        blocks2 = [(0, CTX)] + [(CTX + i * BLK2, BLK2) for i in range(T // BLK2)]
        hTs = [sb("hT%d" % i, [128, KC, BLK2], BF) for i in range(2)]
        wbuf = [sb("wbuf%d" % i, [128, KC, 512], BF) for i in range(2)]
        hb = [sb("hb%d" % i, [128, 2048], BF) for i in range(2)]
        ev = [sb("ev%d" % i, [128, 512]) for i in range(2)]
        evb = [sb("evb%d" % i, [128, 512], BF) for i in range(2)]
        wdt = sb("wdt", [128, KC, 16], BF)
        load_GS(l, 0, False)

        def a_tiles(bi):
            t0, n = blocks2[bi]
            hT = hTs[bi % 2]
            tt0 = t0 // 128
            for ti in range(n // 128):
                hbi = (tt0 + ti) % 2
                norm_tile(tt0 + ti, None, hb[hbi][:], ("hb", hbi))
                if tt0 + ti + 1 < NTT:
                    load_x(tt0 + ti + 1)
                for half in range(2):
                    for j in range(8):
                        kc = half * 8 + j
                        tr(psT[half][:, j * 128:(j + 1) * 128], hb[hbi][:, kc * 128:(kc + 1) * 128], cstb[:, IDN, :],
                           r=[("hb", hbi), "cstb"], w=[PT[half]])
                    cp("act" if half else "dve", hT[:, half * 8:(half + 1) * 8, ti * 128:(ti + 1) * 128],
                       psT[half][:].rearrange("p (k t) -> p k t", k=8), r=[PT[half]], w=[("hT", bi % 2, ti)])
                yield

        def b_groups(bi):
            t0, n = blocks2[bi]
            hT = hTs[bi % 2]
            hk = lambda ti: ("hT", bi % 2, ti)
            nt = n // 128
            tt0 = t0 // 128
            groups = [(C_Q, "q0"), (C_Q + 512, "q1"), (C_K, "kv"), (C_Z, "z")]
            for (c0, gname) in groups:
                b = rr(2)
                load_w(wbuf[b], w_in[l, :, c0:c0 + 512], KC, ("wbuf", b), "wb%d" % b)
                for ti in range(nt):
                    pb = rr(6)
                    rows = slice(t0 + ti * 128, t0 + (ti + 1) * 128)
                    for kc in range(KC):
                        mm(psA[pb][:, :], hT[:, kc, ti * 128:(ti + 1) * 128], wbuf[b][:, kc, :], kc == 0, kc == KC - 1,
                           r=[hk(ti), ("wbuf", b)], w=[PA[pb]])
                    eb = rr(2)
                    eng = evac_eng()
                    tkey = tt0 + ti
                    cp(eng, ev[eb][:], psA[pb][:], r=[PA[pb]], w=[("ev", eb)])
                    if gname == "kv":
                        cp("pool", evb[eb][:, 0:256], ev[eb][:, 256:512], r=[("ev", eb)], w=[("evb", eb)])
                        dma("st", k_raw[rows, :], ev[eb][:, 0:256], r=[("ev", eb)], w=[("k_raw", tkey)], slot="ev%d" % eb)
                        dma("st", v_tok[rows, :], evb[eb][:, 0:256], r=[("evb", eb)], w=[("v_tok", tkey)], slot="evb%d" % eb)
                    elif gname == "q0":
                        dma("st", q_raw[rows, 0:512], ev[eb][:], r=[("ev", eb)], w=[("q_raw0", tkey)], slot="ev%d" % eb)
                    elif gname == "q1":
                        dma("st", q_raw[rows, 512:1024], ev[eb][:], r=[("ev", eb)], w=[("q_raw1", tkey)], slot="ev%d" % eb)
                    else:
                        dma("st", z_tok[rows, :], ev[eb][:], r=[("ev", eb)], w=[("z_tok", tkey)], slot="ev%d" % eb)
                yield
            load_w(wdt, w_in[l, :, C_DT:C_DT + 16], KC, "wdt", "wdt")
            for ti in range(nt):
                pb = rr(6)
                rows = slice(t0 + ti * 128, t0 + (ti + 1) * 128)
                for kc in range(KC):
                    mm(psA[pb][:, 0:16], hT[:, kc, ti * 128:(ti + 1) * 128], wdt[:, kc, :], kc == 0, kc == KC - 1,
                       r=[hk(ti), "wdt"], w=[PA[pb]])
                eb = rr(2)
                cp(evac_eng(), ev[eb][:, 0:16], psA[pb][:, 0:16], r=[PA[pb]], w=[("ev", eb)])
                dma("st", dt_raw[rows, :], ev[eb][:, 0:16], r=[("ev", eb)], w=[("dt_raw", tt0 + ti)], slot="ev%d" % eb)
            yield
            fgroups = [(C_XBC + i * 512, xbcT, i * 512, "xbcT") for i in range(2)] + \
                      [(C_SB + i * 512, scT, i * 512, "scT") for i in range(3)]
            cw = min(512, n)
            for (c0, dst, row0, dname) in fgroups:
                b = rr(2)
                load_w(wbuf[b], w_in[l, :, c0:c0 + 512], KC, ("wbuf", b), "wb%d" % b)
                for j in range(4):
                    for ci in range(n // cw):
                        pb = rr(6)
                        for kc in range(KC):
                            mm(psA[pb][:, 0:cw], wbuf[b][:, kc, j * 128:(j + 1) * 128], hT[:, kc, ci * cw:(ci + 1) * cw],
                               kc == 0, kc == KC - 1,
                               r=[hk(q) for q in range(ci * cw // 128, (ci + 1) * cw // 128)] + [("wbuf", b)], w=[PA[pb]])
                        eb = rr(2)
                        cp(evac_eng(), ev[eb][:, 0:cw], psA[pb][:, 0:cw], r=[PA[pb]], w=[("ev", eb)])
                        ch0 = row0 + j * 128
                        dma("st", dst[ch0:ch0 + 128, t0 + ci * cw:t0 + (ci + 1) * cw], ev[eb][:, 0:cw], r=[("ev", eb)],
                            w=[(dname, ch0 // 128, (t0 + ci * cw) // 128 + q) for q in range(cw // 128)], slot="ev%d" % eb)
                    if j == 1:
                        yield
                yield

        load_x(0)
        for _ in a_tiles(0):
            pass
        for bi in range(len(blocks2)):
            nxt = a_tiles(bi + 1) if bi + 1 < len(blocks2) else iter(())
            for _ in b_groups(bi):
                next(nxt, None)
            for _ in nxt:
                pass

    def conv_pieces():
        P = [(0, CTX, 0, CTX)]
        step = min(T, 2048)
        for a in range(0, T, step):
            P.append((CTX + a, step, CTX, CTX + T))
        return P

    def load_halo(dst, src_rows, t0, n, lo, hi, key, slot, rkeys):
        a = max(t0 - 1, lo)
        b = min(t0 + n + 1, hi)
        if a > t0 - 1:
            S.op("pool", lambda e: e.memset(dst[:, 0:1], 0.0), w=[key])
        if b < t0 + n + 1:
            S.op("pool", lambda e: e.memset(dst[:, n + 1:n + 2], 0.0), w=[key])
        dma("sp", dst[:, a - (t0 - 1):b - (t0 - 1)], src_rows[:, a:b], r=rkeys, w=[key], slot=slot)

    def phase_E(l):
        phase_begin()
        NP = min(T, 2048) + 2
        cin = sb("cin", [128, NP])
        hin = sb("hin", [128, NP])
        bin_ = sb("bin", [128, NP])
        acc = sb("acc", [128, NP])
        ob = sb("ob", [128, NP], BF)
        scw = sb("scw_s", [128, 4, 3])
        dma("sp", scw[:], scw_d[l], r=[], w=["scw"], slot="ld0")
        for j in range(4):
            for (t0, n, lo, hi) in conv_pieces():
                tts = range(t0 // 128, (t0 + n) // 128)
                tth = range(max(t0 - 128, lo) // 128, min(t0 + n + 128, hi) // 128)
                load_halo(cin, scT[512 + j * 128:512 + (j + 1) * 128, :], t0, n, lo, hi, "cin", "ld1",
                          [("scT", 4 + j, q) for q in tth])
                load_halo(hin, scT[1024 + j * 128:1024 + (j + 1) * 128, :], t0, n, lo, hi, "hin", "ld2",
                          [("scT", 8 + j, q) for q in tth])
                dma("sp", bin_[:, 0:n], scT[j * 128:(j + 1) * 128, t0:t0 + n], r=[("scT", j, q) for q in tts], w=["bin"], slot="ld3")
                tt("dve", cin[:, 0:n + 2], cin[:, 0:n + 2], hin[:, 0:n + 2], ALU.mult, r=["cin", "hin"], w=["cin"])
                ts("pool", acc[:, 0:n], cin[:, 1:n + 1], scw[:, j, 1:2], None, ALU.mult, None, r=["cin", "scw"], w=["acc"])
                stt_("dve", acc[:, 0:n], cin[:, 0:n], scw[:, j, 0:1], acc[:, 0:n], ALU.mult, ALU.add, r=["cin", "scw", "acc"], w=["acc"])
                stt_("dve", acc[:, 0:n], cin[:, 2:n + 2], scw[:, j, 2:3], acc[:, 0:n], ALU.mult, ALU.add, r=["cin", "scw", "acc"], w=["acc"])
                tt("pool", ob[:, 0:n], acc[:, 0:n], bin_[:, 0:n], ALU.mult, r=["acc", "bin"], w=["ob"])
                dma("st", mixT[1536 + j * 128:1536 + (j + 1) * 128, t0:t0 + n], ob[:, 0:n], r=["ob"],
                    w=[("mixT", 12 + j, q) for q in tts], slot="ev0")

    def phase_F(l):
        phase_begin()
        nonlocal gate
        hT = sb("hT", [128, KC, 2048], BF)
        wbuf = [sb("wbuf%d" % i, [128, KC, 512], BF) for i in range(2)]
        gate = sb("gate", [128, 2, 2048])
        xo = [sb("xo%d" % i, [128, 512]) for i in range(2)]
        t1 = [sb("t1%d" % i, [128, 512]) for i in range(2)]
        for st in range(2):
            dma("sp", gate[:, st, :], bc_rows(gsd[l, st, 2:3, :]), r=[("gsd", l)], w=["gate"], slot="ld%d" % (4 + st))
        for (t0, n) in blocks:
            nt = n // 128
            tt0 = t0 // 128
            st = 1 if t0 < CTX else 0
            for kc in range(KC):
                dma("sp", hT[:, kc, 0:n], mixT[kc * 128:(kc + 1) * 128, t0:t0 + n],
                    r=[("mixT", kc, q) for q in range(tt0, tt0 + nt)], w=[("hT", q) for q in range(nt)], slot="ld%d" % (kc % 4))
            for cg in range(4):
                b = rr(2)
                load_w(wbuf[b], w_out[l, :, cg * 512:(cg + 1) * 512], KC, ("wbuf", b), "wb%d" % b)
                for ti in range(nt):
                    pb = rr(6)
                    eb = rr(2)
                    rows = slice(t0 + ti * 128, t0 + (ti + 1) * 128)
                    dma("sp", xo[eb][:], xres[rows, cg * 512:(cg + 1) * 512], r=[("xres", tt0 + ti)], w=[("xo", eb)], slot="xo%d" % eb)
                    for kc in range(KC):
                        mm(psA[pb][:, :], hT[:, kc, ti * 128:(ti + 1) * 128], wbuf[b][:, kc, :], kc == 0, kc == KC - 1,
                           r=[("hT", ti), ("wbuf", b)], w=[PA[pb]])
                    tt("dve", t1[eb][:], psA[pb][:], gate[:, st, cg * 512:(cg + 1) * 512], ALU.mult, r=[PA[pb], "gate"], w=[("t1", eb)])
                    tt("pool", t1[eb][:], t1[eb][:], xo[eb][:], ALU.add, r=[("t1", eb), ("xo", eb)], w=[("t1", eb)])
                    dma("st", xres[rows, cg * 512:(cg + 1) * 512], t1[eb][:], r=[("t1", eb)], w=[("xres", tt0 + ti)], slot="ev%d" % eb)

    def phase_C(l):
        phase_begin()
        NL = T // 128
        gq = sb("gq", [128, 128])
        gk = sb("gk", [128, 128])
        esink = sb("esink", [128, 8])
        dma("sp", gq[:], bc_rows(qng[l:l + 1, :]), r=[], w=["gq"], slot="ld0")
        dma("sp", gk[:], bc_rows(kng[l:l + 1, :]), r=[], w=["gk"], slot="ld1")
        dma("sp", esink[:], bc_rows(sink_d[l:l + 1, :]), r=[], w=["esink"], slot="ld2")
        act(esink[:], esink[:], AF.Exp, r=["esink"], w=["esink"])
        mark = ar["off"]
        src = [sb("src%d" % i, [128, 1024]) for i in range(2)]
        sq2 = [sb("sq%d" % i, [128, 1024]) for i in range(2)]
        xn2 = [sb("xn%d" % i, [128, 1024]) for i in range(2)]
        t12 = [sb("t1_%d" % i, [128, 1024]) for i in range(2)]
        t22 = [sb("t2_%d" % i, [128, 1024]) for i in range(2)]
        cosb = [sb("cos%d" % i, [128, 1024]) for i in range(2)]
        sinb = [sb("sin%d" % i, [128, 1024]) for i in range(2)]
        ob2 = [sb("ob%d" % i, [128, 1024], BF) for i in range(2)]
        stg = [sb("stg%d" % i, [128, 8, 128], BF) for i in range(2)]
        st82 = [sb("st8_%d" % i, [128, 32]) for i in range(2)]
        pc = {"i": 0}

        def prep(tt_i, H, srcd, colkey, gt, dstT, dkey):
            pc["i"] += 1
            pb_ = pc["i"] % 2
            sq, xn, t1, t2, ob, st8 = sq2[pb_], xn2[pb_], t12[pb_], t22[pb_], ob2[pb_], st82[pb_]
            kq = lambda n_: (n_, pb_)
            b = pc["i"] % 2
            lat = tt_i >= CTX // 128
            rows = slice(tt_i * 128, (tt_i + 1) * 128)
            W = H * 128
            dma("sp", src[b][:, 0:W], srcd[rows, :], r=[(k_, tt_i) for k_ in colkey], w=[("src", b)], slot="ld%d" % (3 + b))
            if lat:
                lr = slice((tt_i - 2) * 128, (tt_i - 1) * 128)
                dma("sp", cosb[b][:, 0:W], ropec_d[lr, 0:W], r=[], w=[("cos", b)], slot="ld%d" % (5 + b))
                dma("sp", sinb[b][:, 0:W], ropes_d[lr, 0:W], r=[], w=[("sin", b)], slot="ld%d" % (7 + b))
            tt("pool", sq[:, 0:W], src[b][:, 0:W], src[b][:, 0:W], ALU.mult, r=[("src", b)], w=[kq("sq")])
            S.op("dve", lambda e: e.reduce_sum(out=st8[:, 0:H], in_=sq[:, 0:W].rearrange("p (h d) -> p h d", d=128), axis=AX.X),
                 r=[kq("sq")], w=[kq("st8")])
            rstd_from_ss(st8[:, 0:H], st8[:, 16:16 + H], 128, st8[:, 8:8 + H], [kq("st8")])
            v3 = lambda a: a[:, 0:W].rearrange("p (h d) -> p h d", d=128)
            tt("dve", v3(xn), v3(src[b]), bc_last(st8[:, 16:16 + H], 128), ALU.mult, r=[("src", b), kq("st8")], w=[kq("xn")])
            tt("pool", v3(xn), v3(xn), bc_mid(gt[:], H), ALU.mult, r=[kq("xn"), "gq", "gk"], w=[kq("xn")])
            if lat:
                tt("dve", t1[:, 0:W], xn[:, 0:W], cosb[b][:, 0:W], ALU.mult, r=[kq("xn"), ("cos", b)], w=[kq("t1")])
                v4 = lambda a: a[:, 0:W].rearrange("p (a two c) -> p a two c", two=2, c=32)
                tt("pool", v4(t2)[:, :, 0, :], v4(xn)[:, :, 1, :], v4(sinb[b])[:, :, 0, :], ALU.mult, r=[kq("xn"), ("sin", b)], w=[kq("t2")])
                tt("pool", v4(t2)[:, :, 1, :], v4(xn)[:, :, 0, :], v4(sinb[b])[:, :, 1, :], ALU.mult, r=[kq("xn"), ("sin", b)], w=[kq("t2")])
                tt("dve", ob[:, 0:W], t1[:, 0:W], t2[:, 0:W], ALU.add, r=[kq("t1"), kq("t2")], w=[kq("ob")])
            else:
                cp("dve", ob[:, 0:W], xn[:, 0:W], r=[kq("xn")], w=[kq("ob")])
            pt = rr(2)
            for h in range(H):
                tr(psT[pt][:, h * 128:(h + 1) * 128], ob[:, h * 128:(h + 1) * 128], cstb[:, IDN, :], r=[kq("ob"), "cstb"], w=[PT[pt]])
            sg = rr(2)
            cp("act", stg[sg][:, 0:H, :], psT[pt][:, 0:W].rearrange("p (h t) -> p h t", t=128), r=[PT[pt]], w=[("stg", sg)])
            dma("st", dstT[:, :, rows].rearrange("h d t -> d h t"), stg[sg][:, 0:H, :], r=[("stg", sg)], w=[(dkey, tt_i)], slot="ev%d" % sg)

        for tt_i in range(NTT):
            prep(tt_i, 2, k_raw, ["k_raw"], gk, kT, "kT")
            prep(tt_i, 8, q_raw, ["q_raw0", "q_raw1"], gq, qT, "qT")

        S.barrier()
        ar["off"] = mark
        kTg = sb("kTg", [128, TA], BF)
        vg = sb("vg", [128, NTT, 128], BF)
        q4 = [sb("q4%d" % i, [128, 4, 128], BF) for i in range(2)]
        pTb = [sb("pT%d" % i, [128, 512], BF) for i in range(3)]
        den = sb("den", [128, 512])
        osb = [sb("osb%d" % i, [128, 4, 128], BF) for i in range(2)]
        sc = 1.0 / math.sqrt(128.0)
        for g in range(2):
            dma("sp", kTg[:], kT[g], r=K("kT", range(NTT)), w=["kTg"], slot="ld0")
            dma("sp", vg[:], v_tok[:, g * 128:(g + 1) * 128].rearrange("(c p) d -> p c d", p=128),
                r=K("v_tok", range(NTT)), w=["vg"], slot="ld1")
            for qt in range(NTT):
                b = qt % 2
                rows = slice(qt * 128, (qt + 1) * 128)
                dma("sp", q4[b][:], qT[g * 4:(g + 1) * 4, :, rows].rearrange("h d t -> d h t"), r=[("qT", qt)], w=[("q4", b)], slot="ld%d" % (2 + b))
                chunks = [(0, None), (1, None)]
                if qt >= 2:
                    i = qt - 2
                    if i > 0:
                        chunks.append((qt - 1, GE))
                    chunks.append((qt, None))
                    if i < NL - 1:
                        chunks.append((qt + 1, LE))
                ops, dps = psA[2 + b], psA[4 + b]
                opk, dpk = PA[2 + b], PA[4 + b]
                q4f = q4[b][:].rearrange("p h t -> p (h t)")
                for ci, (kt, mk) in enumerate(chunks):
                    sb_i = rr(2)
                    pi = rr(3)
                    mm(psA[sb_i][:, :], kTg[:, kt * 128:(kt + 1) * 128], q4f, True, True, r=["kTg", ("q4", b)], w=[PA[sb_i]])
                    act(pTb[pi][:], psA[sb_i][:, :], AF.Exp, r=[PA[sb_i]], w=[("pT", pi)], scale=sc)
                    if mk is not None:
                        pv = pTb[pi][:].rearrange("p (h t) -> p h t", t=128)
                        tt("pool" if ci % 2 else "dve", pv, pv, bc_mid(cstb[:, mk, :], 4), ALU.mult, r=[("pT", pi), "cstb"], w=[("pT", pi)])
                    mm(ops[:, :], vg[:, kt, :], pTb[pi][:], ci == 0, ci == len(chunks) - 1, r=["vg", ("pT", pi)], w=[opk])
                    mm(dps[:, :], onesb[:], pTb[pi][:], ci == 0, ci == len(chunks) - 1, r=["onesb", ("pT", pi)], w=[dpk])
                d3 = den[:].rearrange("p (h t) -> p h t", t=128)
                tt("dve", d3, dps[:, :].rearrange("p (h t) -> p h t", t=128), bc_last(esink[:, g * 4:(g + 1) * 4], 128), ALU.add,
                   r=[dpk, "esink"], w=["den"])
                S.op("dve", lambda e: e.reciprocal(out=den[:], in_=den[:]), r=["den"], w=["den"])
                tt("dve", osb[b][:], ops[:, :].rearrange("p (h t) -> p h t", t=128), d3, ALU.mult, r=[opk, "den"], w=[("osb", b)])
                dma("st", mixT[g * 512:(g + 1) * 512, rows].rearrange("(h d) t -> d h t", d=128), osb[b][:], r=[("osb", b)],
                    w=[("mixT", g * 4 + h, qt) for h in range(4)], slot="ev%d" % b)

    def phase_D(l):
        phase_begin()
        NP = min(T, 2048) + 2
        xin = sb("xin", [128, NP])
        acc = sb("acc", [128, NP])
        cvo = sb("cvo", [128, NP], BF)
        cw = sb("cw", [128, 8, 3])
        cbias = sb("cbias", [128, 8])
        stg = [sb("stgd%d" % i, [128, 8, 128], BF) for i in range(2)]
        dma("sp", cw[:], convw_d[l], r=[], w=["cw"], slot="ld0")
        dma("sp", cbias[:], convb_d[l], r=[], w=["cbias"], slot="ld1")
        for j in range(8):
            for (t0, n, lo, hi) in conv_pieces():
                tts = range(t0 // 128, (t0 + n) // 128)
                tth = range(max(t0 - 128, lo) // 128, min(t0 + n + 128, hi) // 128)
                load_halo(xin, xbcT[j * 128:(j + 1) * 128, :], t0, n, lo, hi, "xin", "ld2", [("xbcT", j, q) for q in tth])
                ts("dve", acc[:, 0:n], xin[:, 1:n + 1], cw[:, j, 1:2], cbias[:, j:j + 1], ALU.mult, ALU.add, r=["xin", "cw", "cbias"], w=["acc"])
                stt_("dve", acc[:, 0:n], xin[:, 0:n], cw[:, j, 0:1], acc[:, 0:n], ALU.mult, ALU.add, r=["xin", "cw", "acc"], w=["acc"])
                stt_("dve", acc[:, 0:n], xin[:, 2:n + 2], cw[:, j, 2:3], acc[:, 0:n], ALU.mult, ALU.add, r=["xin", "cw", "acc"], w=["acc"])
                act(cvo[:, 0:n], acc[:, 0:n], AF.Silu, r=["acc"], w=["cvo"])
                dma("st", convT[j * 128:(j + 1) * 128, t0:t0 + n], cvo[:, 0:n], r=["cvo"], w=[("convT", j, q) for q in tts], slot="ev0")
                if j < 6:
                    for g8 in range(0, n // 128, 8):
                        m = min(8, n // 128 - g8)
                        pt = rr(2)
                        sg = rr(2)
                        for q in range(m):
                            tr(psT[pt][:, q * 128:(q + 1) * 128], cvo[:, (g8 + q) * 128:(g8 + q + 1) * 128], cstb[:, IDN, :],
                               r=["cvo", "cstb"], w=[PT[pt]])
                        cp("act", stg[sg][:, 0:m, :], psT[pt][:, 0:m * 128].rearrange("p (c d) -> p c d", d=128), r=[PT[pt]], w=[("stgd", sg)])
                        r0 = t0 + g8 * 128
                        dma("st", xbc_tok[r0:r0 + m * 128, j * 128:(j + 1) * 128].rearrange("(c p) d -> p c d", p=128), stg[sg][:, 0:m, :],
                            r=[("stgd", sg)], w=[("xbc_tok", j, r0 // 128 + q) for q in range(m)], slot="ev%d" % (1 + sg))
        phase_begin()
        dtb = sb("dtb", [128, 16])
        A16 = sb("A16", [128, 16])
        Dsk = sb("Dsk", [128, 8])
        gn = sb("gn", [128, 512])
        dma("sp", dtb[:], bc_rows(dtb_d[l:l + 1, :]), r=[], w=["dtb"], slot="ld0")
        dma("sp", A16[:], bc_rows(alog_d[l:l + 1, :]), r=[], w=["A16"], slot="ld1")
        dma("sp", Dsk[:], bc_rows(dsk_d[l:l + 1, :]), r=[], w=["Dsk"], slot="ld2")
        dma("sp", gn[:], bc_rows(sng_d[l:l + 1, :]), r=[], w=["gn"], slot="ld3")
        act(A16[:], A16[:], AF.Exp, r=["A16"], w=["A16"])
        ts("dve", A16[:], A16[:], -1.0, None, ALU.mult, None, r=["A16"], w=["A16"])
        NLB = 3
        dtr = [sb("dtr%d" % i, [128, 16]) for i in range(NLB)]
        BT = [sb("BT%d" % i, [128, 2, 128], BF) for i in range(NLB)]
        CT = [sb("CT%d" % i, [128, 2, 128], BF) for i in range(NLB)]
        xsb = [sb("xsb%d" % i, [128, 512], BF) for i in range(NLB)]
        Btk = [sb("Btk%d" % i, [128, 256], BF) for i in range(NLB)]
        sm = sb("sm", [128, 64])
        sm2 = sb("sm2", [128, 16])
        ex = [sb("ex%d" % i, [128, 24]) for i in range(2)]
        lt = sb("lt", [128, 8, 128])
        dec = sb("dec", [128, 8, 128])
        cbm = sb("cbm", [128, 2, 128])
        MT = [sb("MT%d" % i, [128, 8, 128], BF) for i in range(2)]
        xdt = [sb("xdt%d" % i, [128, 512], BF) for i in range(2)]
        xw = [sb("xw%d" % i, [128, 512], BF) for i in range(2)]
        t1 = sb("t1d", [128, 512])
        ysb = [sb("ysb%d" % i, [128, 512]) for i in range(2)]
        hst = sb("hst", [128, 512])
        hTb = sb("hTb", [128, 512], BF)
        yfl = sb("yfl", [128, 512])
        zl = sb("zl", [128, 512])
        u = sb("u", [128, 512])
        ub = sb("ub", [128, 512], BF)
        stg2 = [sb("stg2%d" % i, [128, 4, 128], BF) for i in range(2)]
        v8 = lambda a: a.rearrange("p (h d) -> p h d", d=64)
        p0, pS0, pS1, pY, pO, pS2 = psA
        k0, kS0, kS1, kY, kO, kS2 = PA

        def loads(c, b):
            rows = slice(c * 128, (c + 1) * 128)
            dma("sp", dtr[b][:], dt_raw[rows, :], r=[("dt_raw", c)], w=[("dtr", b)], slot="ld%d" % (4 + b))
            dma("sp", BT[b][:], convT[512:768, rows].rearrange("(g n) t -> n g t", n=128), r=[("convT", 4, c), ("convT", 5, c)], w=[("BT", b)], slot="ld%d" % (7 + b))
            dma("sp", CT[b][:], convT[768:1024, rows].rearrange("(g n) t -> n g t", n=128), r=[("convT", 6, c), ("convT", 7, c)], w=[("CT", b)], slot="ld%d" % (10 + b))
            dma("sp", xsb[b][:], xbc_tok[rows, 0:512], r=[("xbc_tok", j, c) for j in range(4)], w=[("xsb", b)], slot="ld%d" % (13 + b))
            dma("sp", Btk[b][:], xbc_tok[rows, 512:768], r=[("xbc_tok", j, c) for j in (4, 5)], w=[("Btk", b)], slot="ld%d" % (16 + b))

        for d in range(2):
            Ud, Ld, Md = (LE, GT, LE) if d == 0 else (GE, LT, GE)
            order = list(range(NTT)) if d == 0 else [1, 0] + list(range(NTT - 1, 1, -1))
            S.op("pool", lambda e: e.memset(hst[:], 0.0), w=["hst"])
            S.op("pool", lambda e: e.memset(hTb[:], 0.0), w=["hTb"])

            def stageA(oi):
                b = oi % NLB
                ab = oi % 2
                dt8, dta8, dtw = sm[:, 0:8], sm[:, 8:16], sm[:, 16:24]
                tt("dve", sm[:, 24:32], dtr[b][:, d * 8:(d + 1) * 8], dtb[:, d * 8:(d + 1) * 8], ALU.add, r=[("dtr", b), "dtb"], w=["sm"])
                act(sm[:, 24:32], sm[:, 24:32], AF.Exp, r=["sm"], w=["sm"])
                ts("dve", sm[:, 24:32], sm[:, 24:32], 1.0, None, ALU.add, None, r=["sm"], w=["sm"])
                act(dt8, sm[:, 24:32], AF.Ln, r=["sm"], w=["sm"])
                tt("dve", dta8, dt8, A16[:, d * 8:(d + 1) * 8], ALU.mult, r=["sm", "A16"], w=["sm"])
                mm(p0[:, 0:8], cst[:, Ud, :], dta8, True, True, r=["cst", "sm"], w=[k0])
                mm(p0[:, 8:16], cst[:, Ld, :], dta8, True, True, r=["cst", "sm"], w=[k0])
                mm(p0[:, 16:24], ones32[:], dta8, True, True, r=["ones32", "sm"], w=[k0])
                for g in range(2):
                    mm(p0[:, 128 + g * 128:256 + g * 128], BT[b][:, g, :], CT[b][:, g, :], True, True, r=[("BT", b), ("CT", b)], w=[k0])
                act(ex[ab][:], p0[:, 0:24], AF.Exp, r=[k0], w=[("ex", ab)])
                tt("dve", cbm[:], p0[:, 128:384].rearrange("p (g t) -> p g t", t=128), bc_mid(cst[:, Md, :], 2), ALU.mult,
                   r=[k0, "cst", ("ex", ab)], w=["cbm"])
                tt("pool", lt[:], bc_mid(cst[:, Ld, :], 8), bc_last(dta8, 128), ALU.mult, r=["cst", "sm"], w=["lt"])
                for h in range(8):
                    ps_, pk_ = (pS0, kS0) if h < 4 else (pS1, kS1)
                    mm(ps_[:, (h % 4) * 128:(h % 4 + 1) * 128], lt[:, h, :], cst[:, Ud, :], True, True, r=["lt", "cst"], w=[pk_])
                act(dec[:, 0:4, :], pS0[:, :].rearrange("p (h t) -> p h t", t=128), AF.Exp, r=[kS0], w=["dec0"])
                act(dec[:, 4:8, :], pS1[:, :].rearrange("p (h t) -> p h t", t=128), AF.Exp, r=[kS1], w=["dec1"])
                tt("dve", MT[ab][:, 0:4, :], dec[:, 0:4, :], bc_mid(cbm[:, 0, :], 4), ALU.mult, r=["dec0", "cbm"], w=[("MT0", ab)])
                tt("pool", MT[ab][:, 4:8, :], dec[:, 4:8, :], bc_mid(cbm[:, 1, :], 4), ALU.mult, r=["dec1", "cbm"], w=[("MT1", ab)])
                tt("dve", v8(xdt[ab][:]), v8(xsb[b][:]), bc_last(dt8, 64), ALU.mult, r=[("xsb", b), "sm"], w=[("xdt", ab)])
                tt("dve", dtw, dt8, ex[ab][:, 8:16], ALU.mult, r=["sm", ("ex", ab)], w=["sm"])
                tt("pool", v8(xw[ab][:]), v8(xsb[b][:]), bc_last(dtw, 64), ALU.mult, r=[("xsb", b), "sm"], w=[("xw", ab)])

            def stageB(oi, c):
                b = oi % NLB
                ab = oi % 2
                rows = slice(c * 128, (c + 1) * 128)
                for h in range(8):
                    mm(pY[:, h * 64:(h + 1) * 64], MT[ab][:, h, :], xdt[ab][:, h * 64:(h + 1) * 64], True, True,
                       r=[("MT0", ab) if h < 4 else ("MT1", ab), ("xdt", ab)], w=[kY])
                for g in range(2):
                    mm(pO[:, g * 256:(g + 1) * 256], CT[b][:, g, :], hTb[:, g * 256:(g + 1) * 256], True, True, r=[("CT", b), "hTb"], w=[kO])
                for g in range(2):
                    mm(pS2[:, g * 256:(g + 1) * 256], Btk[b][:, g * 128:(g + 1) * 128], xw[ab][:, g * 256:(g + 1) * 256], True, True,
                       r=[("Btk", b), ("xw", ab)], w=[kS2])
                tt("dve", v8(t1[:]), v8(pO[:, :]), bc_last(ex[ab][:, 0:8], 64), ALU.mult, r=[kO, ("ex", ab)], w=["t1d"])
                yb = ysb[oi % 2]
                tt("dve", yb[:], pY[:, :], t1[:], ALU.add, r=[kY, "t1d"], w=[("ysb", oi % 2)])
                tt("pool", v8(hst[:]), v8(hst[:]), bc_last(ex[ab][:, 16:24], 64), ALU.mult, r=["hst", ("ex", ab)], w=["hst"])
                tt("dve", hst[:], hst[:], pS2[:, :], ALU.add, r=["hst", kS2], w=["hst"])
                cp("act", hTb[:], hst[:], r=["hst"], w=["hTb"])
                if d == 0:
                    dma("st", y_f[rows, :], yb[:], r=[("ysb", oi % 2)], w=[("y_f", c)], slot="ev%d" % (oi % 2))
                else:
                    dma("sp", yfl[:], y_f[rows, :], r=[("y_f", c)], w=["yfl"], slot="ld19")
                    dma("sp", zl[:], z_tok[rows, :], r=[("z_tok", c)], w=["zl"], slot="ld20")
                    tt("pool", yb[:], yb[:], yfl[:], ALU.add, r=[("ysb", oi % 2), "yfl"], w=[("ysb", oi % 2)])
                    tt("dve", v8(u[:]), v8(xsb[b][:]), bc_last(Dsk[:], 64), ALU.mult, r=[("xsb", b), "Dsk"], w=["u"])
                    tt("dve", yb[:], yb[:], u[:], ALU.add, r=[("ysb", oi % 2), "u"], w=[("ysb", oi % 2)])
                    act(zl[:], zl[:], AF.Silu, r=["zl"], w=["zl"])
                    tt("dve", u[:], yb[:], zl[:], ALU.mult, r=[("ysb", oi % 2), "zl"], w=["u"])
                    tt("pool", t1[:], u[:], u[:], ALU.mult, r=["u"], w=["t1d"])
                    S.op("dve", lambda e: e.reduce_sum(out=sm2[:, 0:2], in_=t1[:].rearrange("p (g c) -> p g c", c=256), axis=AX.X),
                         r=["t1d"], w=["sm2"])
                    rstd_from_ss(sm2[:, 0:2], sm2[:, 4:6], 256, sm2[:, 2:4], ["sm2"])
                    tt("dve", u[:].rearrange("p (g c) -> p g c", c=256), u[:].rearrange("p (g c) -> p g c", c=256),
                       bc_last(sm2[:, 4:6], 256), ALU.mult, r=["u", "sm2"], w=["u"])
                    tt("pool", ub[:], u[:], gn[:], ALU.mult, r=["u", "gn"], w=["ub"])
                    pt = rr(2)
                    sg = rr(2)
                    for j in range(4):
                        tr(psT[pt][:, j * 128:(j + 1) * 128], ub[:, j * 128:(j + 1) * 128], cstb[:, IDN, :], r=["ub", "cstb"], w=[PT[pt]])
                    cp("act", stg2[sg][:], psT[pt][:, 0:512].rearrange("p (j t) -> p j t", t=128), r=[PT[pt]], w=[("stg2", sg)])
                    dma("st", mixT[1024:1536, rows].rearrange("(j c) t -> c j t", c=128), stg2[sg][:], r=[("stg2", sg)],
                        w=[("mixT", 8 + j, c) for j in range(4)], slot="ev%d" % (2 + sg))

            n_o = len(order)
            loads(order[0], 0)
            if n_o > 1:
                loads(order[1], 1)
            stageA(0)
            for oi, c in enumerate(order):
                if oi + 2 < n_o:
                    loads(order[oi + 2], (oi + 2) % NLB)
                if oi + 1 < n_o:
                    stageA(oi + 1)
                stageB(oi, c)

    sets = [(0, CTX, cap_c)] + [(CTX, T, cap_l)]
    slot_tiles = []
    col = 0
    for si, (_, _, cap) in enumerate(sets):
        for s0 in range(0, cap, 128):
            ns = min(128, cap - s0)
            slot_tiles.append((si, s0, ns, col))
            col += ns
    NS_TOT = col
    NSL = len(slot_tiles)

    def phase_GH(l):
        phase_begin()
        nonlocal xt, h32, junk, GS, stt, gate
        slotidx = sb("slotidx", [128, NSL, 16], U32)
        gvs = sb("gvs", [128, NSL, 16])
        gate = sb("gate", [128, 2, 2048])
        markH = ar["off"]
        g = alloc_norm()
        xt, h32, junk, GS, stt = g["xt"], g["h32"], g["junk"], g["GS"], g["stt"]
        h2f = [sb("h2f%d" % i, [128, 2048]) for i in range(2)]
        h2b = [sb("h2b%d" % i, [128, 2048], BF) for i in range(2)]
        h2T = [sb("h2T%d" % i, [128, KC, 128]) for i in range(2)]
        wr = sb("wr", [128, KC, 16])
        smg = [sb("smg%d" % i, [128, 64]) for i in range(2)]
        affT = sb("affT", [16, TA])
        work = sb("work", [16, max(T, CTX)])
        maxcap = max(cap_l, cap_c)
        vals = sb("vals", [16, maxcap])
        idxu = sb("idxu", [16, maxcap], U32)
        idxf = sb("idxf", [16, maxcap])
        slotf = sb("slotf", [128, 16])
        load_GS(l, 1, True)
        dma("sp", wr[:], w_r[l].rearrange("(kc p) e -> p kc e", p=128), r=[], w=["wr"], slot="ld6")
        for tt_i in range(NTT):
            dma("sp", moe_d[tt_i * 128:(tt_i + 1) * 128, :], zero32[:], r=["zero32"], w=[("moe", tt_i)], slot="cp%d" % (tt_i % 4))
        def g_front(tt_i):
            if tt_i + 1 < NTT:
                load_x(tt_i + 1)
            rows = slice(tt_i * 128, (tt_i + 1) * 128)
            hb_i = tt_i % 2
            norm_tile(tt_i, None, h2f[hb_i][:], ("h2f", hb_i))
            cp("act", h2b[hb_i][:], h2f[hb_i][:], r=[("h2f", hb_i)], w=[("h2b", hb_i)])
            dma("st", h2_d[rows, :], h2b[hb_i][:], r=[("h2b", hb_i)], w=[("h2", tt_i)], slot="ev%d" % hb_i)
            for q in range(4):
                for j in range(4):
                    kc = q * 4 + j
                    tr(psA[q][:, j * 128:(j + 1) * 128], h2f[hb_i][:, kc * 128:(kc + 1) * 128], cst[:, IDN, :], r=[("h2f", hb_i), "cst"], w=[PA[q]])
                cp("act" if q % 2 else "dve", h2T[hb_i][:, q * 4:(q + 1) * 4, :], psA[q][:, :].rearrange("p (k t) -> p k t", t=128),
                   r=[PA[q]], w=[("h2T", hb_i)])
            for kc in range(KC):
                mm(psA[4 + hb_i][:, 0:16], h2T[hb_i][:, kc, :], wr[:, kc, :], kc == 0, kc == KC - 1, r=[("h2T", hb_i), "wr"], w=[PA[4 + hb_i]])

        def g_back(tt_i):
            b = tt_i % 2
            sm = smg[b]
            kk = ("smg", b)
            pl, pk = psA[4 + b], PA[4 + b]
            S.op("dve", lambda e: e.reduce_max(out=sm[:, 0:1], in_=pl[:, 0:16], axis=AX.X), r=[pk], w=[kk])
            ts("dve", sm[:, 1:2], sm[:, 0:1], -1.0, None, ALU.mult, None, r=[kk], w=[kk])
            S.op("dve", lambda e: e.memset(sm[:, 2:3], 0.0), r=[kk], w=[kk])
            act(sm[:, 16:32], pl[:, 0:16], AF.Exp, r=[pk, kk], w=[kk], bias=sm[:, 1:2], accum_out=sm[:, 2:3])
            S.op("dve", lambda e: e.reciprocal(out=sm[:, 3:4], in_=sm[:, 2:3]), r=[kk], w=[kk])
            ts("dve", sm[:, 32:48], sm[:, 16:32], sm[:, 3:4], None, ALU.mult, None, r=[kk], w=[kk])
            tr(pl[0:16, 128:256], sm[:, 32:48], cst[:, IDN, :], r=[kk, "cst"], w=[pk])
            cp("dve", affT[:, tt_i * 128:(tt_i + 1) * 128], pl[0:16, 128:256], r=[pk], w=["affT"])

        load_x(0)
        g_front(0)
        for tt_i in range(NTT):
            if tt_i + 1 < NTT:
                g_front(tt_i + 1)
            g_back(tt_i)
        for si, (toff, n, cap) in enumerate(sets):
            cp("dve", work[:, 0:n], affT[:, toff:toff + n], r=["affT"], w=["work"])
            for r8 in range(cap // 8):
                sl = slice(r8 * 8, (r8 + 1) * 8)
                S.op("dve", lambda e, sl=sl, n=n: e.max(out=vals[:, sl], in_=work[:, 0:n]), r=["work"], w=["vals"])
                S.op("dve", lambda e, sl=sl, n=n: e.max_index(out=idxu[:, sl], in_max=vals[:, sl], in_values=work[:, 0:n]), r=["work", "vals"], w=["idxu"])
                S.op("dve", lambda e, sl=sl, n=n: e.match_replace(out=work[:, 0:n], in_to_replace=vals[:, sl], in_values=work[:, 0:n], imm_value=-1.0),
                     r=["work", "vals", "idxu"], w=["work"])
            cp("dve", idxf[:, 0:cap], idxu[:, 0:cap], r=["idxu"], w=["idxf"])
            ts("dve", idxf[:, 0:cap], idxf[:, 0:cap], float(toff), None, ALU.add, None, r=["idxf"], w=["idxf"])
            for sti, (sj, s0, ns, c0) in enumerate(slot_tiles):
                if sj != si:
                    continue
                tr(psA[0][0:ns, 0:16], idxf[:, s0:s0 + ns], cst[0:16, IDN, 0:16], r=["idxf", "cst"], w=[PA[0]])
                cp("dve", slotf[0:ns, :], psA[0][0:ns, 0:16], r=[PA[0]], w=["slotf"])
                cp("dve", slotidx[0:ns, sti, :], slotf[0:ns, :], r=["slotf"], w=["slotidx"])
                tr(psA[1][0:ns, 0:16], vals[:, s0:s0 + ns], cst[0:16, IDN, 0:16], r=["vals", "cst"], w=[PA[1]])
                cp("dve", gvs[0:ns, sti, :], psA[1][0:ns, 0:16], r=[PA[1]], w=["gvs"])
        if debug:
            dbg_aff = nc.dram_tensor("dbg_aff", [16, TA], F32, kind="ExternalOutput").ap()
            dbg_idx = nc.dram_tensor("dbg_idx", [128, NSL, 16], U32, kind="ExternalOutput").ap()
            dbg_gv = nc.dram_tensor("dbg_gv", [128, NSL, 16], F32, kind="ExternalOutput").ap()
            dma("sp", dbg_aff, affT[:], r=["affT"], w=["dbg_aff"], slot="ld0")
            dma("sp", dbg_idx, slotidx[:], r=["slotidx"], w=["dbg_idx"], slot="ld1")
            dma("sp", dbg_gv, gvs[:], r=["gvs"], w=["dbg_gv"], slot="ld2")
        S.barrier()
        ar["off"] = markH
        xgT = sb("xgT", [128, KC, NS_TOT], BF)
        hTe = sb("hTe", [128, 8, NS_TOT], BF)
        wg = [sb("wg%d" % i, [128, KC, 512], BF) for i in range(2)]
        wu = [sb("wu%d" % i, [128, KC, 512], BF) for i in range(2)]
        wd = [sb("wd%d" % i, [128, 8, 1024], BF) for i in range(2)]
        xg = [sb("xg%d" % i, [128, 2048], BF) for i in range(NSL)]
        ysb = [sb("ysbm%d" % i, [128, 2048]) for i in range(2)]
        sgt = [sb("sgt%d" % i, [128, 512]) for i in range(2)]
        cchunks = []
        c = 0
        for si, (_, _, cap) in enumerate(sets):
            for a in range(0, cap, 512):
                w_ = min(512, cap - a)
                cchunks.append((c + a, w_))
            c += cap
        def gathers(e_i):
            for sti, (sj, s0, ns, c0) in enumerate(slot_tiles):
                S.op("pool", lambda e, ns=ns, sti=sti, e_i=e_i: e.indirect_dma_start(
                    out=xg[sti][0:ns, :], out_offset=None, in_=h2_d[:, :],
                    in_offset=bass.IndirectOffsetOnAxis(ap=slotidx[0:ns, sti, e_i:e_i + 1], axis=0)),
                    r=K("h2", range(NTT)) + ["slotidx"], w=[("xg", sti)], slot="xg%d" % sti)

        def transposes(e_i):
            for sti, (sj, s0, ns, c0) in enumerate(slot_tiles):
                for half in range(2):
                    pt = rr(2)
                    for j in range(8):
                        kc = half * 8 + j
                        tr(psT[pt][:, j * 128:j * 128 + ns], xg[sti][0:ns, kc * 128:(kc + 1) * 128], cstb[0:ns, IDN, 0:ns],
                           r=[("xg", sti), "cstb"], w=[PT[pt]])
                    cp("act" if half else "dve", xgT[:, half * 8:(half + 1) * 8, c0:c0 + ns],
                       psT[pt][:, :].rearrange("p (k t) -> p k t", t=128)[:, :, 0:ns], r=[PT[pt]], w=["xgT"])

        def load_up(e_i, f4):
            load_w(wg[f4], w_eg[l, e_i, :, f4 * 512:(f4 + 1) * 512], KC, ("wg", f4), "wg%d" % f4)
            load_w(wu[f4], w_eu[l, e_i, :, f4 * 512:(f4 + 1) * 512], KC, ("wu", f4), "wu%d" % f4)

        def load_down(e_i):
            for half in range(2):
                load_w(wd[half], w_ed[l, e_i, :, half * 1024:(half + 1) * 1024], 8, ("wd", half), "wd%d" % half)

        def up(e_i, f4):
            b = f4
            for fc in range(4):
                for (cc0, cw_) in cchunks:
                    pg, pu = (0, 1) if rr(2) else (2, 3)
                    for kc in range(KC):
                        mm(psA[pg][:, 0:cw_], wg[b][:, kc, fc * 128:(fc + 1) * 128], xgT[:, kc, cc0:cc0 + cw_], kc == 0, kc == KC - 1,
                           r=[("wg", b), "xgT"], w=[PA[pg]])
                    for kc in range(KC):
                        mm(psA[pu][:, 0:cw_], wu[b][:, kc, fc * 128:(fc + 1) * 128], xgT[:, kc, cc0:cc0 + cw_], kc == 0, kc == KC - 1,
                           r=[("wu", b), "xgT"], w=[PA[pu]])
                    sgi = rr(2)
                    act(sgt[sgi][:, 0:cw_], psA[pg][:, 0:cw_], AF.Silu, r=[PA[pg]], w=[("sgt", sgi)])
                    tt("dve", hTe[:, f4 * 4 + fc, cc0:cc0 + cw_], sgt[sgi][:, 0:cw_], psA[pu][:, 0:cw_], ALU.mult,
                       r=[("sgt", sgi), PA[pu]], w=["hTe"])

        def down(e_i):
            for sti, (sj, s0, ns, c0) in enumerate(slot_tiles):
                yb = rr(2)
                for cg in range(4):
                    b = cg // 2
                    pb = 4 + (cg % 2)
                    for fc in range(8):
                        mm(psA[pb][0:ns, :], hTe[:, fc, c0:c0 + ns], wd[b][:, fc, (cg % 2) * 512:(cg % 2 + 1) * 512], fc == 0, fc == 7,
                           r=["hTe", ("wd", b)], w=[PA[pb]])
                    ts("dve", ysb[yb][0:ns, cg * 512:(cg + 1) * 512], psA[pb][0:ns, :], gvs[0:ns, sti, e_i:e_i + 1], None,
                       ALU.mult, None, r=[PA[pb], "gvs"], w=[("ysbm", yb)])
                S.op("pool", lambda e, yb=yb, ns=ns, sti=sti, e_i=e_i: e.indirect_dma_start(
                    out=moe_d[:, :], out_offset=bass.IndirectOffsetOnAxis(ap=slotidx[0:ns, sti, e_i:e_i + 1], axis=0),
                    in_=ysb[yb][0:ns, :], in_offset=None, compute_op=ALU.add),
                    r=[("ysbm", yb), "slotidx"], w=K("moe", range(NTT)), slot="sc%d" % yb)

        gathers(0)
        load_up(0, 0)
        for e_i in range(NE):
            transposes(e_i)
            load_up(e_i, 1)
            up(e_i, 0)
            load_down(e_i)
            up(e_i, 1)
            if e_i + 1 < NE:
                gathers(e_i + 1)
                load_up(e_i + 1, 0)
            down(e_i)
        S.barrier()
        ar["off"] = markH
        xo = [sb("xo%d" % i, [128, 2048]) for i in range(2)]
        mo = [sb("mo%d" % i, [128, 2048]) for i in range(2)]
        for tt_i in range(NTT):
            b = tt_i % 2
            st = 1 if tt_i < CTX // 128 else 0
            rows = slice(tt_i * 128, (tt_i + 1) * 128)
            dma("sp", xo[b][:], xres[rows, :], r=[("xres", tt_i)], w=[("xo", b)], slot="xo%d" % b)
            dma("sp", mo[b][:], moe_d[rows, :], r=[("moe", tt_i)], w=[("mo", b)], slot="mo%d" % b)
            tt("dve", mo[b][:], mo[b][:], gate[:, st, :], ALU.mult, r=[("mo", b), "gate"], w=[("mo", b)])
            tt("pool", xo[b][:], xo[b][:], mo[b][:], ALU.add, r=[("xo", b), ("mo", b)], w=[("xo", b)])
            dma("st", xres[rows, :], xo[b][:], r=[("xo", b)], w=[("xres", tt_i)], slot="ev%d" % b)

    zb = sb("zb", [128, 2048], BF)
    S.op("pool", lambda e: e.memset(zb[:], 0.0), w=["zb"])
    ar["perm"] = ar["off"]
    for j4 in range(4):
        for c0 in range(0, TA, 2048):
            n = min(2048, TA - c0)
            dma("sp", mixT[1024 + j4 * 128:1024 + (j4 + 1) * 128, c0:c0 + n], zb[:, 0:n], r=["zb"],
                w=[("mixT", 8 + j4, q) for q in range(c0 // 128, (c0 + n) // 128)], slot="cp%d" % (j4 % 4))
    phase0()
    for l in range(L):
        if upto < 1:
            continue
        phase_AB(l)
        if upto < 2:
            continue
        phase_E(l)
        if upto < 3:
            continue
        phase_C(l)
        if upto < 4:
            continue
        phase_D(l)
        if upto < 5:
            continue
        phase_F(l)
        if upto < 6:
            continue
        phase_GH(l)
    phase_begin()
    for tt_i in range(T // 128):
        rows = slice(tt_i * 128, (tt_i + 1) * 128)
        dma("sp", out_d[rows, :], xres[CTX + tt_i * 128:CTX + (tt_i + 1) * 128, :], r=[("xres", CTX // 128 + tt_i)],
            w=[("out", tt_i)], slot="cp%d" % (tt_i % 4))
    stats = S.emit(nc)
    es.close()
    return nc, stats


def host_inputs(inputs, T, L):
    f = np.float32
    r = {}
    p = np.arange(128)[:, None]
    j = np.arange(128)[None, :]
    consts = np.stack([(p == j), (p <= j), (p >= j), (p > j), (p < j)], axis=1).astype(f)
    r["consts"] = np.ascontiguousarray(consts)
    rows = T // 64
    row = np.repeat(np.arange(rows), 64).astype(f)
    col = np.tile(np.arange(64), rows).astype(f)
    inv = (10000.0 ** (-np.arange(32, dtype=f) / 32)).astype(f)
    ar = row[:, None] * inv
    ac = col[:, None] * inv
    cos = np.concatenate([np.cos(ar), np.cos(ar), np.cos(ac), np.cos(ac)], axis=1).astype(f)
    sin = np.concatenate([-np.sin(ar), np.sin(ar), -np.sin(ac), np.sin(ac)], axis=1).astype(f)
    r["ropec"] = np.ascontiguousarray(np.tile(cos, (1, 8)))
    r["ropes"] = np.ascontiguousarray(np.tile(sin, (1, 8)))
    for k in ("w_mod", "b_mod", "w_in", "w_out", "w_router", "w_expert_gate", "w_expert_up", "w_expert_down",
              "norm1_g", "norm2_g", "q_norm_g", "k_norm_g", "attn_sink", "ssd_d", "ssd_norm_g"):
        r[k] = np.ascontiguousarray(np.asarray(inputs[k], dtype=f)[:L])
    r["ssd_dt_bias"] = np.ascontiguousarray(np.asarray(inputs["ssd_dt_bias"], f)[:L].reshape(L, 16))
    r["ssd_a_log"] = np.ascontiguousarray(np.asarray(inputs["ssd_a_log"], f)[:L].reshape(L, 16))
    cw = np.asarray(inputs["ssd_conv_w"], f)[:L]
    r["convw"] = np.ascontiguousarray(cw.reshape(L, 3, 8, 128).transpose(0, 3, 2, 1))
    r["convb"] = np.ascontiguousarray(np.asarray(inputs["ssd_conv_b"], f)[:L].reshape(L, 8, 128).transpose(0, 2, 1))
    sw = np.asarray(inputs["sc_conv_w"], f)[:L]
    r["scw"] = np.ascontiguousarray(sw.reshape(L, 3, 4, 128).transpose(0, 3, 2, 1))
    x = np.asarray(inputs["x"], f)
    ctx = np.asarray(inputs["ctx"], f)
    c = np.asarray(inputs["c"], f)
    cc = np.asarray(inputs["c_ctx"], f)
    maps = []
    for b in range(x.shape[0]):
        m = dict(r)
        m["xall"] = np.ascontiguousarray(np.concatenate([ctx[b], x[b, :T]], axis=0))
        cs = np.stack([c[b], cc], axis=0)
        m["cT"] = np.ascontiguousarray(cs.reshape(2, KC, 128).transpose(2, 1, 0))
        maps.append(m)
    return maps


_CACHE = {}


def kernel(**inputs):
    T = inputs["x"].shape[1]
    L = inputs["w_mod"].shape[0]
    B = inputs["x"].shape[0]
    key = (T, L)
    if key not in _CACHE:
        _CACHE[key] = build(T, L)[0]
    nc = _CACHE[key]
    maps = host_inputs(inputs, T, L)
    res = run_bass_kernel_spmd(nc, maps, core_ids=list(range(B)))
    return np.stack([np.asarray(res.results[b]["out"]) for b in range(B)], axis=0).astype(np.float32)
```

```python
import math
from contextlib import ExitStack

import numpy as np
import concourse.bass as bass
import concourse.mybir as mybir
from concourse.bass_utils import run_bass_kernel_spmd

F32 = mybir.dt.float32
BF = mybir.dt.bfloat16
U32 = mybir.dt.uint32
AF = mybir.ActivationFunctionType
ALU = mybir.AluOpType
AX = mybir.AxisListType

D = 2048
KC = 16
CTX = 256
NE = 16
FF = 1024
EPS = 1e-6
C_Q, C_K, C_V, C_Z, C_XBC, C_DT, C_SB, C_SC, C_SH, C_END = 0, 1024, 1280, 1536, 2048, 3072, 3088, 3600, 4112, 4624

SAME_ENGINE_SYNC = True
EPOCH = 30000


class Op:
    __slots__ = ("eng", "fn", "slot", "deps", "needs_inc", "val", "idx", "ep")

    def __init__(self, eng, fn, slot):
        self.eng, self.fn, self.slot = eng, fn, slot
        self.deps = set()
        self.needs_inc = False
        self.val = 0


class Sch:
    ENGS = ("pe", "act", "dve", "pool", "sp")

    def __init__(self):
        self.ops = []
        self.last_w = {}
        self.readers = {}
        self.slot_last = {}

    def barrier(self):
        last = {}
        for o in self.ops:
            if o.fn is None:
                continue
            last[o.slot if o.slot is not None else ("eng", o.eng)] = o
        for e in self.ENGS:
            b = Op(e, None, None)
            b.idx = len(self.ops)
            b.deps = set(last.values())
            self.ops.append(b)

    def op(self, eng, fn, r=(), w=(), slot=None):
        o = Op(eng, fn, slot)
        o.idx = len(self.ops)
        deps = o.deps
        for k in r:
            lw = self.last_w.get(k)
            if lw is not None:
                deps.add(lw)
        for k in w:
            lw = self.last_w.get(k)
            if lw is not None:
                deps.add(lw)
            for rd in self.readers.get(k, ()):
                deps.add(rd)
        if slot is not None:
            p = self.slot_last.get(slot)
            if p is not None:
                deps.add(p)
            self.slot_last[slot] = o
        deps.discard(o)
        for k in r:
            self.readers.setdefault(k, []).append(o)
        for k in w:
            self.last_w[k] = o
            self.readers[k] = []
        self.ops.append(o)
        return o

    def emit(self, nc):
        for o in self.ops:
            keep = set()
            for d in o.deps:
                if o.fn is not None and d.slot is None and d.eng == o.eng and (o.eng == "pe" or not SAME_ENGINE_SYNC):
                    continue
                keep.add(d)
            o.deps = keep
            for d in keep:
                d.needs_inc = True
        eng_cnt = {e: 0 for e in self.ENGS}
        slot_cnt = {}
        slots = []
        for o in self.ops:
            if o.slot is not None:
                if o.slot not in slot_cnt:
                    slot_cnt[o.slot] = 0
                    slots.append(o.slot)
                slot_cnt[o.slot] += 16
                o.val = slot_cnt[o.slot]
                o.needs_inc = True
            elif o.needs_inc:
                o.ep = eng_cnt[o.eng] // EPOCH
                o.val = eng_cnt[o.eng] % EPOCH + 1
                eng_cnt[o.eng] += 1
        with ExitStack() as es:
            esem = {(e, k): es.enter_context(nc.semaphore("e_%s%d" % (e, k)))
                    for e in self.ENGS if e != "sp" for k in range(eng_cnt[e] // EPOCH + 1)}
            ssem = {s: es.enter_context(nc.semaphore("s_%d" % i)) for i, s in enumerate(slots)}
            block = es.enter_context(nc.Block())
            engobj = {"pe": "tensor", "act": "scalar", "dve": "vector", "pool": "gpsimd", "sp": "sync"}

            def semof(o):
                return ssem[o.slot] if o.slot is not None else esem[(o.eng, o.ep)]

            def run(e):
                def body(eng):
                    waited = {}
                    for o in self.ops:
                        if o.eng != e:
                            continue
                        need = {}
                        for d in o.deps:
                            s = semof(d)
                            key = id(s)
                            if need.get(key, (0, None))[0] < d.val:
                                need[key] = (d.val, s)
                        for key, (val, s) in need.items():
                            if waited.get(key, 0) >= val:
                                continue
                            eng.wait_ge(s, val)
                            waited[key] = val
                        if o.fn is None:
                            continue
                        ins = o.fn(eng)
                        if o.needs_inc:
                            ins.then_inc(semof(o), 16 if o.slot is not None else 1)
                    if e == "sp":
                        for s in slots:
                            eng.wait_ge(ssem[s], slot_cnt[s])
                return body

            for e in self.ENGS:
                getattr(block, engobj[e])(run(e))
        return eng_cnt, slot_cnt


def build(T, L, debug=False, upto=99):
    TA = CTX + T
    NTT = TA // 128
    NCH = NTT
    cap_l = 2 * T // NE
    cap_c = 2 * CTX // NE
    assert cap_l % 128 == 0 or cap_l <= 128
    nc = bass.Bass("TRN2", target_bir_lowering=False)
    S = Sch()
    es = ExitStack()

    def din(name, shape, dt=F32):
        return nc.dram_tensor(name, list(shape), dt, kind="ExternalInput").ap()

    def dscr(name, shape, dt=F32):
        return nc.dram_tensor(name, list(shape), dt, kind="ExternalOutput" if debug else "Internal").ap()

    ARENA_N = 52224
    arena = es.enter_context(nc.sbuf_tensor("arena", [128, ARENA_N], F32))
    ar = {"off": 0}

    def sb(name, shape, dt=F32):
        shape = list(shape)
        nelem = 1
        for x in shape[1:]:
            nelem *= x
        size = 4 if dt in (F32, U32) else 2
        n32 = (nelem * size + 3) // 4
        n32 = (n32 + 7) // 8 * 8
        off = ar["off"]
        assert off + n32 <= ARENA_N, ("arena overflow", name, off, n32)
        ar["off"] = off + n32
        ap = arena[0:shape[0], off:off + n32]
        if dt != F32:
            ap = ap.bitcast(dt)
        ap = ap[:, 0:nelem]
        if len(shape) == 3:
            ap = ap.rearrange("p (a b) -> p a b", b=shape[2])
        elif len(shape) == 4:
            ap = ap.rearrange("p (a b c) -> p a b c", b=shape[2], c=shape[3])
        return ap

    def phase_begin():
        S.barrier()
        ar["off"] = ar["perm"]

    xall = din("xall", [TA, D])
    cT_d = din("cT", [128, KC, 2])
    ropec_d = din("ropec", [T, 1024])
    ropes_d = din("ropes", [T, 1024])
    consts_d = din("consts", [128, 5, 128])
    w_mod = din("w_mod", [L, D, 6 * D])
    b_mod = din("b_mod", [L, 6 * D])
    w_in = din("w_in", [L, D, C_END])
    w_out = din("w_out", [L, D, D])
    w_r = din("w_router", [L, D, NE])
    w_eg = din("w_expert_gate", [L, NE, D, FF])
    w_eu = din("w_expert_up", [L, NE, D, FF])
    w_ed = din("w_expert_down", [L, NE, FF, D])
    norm1_g = din("norm1_g", [L, D])
    norm2_g = din("norm2_g", [L, D])
    qng = din("q_norm_g", [L, 128])
    kng = din("k_norm_g", [L, 128])
    sink_d = din("attn_sink", [L, 8])
    convw_d = din("convw", [L, 128, 8, 3])
    convb_d = din("convb", [L, 128, 8])
    dtb_d = din("ssd_dt_bias", [L, 16])
    alog_d = din("ssd_a_log", [L, 16])
    dsk_d = din("ssd_d", [L, 8])
    sng_d = din("ssd_norm_g", [L, 512])
    scw_d = din("scw", [L, 128, 4, 3])
    out_d = nc.dram_tensor("out", [T, D], F32, kind="ExternalOutput").ap()

    xres = dscr("xres", [TA, D])
    modd = dscr("modd", [L, 2, 6 * D])
    gsd = dscr("gsd", [L, 2, 6, D])
    q_raw = dscr("q_raw", [TA, 1024])
    k_raw = dscr("k_raw", [TA, 256])
    v_tok = dscr("v_tok", [TA, 256], BF)
    z_tok = dscr("z_tok", [TA, 512])
    dt_raw = dscr("dt_raw", [TA, 16])
    xbcT = dscr("xbcT", [1024, TA])
    scT = dscr("scT", [1536, TA])
    qT = dscr("qT", [8, 128, TA], BF)
    kT = dscr("kT", [2, 128, TA], BF)
    mixT = dscr("mixT", [D, TA], BF)
    convT = dscr("convT", [1024, TA], BF)
    xbc_tok = dscr("xbc_tok", [TA, 768], BF)
    y_f = dscr("y_f", [TA, 512])
    h2_d = dscr("h2", [TA, D], BF)
    moe_d = dscr("moe_out", [TA, D])

    cst = sb("cst", [128, 5, 128])
    cstb = sb("cstb", [128, 5, 128], BF)
    onesb = sb("onesb", [128, 128], BF)
    ones32 = sb("ones32", [128, 128])
    zero32 = sb("zero32", [128, 2048])
    IDN, LE, GE, GT, LT = 0, 1, 2, 3, 4
    S.op("sp", lambda e: e.dma_start(out=cst[:], in_=consts_d), w=["cst"], slot="ld0")
    S.op("dve", lambda e: e.tensor_copy(out=cstb[:], in_=cst[:]), r=["cst"], w=["cstb"])
    S.op("dve", lambda e: e.memset(onesb[:], 1.0), w=["onesb"])
    S.op("dve", lambda e: e.memset(ones32[:], 1.0), w=["ones32"])
    S.op("pool", lambda e: e.memset(zero32[:], 0.0), w=["zero32"])

    psA = [es.enter_context(nc.psum_tensor("psA%d" % i, [128, 512], F32)) for i in range(6)]
    psT = [es.enter_context(nc.psum_tensor("psT%d" % i, [128, 1024], BF)) for i in range(2)]
    PA = ["psA%d" % i for i in range(6)]
    PT = ["psT0", "psT1"]

    cnt = {"i": 0}

    def rr(n):
        cnt["i"] += 1
        return cnt["i"] % n

    for tt in range(NTT):
        rows = slice(tt * 128, (tt + 1) * 128)
        S.op("sp", lambda e, rows=rows: e.dma_start(out=xres[rows, :], in_=xall[rows, :]),
             w=[("xres", tt)], slot="cp%d" % (tt % 4))

    def mm(out, lhsT, rhs, start, stop, r, w):
        import os
        if os.environ.get("NOMM"):
            return
        S.op("pe", lambda e: e.matmul(out, lhsT=lhsT, rhs=rhs, start=start, stop=stop), r=r, w=w)

    def tr(out, in_, ident, r, w):
        S.op("pe", lambda e: e.transpose(out=out, in_=in_, identity=ident), r=r, w=w)

    def act(out, in_, func, r, w, **kw):
        S.op("act", lambda e: e.activation(out=out, in_=in_, func=func, **kw), r=r, w=w)

    def tt(eng, out, in0, in1, op, r, w):
        S.op(eng, lambda e: e.tensor_tensor(out=out, in0=in0, in1=in1, op=op), r=r, w=w)

    def ts(eng, out, in0, s1, s2, op0, op1, r, w):
        if s2 is None:
            S.op(eng, lambda e: e.tensor_scalar(out=out, in0=in0, scalar1=s1, scalar2=None, op0=op0), r=r, w=w)
        else:
            S.op(eng, lambda e: e.tensor_scalar(out=out, in0=in0, scalar1=s1, scalar2=s2, op0=op0, op1=op1), r=r, w=w)

    def stt_(eng, out, in0, scalar, in1, op0, op1, r, w):
        S.op(eng, lambda e: e.scalar_tensor_tensor(out=out, in0=in0, scalar=scalar, in1=in1, op0=op0, op1=op1), r=r, w=w)

    def cp(eng, out, in_, r, w):
        if eng == "act":
            S.op(eng, lambda e: e.copy(out=out, in_=in_), r=r, w=w)
        else:
            S.op(eng, lambda e: e.tensor_copy(out=out, in_=in_), r=r, w=w)

    def dma(eng, out, in_, r, w, slot, **kw):
        import os
        if os.environ.get("NOST") and slot.startswith("ev"):
            return
        if eng == "st":
            eng = "act"
        S.op(eng, lambda e: e.dma_start(out=out, in_=in_, **kw), r=r, w=w, slot=slot)

    def bc_rows(ap, P=128):
        pat = [list(x) for x in ap.ap]
        while len(pat) > 1 and pat[0][1] == 1:
            pat = pat[1:]
        return bass.AP(ap.tensor, ap.offset, [[0, P]] + pat)

    def bc_last(ap, n):
        pat = [list(x) for x in ap.ap]
        return bass.AP(ap.tensor, ap.offset, pat + [[0, n]])

    def bc_mid(ap, n):
        pat = [list(x) for x in ap.ap]
        return bass.AP(ap.tensor, ap.offset, [pat[0], [0, n]] + pat[1:])

    wslot = {"i": 0}
    NWS = 8

    def load_w(dst, src, nk, key, slot):
        for k0 in range(0, nk, 4):
            m = min(4, nk - k0)
            wslot["i"] += 1
            dma("pool", dst[:, k0:k0 + m, :], src[k0 * 128:(k0 + m) * 128, :].rearrange("(k p) n -> p k n", p=128),
                r=[], w=[key], slot="W%d" % (wslot["i"] % NWS), max_dma_last_dim=8192)

    def evac_eng():
        return "act" if rr(2) else "dve"

    def K(name, rng):
        return [(name, i) for i in rng]

    def rstd_from_ss(ssap, outap, n, tmpap, keys):
        ts("dve", tmpap, ssap, 1.0 / n, EPS, ALU.mult, ALU.add, r=keys, w=keys)
        act(tmpap, tmpap, AF.Sqrt, r=keys, w=keys)
        S.op("dve", lambda e: e.reciprocal(out=outap, in_=tmpap), r=keys, w=keys)

    ar["perm"] = ar["off"]

    def phase0():
        phase_begin()
        cTs = sb("cTs", [128, KC, 2])
        cTb = sb("cTb", [128, KC, 2], BF)
        mblk = sb("mblk", [2, 2048])
        bblk = sb("bblk", [2, 2048])
        gblk = sb("gblk", [2, 2048])
        oblk = sb("oblk", [2, 2048])
        wm = [sb("wm%d" % i, [128, 2048], BF) for i in range(4)]
        dma("sp", cTs[:], cT_d, r=[], w=["cTs"], slot="ld0")
        act(cTb[:], cTs[:], AF.Silu, r=["cTs"], w=["cTb"])
        idxmap = {0: 1, 1: 0, 2: 2, 3: 4, 4: 3, 5: 5}
        for l in range(L):
            for cb in range(6):
                for kc in range(KC):
                    b = rr(4)
                    dma("pool", wm[b][:], w_mod[l, kc * 128:(kc + 1) * 128, cb * 2048:(cb + 1) * 2048],
                        r=[], w=[("wm", b)], slot="W%d" % (rr(NWS)), max_dma_last_dim=8192)
                    for j in range(4):
                        mm(psA[j][0:2, :], cTb[:, kc, :], wm[b][:, j * 512:(j + 1) * 512], kc == 0, kc == KC - 1,
                           r=["cTb", ("wm", b)], w=[PA[j]])
                for p in range(2):
                    dma("sp", bblk[p:p + 1, :], b_mod[l:l + 1, cb * 2048:(cb + 1) * 2048], r=[], w=["bblk"], slot="ld%d" % p)
                    if cb in (1, 4):
                        ng = norm1_g if cb == 1 else norm2_g
                        dma("sp", gblk[p:p + 1, :], ng[l:l + 1, :], r=[], w=["gblk"], slot="ld%d" % (2 + p))
                for j in range(4):
                    tt("dve", mblk[:, j * 512:(j + 1) * 512], psA[j][0:2, :], bblk[:, j * 512:(j + 1) * 512], ALU.add,
                       r=[PA[j], "bblk"], w=["mblk"])
                if cb in (1, 4):
                    stt_("dve", oblk[:], mblk[:], 1.0, gblk[:], ALU.add, ALU.mult, r=["mblk", "gblk"], w=["oblk"])
                else:
                    cp("dve", oblk[:], mblk[:], r=["mblk"], w=["oblk"])
                dma("sp", gsd[l, :, idxmap[cb], :], oblk[:], r=["oblk"], w=[("gsd", l)], slot="st0")

    blocks = [(0, CTX)]
    BLK = min(2048, T)
    for i in range(T // BLK):
        blocks.append((CTX + i * BLK, BLK))

    xt = h32 = junk = GS = stt = gate = None

    def load_GS(l, which, with_gate):
        for st in range(2):
            for j in range(2):
                dma("sp", GS[:, st, j, :], bc_rows(gsd[l, st, 3 * which + j:3 * which + j + 1, :]),
                    r=[("gsd", l)], w=["GS"], slot="ld%d" % (st * 2 + j))
            if with_gate:
                dma("sp", gate[:, st, :], bc_rows(gsd[l, st, 3 * which + 2:3 * which + 3, :]),
                    r=[("gsd", l)], w=["gate"], slot="ld%d" % (4 + st))

    def norm_tile(tt_i, xsrc_key, out_ap, out_key):
        b = tt_i % 2
        st = 1 if tt_i < CTX // 128 else 0
        sq_, h_, s_ = junk[b], h32[b], stt[b]
        S.op("pool", lambda e, a=s_: e.memset(a[:], 0.0), w=[("stt", b)])
        act(sq_[:], xt[b][:], AF.Square, r=[("xt", b)], w=[("junk", b), ("stt", b)], accum_out=s_[:, 0:1])
        rstd_from_ss(s_[:, 0:1], s_[:, 2:3], D, s_[:, 1:2], [("stt", b)])
        stt_("dve", h_[:], xt[b][:], s_[:, 2:3], GS[:, st, 0, :], ALU.mult, ALU.mult, r=[("xt", b), ("stt", b), "GS"], w=[("h32", b)])
        tt("pool", out_ap, h_[:], GS[:, st, 1, :], ALU.add, r=[("h32", b), "GS"], w=[out_key])

    def load_x(tt_i):
        b = tt_i % 2
        dma("sp", xt[b][:], xres[tt_i * 128:(tt_i + 1) * 128, :], r=[("xres", tt_i)], w=[("xt", b)], slot="xt%d" % b)

    def alloc_norm():
        g = {}
        g["xt"] = [sb("xt%d" % i, [128, 2048]) for i in range(2)]
        g["h32"] = [sb("h32_%d" % i, [128, 2048]) for i in range(2)]
        g["junk"] = [sb("junk%d" % i, [128, 2048], BF) for i in range(2)]
        g["GS"] = sb("GS", [128, 2, 2, 2048])
        g["stt"] = [sb("stt%d" % i, [128, 16]) for i in range(2)]
        return g

    def phase_AB(l):
        phase_begin()
        nonlocal xt, h32, junk, GS, stt, gate
        g = alloc_norm()
        xt, h32, junk, GS, stt = g["xt"], g["h32"], g["junk"], g["GS"], g["stt"]
        hT = sb("hT", [128, KC, 2048], BF)
        wbuf = [sb("wbuf%d" % i, [128, KC, 512], BF) for i in range(2)]
        hb = [sb("hb%d" % i, [128, 2048], BF) for i in range(2)]
        ev = [sb("ev%d" % i, [128, 512]) for i in range(2)]
        evb = [sb("evb%d" % i, [128, 512], BF) for i in range(2)]
        wdt = sb("wdt", [128, KC, 16], BF)
        load_GS(l, 0, False)
        for (t0, n) in blocks:
            nt = n // 128
            tt0 = t0 // 128
            load_x(tt0)
            for ti in range(nt):
                if ti + 1 < nt:
                    load_x(tt0 + ti + 1)
                hbi = (tt0 + ti) % 2
                norm_tile(tt0 + ti, None, hb[hbi][:], ("hb", hbi))
                for half in range(2):
                    for j in range(8):
                        kc = half * 8 + j
                        tr(psT[half][:, j * 128:(j + 1) * 128], hb[hbi][:, kc * 128:(kc + 1) * 128], cstb[:, IDN, :],
                           r=[("hb", hbi), "cstb"], w=[PT[half]])
                    cp("act" if half else "dve", hT[:, half * 8:(half + 1) * 8, ti * 128:(ti + 1) * 128],
                       psT[half][:].rearrange("p (k t) -> p k t", k=8), r=[PT[half]], w=[("hT", ti)])
            import os
            if os.environ.get("STOP") == "A":
                continue
            groups = [(C_Q, "q0"), (C_Q + 512, "q1"), (C_K, "kv"), (C_Z, "z")]
            if os.environ.get("GRP"):
                groups = [g for g in groups if g[1] in os.environ["GRP"].split(",")]
            for (c0, gname) in groups:
                b = rr(2)
                load_w(wbuf[b], w_in[l, :, c0:c0 + 512], KC, ("wbuf", b), "wb%d" % b)
                for ti in range(nt):
                    pb = rr(6)
                    rows = slice(t0 + ti * 128, t0 + (ti + 1) * 128)
                    for kc in range(KC):
                        mm(psA[pb][:, :], hT[:, kc, ti * 128:(ti + 1) * 128], wbuf[b][:, kc, :], kc == 0, kc == KC - 1,
                           r=[("hT", ti), ("wbuf", b)], w=[PA[pb]])
                    eb = rr(2)
                    eng = evac_eng()
                    tkey = tt0 + ti
                    if gname == "kv":
                        cp(eng, ev[eb][:], psA[pb][:], r=[PA[pb]], w=[("ev", eb)])
                        cp("pool", evb[eb][:, 0:256], ev[eb][:, 256:512], r=[("ev", eb)], w=[("evb", eb)])
                        dma("st", k_raw[rows, :], ev[eb][:, 0:256], r=[("ev", eb)], w=[("k_raw", tkey)], slot="ev%d" % eb)
                        dma("st", v_tok[rows, :], evb[eb][:, 0:256], r=[("evb", eb)], w=[("v_tok", tkey)], slot="evb%d" % eb)
                    else:
                        cp(eng, ev[eb][:], psA[pb][:], r=[PA[pb]], w=[("ev", eb)])
                        if gname == "q0":
                            dma("st", q_raw[rows, 0:512], ev[eb][:], r=[("ev", eb)], w=[("q_raw0", tkey)], slot="ev%d" % eb)
                        elif gname == "q1":
                            dma("st", q_raw[rows, 512:1024], ev[eb][:], r=[("ev", eb)], w=[("q_raw1", tkey)], slot="ev%d" % eb)
                        else:
                            dma("st", z_tok[rows, :], ev[eb][:], r=[("ev", eb)], w=[("z_tok", tkey)], slot="ev%d" % eb)
            if os.environ.get("STOP") == "B1":
                continue
            load_w(wdt, w_in[l, :, C_DT:C_DT + 16], KC, "wdt", "wdt")
            for ti in range(nt):
                pb = rr(6)
                rows = slice(t0 + ti * 128, t0 + (ti + 1) * 128)
                for kc in range(KC):
                    mm(psA[pb][:, 0:16], hT[:, kc, ti * 128:(ti + 1) * 128], wdt[:, kc, :], kc == 0, kc == KC - 1,
                       r=[("hT", ti), "wdt"], w=[PA[pb]])
                eb = rr(2)
                cp(evac_eng(), ev[eb][:, 0:16], psA[pb][:, 0:16], r=[PA[pb]], w=[("ev", eb)])
                dma("st", dt_raw[rows, :], ev[eb][:, 0:16], r=[("ev", eb)], w=[("dt_raw", tt0 + ti)], slot="ev%d" % eb)
            fgroups = [(C_XBC + i * 512, xbcT, i * 512, "xbcT") for i in range(2)] + \
                      [(C_SB + i * 512, scT, i * 512, "scT") for i in range(3)]
            cw = min(512, n)
            for (c0, dst, row0, dname) in fgroups:
                b = rr(2)
                load_w(wbuf[b], w_in[l, :, c0:c0 + 512], KC, ("wbuf", b), "wb%d" % b)
                for j in range(4):
                    for ci in range(n // cw):
                        pb = rr(6)
                        for kc in range(KC):
                            mm(psA[pb][:, 0:cw], wbuf[b][:, kc, j * 128:(j + 1) * 128], hT[:, kc, ci * cw:(ci + 1) * cw],
                               kc == 0, kc == KC - 1,
                               r=K("hT", range(ci * cw // 128, (ci + 1) * cw // 128)) + [("wbuf", b)], w=[PA[pb]])
                        eb = rr(2)
                        cp(evac_eng(), ev[eb][:, 0:cw], psA[pb][:, 0:cw], r=[PA[pb]], w=[("ev", eb)])
                        ch0 = row0 + j * 128
                        dma("st", dst[ch0:ch0 + 128, t0 + ci * cw:t0 + (ci + 1) * cw], ev[eb][:, 0:cw], r=[("ev", eb)],
                            w=[(dname, ch0 // 128, (t0 + ci * cw) // 128 + q) for q in range(cw // 128)], slot="ev%d" % eb)

    def conv_pieces():
        P = [(0, CTX, 0, CTX)]
        step = min(T, 2048)
        for a in range(0, T, step):
            P.append((CTX + a, step, CTX, CTX + T))
        return P

    def load_halo(dst, src_rows, t0, n, lo, hi, key, slot, rkeys):
        a = max(t0 - 1, lo)
        b = min(t0 + n + 1, hi)
        if a > t0 - 1:
            S.op("pool", lambda e: e.memset(dst[:, 0:1], 0.0), w=[key])
        if b < t0 + n + 1:
            S.op("pool", lambda e: e.memset(dst[:, n + 1:n + 2], 0.0), w=[key])
        dma("sp", dst[:, a - (t0 - 1):b - (t0 - 1)], src_rows[:, a:b], r=rkeys, w=[key], slot=slot)

    def phase_E(l):
        phase_begin()
        NP = min(T, 2048) + 2
        cin = sb("cin", [128, NP])
        hin = sb("hin", [128, NP])
        bin_ = sb("bin", [128, NP])
        acc = sb("acc", [128, NP])
        ob = sb("ob", [128, NP], BF)
        scw = sb("scw_s", [128, 4, 3])
        dma("sp", scw[:], scw_d[l], r=[], w=["scw"], slot="ld0")
        for j in range(4):
            for (t0, n, lo, hi) in conv_pieces():
                tts = range(t0 // 128, (t0 + n) // 128)
                tth = range(max(t0 - 128, lo) // 128, min(t0 + n + 128, hi) // 128)
                load_halo(cin, scT[512 + j * 128:512 + (j + 1) * 128, :], t0, n, lo, hi, "cin", "ld1",
                          [("scT", 4 + j, q) for q in tth])
                load_halo(hin, scT[1024 + j * 128:1024 + (j + 1) * 128, :], t0, n, lo, hi, "hin", "ld2",
                          [("scT", 8 + j, q) for q in tth])
                dma("sp", bin_[:, 0:n], scT[j * 128:(j + 1) * 128, t0:t0 + n], r=[("scT", j, q) for q in tts], w=["bin"], slot="ld3")
                tt("dve", cin[:, 0:n + 2], cin[:, 0:n + 2], hin[:, 0:n + 2], ALU.mult, r=["cin", "hin"], w=["cin"])
                ts("pool", acc[:, 0:n], cin[:, 1:n + 1], scw[:, j, 1:2], None, ALU.mult, None, r=["cin", "scw"], w=["acc"])
                stt_("dve", acc[:, 0:n], cin[:, 0:n], scw[:, j, 0:1], acc[:, 0:n], ALU.mult, ALU.add, r=["cin", "scw", "acc"], w=["acc"])
                stt_("dve", acc[:, 0:n], cin[:, 2:n + 2], scw[:, j, 2:3], acc[:, 0:n], ALU.mult, ALU.add, r=["cin", "scw", "acc"], w=["acc"])
                tt("pool", ob[:, 0:n], acc[:, 0:n], bin_[:, 0:n], ALU.mult, r=["acc", "bin"], w=["ob"])
                dma("st", mixT[1536 + j * 128:1536 + (j + 1) * 128, t0:t0 + n], ob[:, 0:n], r=["ob"],
                    w=[("mixT", 12 + j, q) for q in tts], slot="ev0")

    def phase_F(l):
        phase_begin()
        nonlocal gate
        hT = sb("hT", [128, KC, 2048], BF)
        wbuf = [sb("wbuf%d" % i, [128, KC, 512], BF) for i in range(2)]
        gate = sb("gate", [128, 2, 2048])
        xo = [sb("xo%d" % i, [128, 512]) for i in range(2)]
        t1 = [sb("t1%d" % i, [128, 512]) for i in range(2)]
        for st in range(2):
            dma("sp", gate[:, st, :], bc_rows(gsd[l, st, 2:3, :]), r=[("gsd", l)], w=["gate"], slot="ld%d" % (4 + st))
        for (t0, n) in blocks:
            nt = n // 128
            tt0 = t0 // 128
            st = 1 if t0 < CTX else 0
            for kc in range(KC):
                dma("sp", hT[:, kc, 0:n], mixT[kc * 128:(kc + 1) * 128, t0:t0 + n],
                    r=[("mixT", kc, q) for q in range(tt0, tt0 + nt)], w=[("hT", q) for q in range(nt)], slot="ld%d" % (kc % 4))
            for cg in range(4):
                b = rr(2)
                load_w(wbuf[b], w_out[l, :, cg * 512:(cg + 1) * 512], KC, ("wbuf", b), "wb%d" % b)
                for ti in range(nt):
                    pb = rr(6)
                    eb = rr(2)
                    rows = slice(t0 + ti * 128, t0 + (ti + 1) * 128)
                    dma("sp", xo[eb][:], xres[rows, cg * 512:(cg + 1) * 512], r=[("xres", tt0 + ti)], w=[("xo", eb)], slot="xo%d" % eb)
                    for kc in range(KC):
                        mm(psA[pb][:, :], hT[:, kc, ti * 128:(ti + 1) * 128], wbuf[b][:, kc, :], kc == 0, kc == KC - 1,
                           r=[("hT", ti), ("wbuf", b)], w=[PA[pb]])
                    tt("dve", t1[eb][:], psA[pb][:], gate[:, st, cg * 512:(cg + 1) * 512], ALU.mult, r=[PA[pb], "gate"], w=[("t1", eb)])
                    tt("pool", t1[eb][:], t1[eb][:], xo[eb][:], ALU.add, r=[("t1", eb), ("xo", eb)], w=[("t1", eb)])
                    dma("st", xres[rows, cg * 512:(cg + 1) * 512], t1[eb][:], r=[("t1", eb)], w=[("xres", tt0 + ti)], slot="ev%d" % eb)

    def phase_C(l):
        phase_begin()
        NL = T // 128
        gq = sb("gq", [128, 128])
        gk = sb("gk", [128, 128])
        esink = sb("esink", [128, 8])
        dma("sp", gq[:], bc_rows(qng[l:l + 1, :]), r=[], w=["gq"], slot="ld0")
        dma("sp", gk[:], bc_rows(kng[l:l + 1, :]), r=[], w=["gk"], slot="ld1")
        dma("sp", esink[:], bc_rows(sink_d[l:l + 1, :]), r=[], w=["esink"], slot="ld2")
        act(esink[:], esink[:], AF.Exp, r=["esink"], w=["esink"])
        mark = ar["off"]
        src = [sb("src%d" % i, [128, 1024]) for i in range(2)]
        sq2 = [sb("sq%d" % i, [128, 1024]) for i in range(2)]
        xn2 = [sb("xn%d" % i, [128, 1024]) for i in range(2)]
        t12 = [sb("t1_%d" % i, [128, 1024]) for i in range(2)]
        t22 = [sb("t2_%d" % i, [128, 1024]) for i in range(2)]
        cosb = [sb("cos%d" % i, [128, 1024]) for i in range(2)]
        sinb = [sb("sin%d" % i, [128, 1024]) for i in range(2)]
        ob2 = [sb("ob%d" % i, [128, 1024], BF) for i in range(2)]
        stg = [sb("stg%d" % i, [128, 8, 128], BF) for i in range(2)]
        st82 = [sb("st8_%d" % i, [128, 32]) for i in range(2)]
        pc = {"i": 0}

        def prep(tt_i, H, srcd, colkey, gt, dstT, dkey):
            pc["i"] += 1
            pb_ = pc["i"] % 2
            sq, xn, t1, t2, ob, st8 = sq2[pb_], xn2[pb_], t12[pb_], t22[pb_], ob2[pb_], st82[pb_]
            kq = lambda n_: (n_, pb_)
            b = pc["i"] % 2
            lat = tt_i >= CTX // 128
            rows = slice(tt_i * 128, (tt_i + 1) * 128)
            W = H * 128
            dma("sp", src[b][:, 0:W], srcd[rows, :], r=[(k_, tt_i) for k_ in colkey], w=[("src", b)], slot="ld%d" % (3 + b))
            if lat:
                lr = slice((tt_i - 2) * 128, (tt_i - 1) * 128)
                dma("sp", cosb[b][:, 0:W], ropec_d[lr, 0:W], r=[], w=[("cos", b)], slot="ld%d" % (5 + b))
                dma("sp", sinb[b][:, 0:W], ropes_d[lr, 0:W], r=[], w=[("sin", b)], slot="ld%d" % (7 + b))
            tt("pool", sq[:, 0:W], src[b][:, 0:W], src[b][:, 0:W], ALU.mult, r=[("src", b)], w=[kq("sq")])
            S.op("dve", lambda e: e.reduce_sum(out=st8[:, 0:H], in_=sq[:, 0:W].rearrange("p (h d) -> p h d", d=128), axis=AX.X),
                 r=[kq("sq")], w=[kq("st8")])
            rstd_from_ss(st8[:, 0:H], st8[:, 16:16 + H], 128, st8[:, 8:8 + H], [kq("st8")])
            v3 = lambda a: a[:, 0:W].rearrange("p (h d) -> p h d", d=128)
            tt("dve", v3(xn), v3(src[b]), bc_last(st8[:, 16:16 + H], 128), ALU.mult, r=[("src", b), kq("st8")], w=[kq("xn")])
            tt("pool", v3(xn), v3(xn), bc_mid(gt[:], H), ALU.mult, r=[kq("xn"), "gq", "gk"], w=[kq("xn")])
            if lat:
                tt("dve", t1[:, 0:W], xn[:, 0:W], cosb[b][:, 0:W], ALU.mult, r=[kq("xn"), ("cos", b)], w=[kq("t1")])
                v4 = lambda a: a[:, 0:W].rearrange("p (a two c) -> p a two c", two=2, c=32)
                tt("pool", v4(t2)[:, :, 0, :], v4(xn)[:, :, 1, :], v4(sinb[b])[:, :, 0, :], ALU.mult, r=[kq("xn"), ("sin", b)], w=[kq("t2")])
                tt("pool", v4(t2)[:, :, 1, :], v4(xn)[:, :, 0, :], v4(sinb[b])[:, :, 1, :], ALU.mult, r=[kq("xn"), ("sin", b)], w=[kq("t2")])
                tt("dve", ob[:, 0:W], t1[:, 0:W], t2[:, 0:W], ALU.add, r=[kq("t1"), kq("t2")], w=[kq("ob")])
            else:
                cp("dve", ob[:, 0:W], xn[:, 0:W], r=[kq("xn")], w=[kq("ob")])
            pt = rr(2)
            for h in range(H):
                tr(psT[pt][:, h * 128:(h + 1) * 128], ob[:, h * 128:(h + 1) * 128], cstb[:, IDN, :], r=[kq("ob"), "cstb"], w=[PT[pt]])
            sg = rr(2)
            cp("act", stg[sg][:, 0:H, :], psT[pt][:, 0:W].rearrange("p (h t) -> p h t", t=128), r=[PT[pt]], w=[("stg", sg)])
            dma("st", dstT[:, :, rows].rearrange("h d t -> d h t"), stg[sg][:, 0:H, :], r=[("stg", sg)], w=[(dkey, tt_i)], slot="ev%d" % sg)

        for tt_i in range(NTT):
            prep(tt_i, 2, k_raw, ["k_raw"], gk, kT, "kT")
            prep(tt_i, 8, q_raw, ["q_raw0", "q_raw1"], gq, qT, "qT")

        S.barrier()
        ar["off"] = mark
        kTg = sb("kTg", [128, TA], BF)
        vg = sb("vg", [128, NTT, 128], BF)
        q4 = [sb("q4%d" % i, [128, 4, 128], BF) for i in range(2)]
        pTb = [sb("pT%d" % i, [128, 512], BF) for i in range(3)]
        den = sb("den", [128, 512])
        osb = [sb("osb%d" % i, [128, 4, 128], BF) for i in range(2)]
        sc = 1.0 / math.sqrt(128.0)
        for g in range(2):
            dma("sp", kTg[:], kT[g], r=K("kT", range(NTT)), w=["kTg"], slot="ld0")
            dma("sp", vg[:], v_tok[:, g * 128:(g + 1) * 128].rearrange("(c p) d -> p c d", p=128),
                r=K("v_tok", range(NTT)), w=["vg"], slot="ld1")
            for qt in range(NTT):
                b = qt % 2
                rows = slice(qt * 128, (qt + 1) * 128)
                dma("sp", q4[b][:], qT[g * 4:(g + 1) * 4, :, rows].rearrange("h d t -> d h t"), r=[("qT", qt)], w=[("q4", b)], slot="ld%d" % (2 + b))
                chunks = [(0, None), (1, None)]
                if qt >= 2:
                    i = qt - 2
                    if i > 0:
                        chunks.append((qt - 1, GE))
                    chunks.append((qt, None))
                    if i < NL - 1:
                        chunks.append((qt + 1, LE))
                ops, dps = psA[2 + b], psA[4 + b]
                opk, dpk = PA[2 + b], PA[4 + b]
                q4f = q4[b][:].rearrange("p h t -> p (h t)")
                nch = len(chunks)
                sbank = {}

                def score(ci):
                    kt = chunks[ci][0]
                    sb_i = ci % 2
                    sbank[ci] = sb_i
                    mm(psA[sb_i][:, :], kTg[:, kt * 128:(kt + 1) * 128], q4f, True, True, r=["kTg", ("q4", b)], w=[PA[sb_i]])

                score(0)
                for ci, (kt, mk) in enumerate(chunks):
                    if ci + 1 < nch:
                        score(ci + 1)
                    sb_i = sbank[ci]
                    pi = ci % 3
                    act(pTb[pi][:], psA[sb_i][:, :], AF.Exp, r=[PA[sb_i]], w=[("pT", pi)], scale=sc)
                    if mk is not None:
                        pv = pTb[pi][:].rearrange("p (h t) -> p h t", t=128)
                        tt("pool", pv, pv, bc_mid(cstb[:, mk, :], 4), ALU.mult, r=[("pT", pi), "cstb"], w=[("pT", pi)])
                    mm(ops[:, :], vg[:, kt, :], pTb[pi][:], ci == 0, ci == nch - 1, r=["vg", ("pT", pi)], w=[opk])
                    mm(dps[:, :], onesb[:], pTb[pi][:], ci == 0, ci == nch - 1, r=["onesb", ("pT", pi)], w=[dpk])
                d3 = den[:].rearrange("p (h t) -> p h t", t=128)
                tt("dve", d3, dps[:, :].rearrange("p (h t) -> p h t", t=128), bc_last(esink[:, g * 4:(g + 1) * 4], 128), ALU.add,
                   r=[dpk, "esink"], w=["den"])
                S.op("dve", lambda e: e.reciprocal(out=den[:], in_=den[:]), r=["den"], w=["den"])
                tt("dve", osb[b][:], ops[:, :].rearrange("p (h t) -> p h t", t=128), d3, ALU.mult, r=[opk, "den"], w=[("osb", b)])
                dma("st", mixT[g * 512:(g + 1) * 512, rows].rearrange("(h d) t -> d h t", d=128), osb[b][:], r=[("osb", b)],
                    w=[("mixT", g * 4 + h, qt) for h in range(4)], slot="ev%d" % b)

    def phase_D(l):
        phase_begin()
        NP = min(T, 2048) + 2
        xin = sb("xin", [128, NP])
        acc = sb("acc", [128, NP])
        cvo = sb("cvo", [128, NP], BF)
        cw = sb("cw", [128, 8, 3])
        cbias = sb("cbias", [128, 8])
        stg = [sb("stgd%d" % i, [128, 8, 128], BF) for i in range(2)]
        dma("sp", cw[:], convw_d[l], r=[], w=["cw"], slot="ld0")
        dma("sp", cbias[:], convb_d[l], r=[], w=["cbias"], slot="ld1")
        for j in range(8):
            for (t0, n, lo, hi) in conv_pieces():
                tts = range(t0 // 128, (t0 + n) // 128)
                tth = range(max(t0 - 128, lo) // 128, min(t0 + n + 128, hi) // 128)
                load_halo(xin, xbcT[j * 128:(j + 1) * 128, :], t0, n, lo, hi, "xin", "ld2", [("xbcT", j, q) for q in tth])
                ts("dve", acc[:, 0:n], xin[:, 1:n + 1], cw[:, j, 1:2], cbias[:, j:j + 1], ALU.mult, ALU.add, r=["xin", "cw", "cbias"], w=["acc"])
                stt_("dve", acc[:, 0:n], xin[:, 0:n], cw[:, j, 0:1], acc[:, 0:n], ALU.mult, ALU.add, r=["xin", "cw", "acc"], w=["acc"])
                stt_("dve", acc[:, 0:n], xin[:, 2:n + 2], cw[:, j, 2:3], acc[:, 0:n], ALU.mult, ALU.add, r=["xin", "cw", "acc"], w=["acc"])
                act(cvo[:, 0:n], acc[:, 0:n], AF.Silu, r=["acc"], w=["cvo"])
                dma("st", convT[j * 128:(j + 1) * 128, t0:t0 + n], cvo[:, 0:n], r=["cvo"], w=[("convT", j, q) for q in tts], slot="ev0")
                if j < 6:
                    for g8 in range(0, n // 128, 8):
                        m = min(8, n // 128 - g8)
                        pt = rr(2)
                        sg = rr(2)
                        for q in range(m):
                            tr(psT[pt][:, q * 128:(q + 1) * 128], cvo[:, (g8 + q) * 128:(g8 + q + 1) * 128], cstb[:, IDN, :],
                               r=["cvo", "cstb"], w=[PT[pt]])
                        cp("act", stg[sg][:, 0:m, :], psT[pt][:, 0:m * 128].rearrange("p (c d) -> p c d", d=128), r=[PT[pt]], w=[("stgd", sg)])
                        r0 = t0 + g8 * 128
                        dma("st", xbc_tok[r0:r0 + m * 128, j * 128:(j + 1) * 128].rearrange("(c p) d -> p c d", p=128), stg[sg][:, 0:m, :],
                            r=[("stgd", sg)], w=[("xbc_tok", j, r0 // 128 + q) for q in range(m)], slot="ev%d" % (1 + sg))
        phase_begin()
        dtb = sb("dtb", [128, 16])
        A16 = sb("A16", [128, 16])
        Dsk = sb("Dsk", [128, 8])
        gn = sb("gn", [128, 512])
        dma("sp", dtb[:], bc_rows(dtb_d[l:l + 1, :]), r=[], w=["dtb"], slot="ld0")
        dma("sp", A16[:], bc_rows(alog_d[l:l + 1, :]), r=[], w=["A16"], slot="ld1")
        dma("sp", Dsk[:], bc_rows(dsk_d[l:l + 1, :]), r=[], w=["Dsk"], slot="ld2")
        dma("sp", gn[:], bc_rows(sng_d[l:l + 1, :]), r=[], w=["gn"], slot="ld3")
        act(A16[:], A16[:], AF.Exp, r=["A16"], w=["A16"])
        ts("dve", A16[:], A16[:], -1.0, None, ALU.mult, None, r=["A16"], w=["A16"])
        NLB = 3
        dtr = [sb("dtr%d" % i, [128, 16]) for i in range(NLB)]
        BT = [sb("BT%d" % i, [128, 2, 128], BF) for i in range(NLB)]
        CT = [sb("CT%d" % i, [128, 2, 128], BF) for i in range(NLB)]
        xsb = [sb("xsb%d" % i, [128, 512], BF) for i in range(NLB)]
        Btk = [sb("Btk%d" % i, [128, 256], BF) for i in range(NLB)]
        sm = sb("sm", [128, 64])
        sm2 = sb("sm2", [128, 16])
        ex = [sb("ex%d" % i, [128, 24]) for i in range(2)]
        lt = sb("lt", [128, 8, 128])
        dec = sb("dec", [128, 8, 128])
        cbm = sb("cbm", [128, 2, 128])
        MT = [sb("MT%d" % i, [128, 8, 128], BF) for i in range(2)]
        xdt = [sb("xdt%d" % i, [128, 512], BF) for i in range(2)]
        xw = [sb("xw%d" % i, [128, 512], BF) for i in range(2)]
        t1 = sb("t1d", [128, 512])
        ysb = [sb("ysb%d" % i, [128, 512]) for i in range(2)]
        hst = sb("hst", [128, 512])
        hTb = sb("hTb", [128, 512], BF)
        yfl = sb("yfl", [128, 512])
        zl = sb("zl", [128, 512])
        u = sb("u", [128, 512])
        ub = sb("ub", [128, 512], BF)
        stg2 = [sb("stg2%d" % i, [128, 4, 128], BF) for i in range(2)]
        v8 = lambda a: a.rearrange("p (h d) -> p h d", d=64)
        p0, pS0, pS1, pY, pO, pS2 = psA
        k0, kS0, kS1, kY, kO, kS2 = PA

        def loads(c, b):
            rows = slice(c * 128, (c + 1) * 128)
            dma("sp", dtr[b][:], dt_raw[rows, :], r=[("dt_raw", c)], w=[("dtr", b)], slot="ld%d" % (4 + b))
            dma("sp", BT[b][:], convT[512:768, rows].rearrange("(g n) t -> n g t", n=128), r=[("convT", 4, c), ("convT", 5, c)], w=[("BT", b)], slot="ld%d" % (7 + b))
            dma("sp", CT[b][:], convT[768:1024, rows].rearrange("(g n) t -> n g t", n=128), r=[("convT", 6, c), ("convT", 7, c)], w=[("CT", b)], slot="ld%d" % (10 + b))
            dma("sp", xsb[b][:], xbc_tok[rows, 0:512], r=[("xbc_tok", j, c) for j in range(4)], w=[("xsb", b)], slot="ld%d" % (13 + b))
            dma("sp", Btk[b][:], xbc_tok[rows, 512:768], r=[("xbc_tok", j, c) for j in (4, 5)], w=[("Btk", b)], slot="ld%d" % (16 + b))

        for d in range(2):
            Ud, Ld, Md = (LE, GT, LE) if d == 0 else (GE, LT, GE)
            order = list(range(NTT)) if d == 0 else [1, 0] + list(range(NTT - 1, 1, -1))
            S.op("pool", lambda e: e.memset(hst[:], 0.0), w=["hst"])
            S.op("pool", lambda e: e.memset(hTb[:], 0.0), w=["hTb"])

            def stageA(oi):
                b = oi % NLB
                ab = oi % 2
                dt8, dta8, dtw = sm[:, 0:8], sm[:, 8:16], sm[:, 16:24]
                tt("dve", sm[:, 24:32], dtr[b][:, d * 8:(d + 1) * 8], dtb[:, d * 8:(d + 1) * 8], ALU.add, r=[("dtr", b), "dtb"], w=["sm"])
                act(sm[:, 24:32], sm[:, 24:32], AF.Exp, r=["sm"], w=["sm"])
                ts("dve", sm[:, 24:32], sm[:, 24:32], 1.0, None, ALU.add, None, r=["sm"], w=["sm"])
                act(dt8, sm[:, 24:32], AF.Ln, r=["sm"], w=["sm"])
                tt("dve", dta8, dt8, A16[:, d * 8:(d + 1) * 8], ALU.mult, r=["sm", "A16"], w=["sm"])
                mm(p0[:, 0:8], cst[:, Ud, :], dta8, True, True, r=["cst", "sm"], w=[k0])
                mm(p0[:, 8:16], cst[:, Ld, :], dta8, True, True, r=["cst", "sm"], w=[k0])
                mm(p0[:, 16:24], ones32[:], dta8, True, True, r=["ones32", "sm"], w=[k0])
                for g in range(2):
                    mm(p0[:, 128 + g * 128:256 + g * 128], BT[b][:, g, :], CT[b][:, g, :], True, True, r=[("BT", b), ("CT", b)], w=[k0])
                act(ex[ab][:], p0[:, 0:24], AF.Exp, r=[k0], w=[("ex", ab)])
                tt("dve", cbm[:], p0[:, 128:384].rearrange("p (g t) -> p g t", t=128), bc_mid(cst[:, Md, :], 2), ALU.mult,
                   r=[k0, "cst", ("ex", ab)], w=["cbm"])
                tt("pool", lt[:], bc_mid(cst[:, Ld, :], 8), bc_last(dta8, 128), ALU.mult, r=["cst", "sm"], w=["lt"])
                for h in range(8):
                    ps_, pk_ = (pS0, kS0) if h < 4 else (pS1, kS1)
                    mm(ps_[:, (h % 4) * 128:(h % 4 + 1) * 128], lt[:, h, :], cst[:, Ud, :], True, True, r=["lt", "cst"], w=[pk_])
                act(dec[:, 0:4, :], pS0[:, :].rearrange("p (h t) -> p h t", t=128), AF.Exp, r=[kS0], w=["dec0"])
                act(dec[:, 4:8, :], pS1[:, :].rearrange("p (h t) -> p h t", t=128), AF.Exp, r=[kS1], w=["dec1"])
                tt("dve", MT[ab][:, 0:4, :], dec[:, 0:4, :], bc_mid(cbm[:, 0, :], 4), ALU.mult, r=["dec0", "cbm"], w=[("MT0", ab)])
                tt("pool", MT[ab][:, 4:8, :], dec[:, 4:8, :], bc_mid(cbm[:, 1, :], 4), ALU.mult, r=["dec1", "cbm"], w=[("MT1", ab)])
                tt("dve", v8(xdt[ab][:]), v8(xsb[b][:]), bc_last(dt8, 64), ALU.mult, r=[("xsb", b), "sm"], w=[("xdt", ab)])
                tt("dve", dtw, dt8, ex[ab][:, 8:16], ALU.mult, r=["sm", ("ex", ab)], w=["sm"])
                tt("pool", v8(xw[ab][:]), v8(xsb[b][:]), bc_last(dtw, 64), ALU.mult, r=[("xsb", b), "sm"], w=[("xw", ab)])

            def stageB(oi, c):
                b = oi % NLB
                ab = oi % 2
                rows = slice(c * 128, (c + 1) * 128)
                for h in range(8):
                    mm(pY[:, h * 64:(h + 1) * 64], MT[ab][:, h, :], xdt[ab][:, h * 64:(h + 1) * 64], True, True,
                       r=[("MT0", ab) if h < 4 else ("MT1", ab), ("xdt", ab)], w=[kY])
                for g in range(2):
                    mm(pO[:, g * 256:(g + 1) * 256], CT[b][:, g, :], hTb[:, g * 256:(g + 1) * 256], True, True, r=[("CT", b), "hTb"], w=[kO])
                for g in range(2):
                    mm(pS2[:, g * 256:(g + 1) * 256], Btk[b][:, g * 128:(g + 1) * 128], xw[ab][:, g * 256:(g + 1) * 256], True, True,
                       r=[("Btk", b), ("xw", ab)], w=[kS2])
                tt("dve", v8(t1[:]), v8(pO[:, :]), bc_last(ex[ab][:, 0:8], 64), ALU.mult, r=[kO, ("ex", ab)], w=["t1d"])
                yb = ysb[oi % 2]
                tt("dve", yb[:], pY[:, :], t1[:], ALU.add, r=[kY, "t1d"], w=[("ysb", oi % 2)])
                tt("pool", v8(hst[:]), v8(hst[:]), bc_last(ex[ab][:, 16:24], 64), ALU.mult, r=["hst", ("ex", ab)], w=["hst"])
                tt("dve", hst[:], hst[:], pS2[:, :], ALU.add, r=["hst", kS2], w=["hst"])
                cp("act", hTb[:], hst[:], r=["hst"], w=["hTb"])
                if d == 0:
                    dma("st", y_f[rows, :], yb[:], r=[("ysb", oi % 2)], w=[("y_f", c)], slot="ev%d" % (oi % 2))
                else:
                    dma("sp", yfl[:], y_f[rows, :], r=[("y_f", c)], w=["yfl"], slot="ld19")
                    dma("sp", zl[:], z_tok[rows, :], r=[("z_tok", c)], w=["zl"], slot="ld20")
                    tt("pool", yb[:], yb[:], yfl[:], ALU.add, r=[("ysb", oi % 2), "yfl"], w=[("ysb", oi % 2)])
                    tt("dve", v8(u[:]), v8(xsb[b][:]), bc_last(Dsk[:], 64), ALU.mult, r=[("xsb", b), "Dsk"], w=["u"])
                    tt("dve", yb[:], yb[:], u[:], ALU.add, r=[("ysb", oi % 2), "u"], w=[("ysb", oi % 2)])
                    act(zl[:], zl[:], AF.Silu, r=["zl"], w=["zl"])
                    tt("dve", u[:], yb[:], zl[:], ALU.mult, r=[("ysb", oi % 2), "zl"], w=["u"])
                    tt("pool", t1[:], u[:], u[:], ALU.mult, r=["u"], w=["t1d"])
                    S.op("dve", lambda e: e.reduce_sum(out=sm2[:, 0:2], in_=t1[:].rearrange("p (g c) -> p g c", c=256), axis=AX.X),
                         r=["t1d"], w=["sm2"])
                    rstd_from_ss(sm2[:, 0:2], sm2[:, 4:6], 256, sm2[:, 2:4], ["sm2"])
                    tt("dve", u[:].rearrange("p (g c) -> p g c", c=256), u[:].rearrange("p (g c) -> p g c", c=256),
                       bc_last(sm2[:, 4:6], 256), ALU.mult, r=["u", "sm2"], w=["u"])
                    tt("pool", ub[:], u[:], gn[:], ALU.mult, r=["u", "gn"], w=["ub"])
                    pt = rr(2)
                    sg = rr(2)
                    for j in range(4):
                        tr(psT[pt][:, j * 128:(j + 1) * 128], ub[:, j * 128:(j + 1) * 128], cstb[:, IDN, :], r=["ub", "cstb"], w=[PT[pt]])
                    cp("act", stg2[sg][:], psT[pt][:, 0:512].rearrange("p (j t) -> p j t", t=128), r=[PT[pt]], w=[("stg2", sg)])
                    dma("st", mixT[1024:1536, rows].rearrange("(j c) t -> c j t", c=128), stg2[sg][:], r=[("stg2", sg)],
                        w=[("mixT", 8 + j, c) for j in range(4)], slot="ev%d" % (2 + sg))

            n_o = len(order)
            loads(order[0], 0)
            if n_o > 1:
                loads(order[1], 1)
            stageA(0)
            for oi, c in enumerate(order):
                if oi + 2 < n_o:
                    loads(order[oi + 2], (oi + 2) % NLB)
                if oi + 1 < n_o:
                    stageA(oi + 1)
                stageB(oi, c)

    sets = [(0, CTX, cap_c)] + [(CTX, T, cap_l)]
    slot_tiles = []
    col = 0
    for si, (_, _, cap) in enumerate(sets):
        for s0 in range(0, cap, 128):
            ns = min(128, cap - s0)
            slot_tiles.append((si, s0, ns, col))
            col += ns
    NS_TOT = col
    NSL = len(slot_tiles)

    def phase_GH(l):
        phase_begin()
        nonlocal xt, h32, junk, GS, stt, gate
        slotidx = sb("slotidx", [128, NSL, 16], U32)
        gvs = sb("gvs", [128, NSL, 16])
        gate = sb("gate", [128, 2, 2048])
        markH = ar["off"]
        g = alloc_norm()
        xt, h32, junk, GS, stt = g["xt"], g["h32"], g["junk"], g["GS"], g["stt"]
        h2f = [sb("h2f%d" % i, [128, 2048]) for i in range(2)]
        h2b = [sb("h2b%d" % i, [128, 2048], BF) for i in range(2)]
        h2T = sb("h2T", [128, KC, 128])
        wr = sb("wr", [128, KC, 16])
        sm = sb("smg", [128, 64])
        affT = sb("affT", [16, TA])
        work = sb("work", [16, max(T, CTX)])
        maxcap = max(cap_l, cap_c)
        vals = sb("vals", [16, maxcap])
        idxu = sb("idxu", [16, maxcap], U32)
        idxf = sb("idxf", [16, maxcap])
        slotf = sb("slotf", [128, 16])
        load_GS(l, 1, True)
        dma("sp", wr[:], w_r[l].rearrange("(kc p) e -> p kc e", p=128), r=[], w=["wr"], slot="ld6")
        for tt_i in range(NTT):
            dma("sp", moe_d[tt_i * 128:(tt_i + 1) * 128, :], zero32[:], r=["zero32"], w=[("moe", tt_i)], slot="cp%d" % (tt_i % 4))
        load_x(0)
        for tt_i in range(NTT):
            if tt_i + 1 < NTT:
                load_x(tt_i + 1)
            rows = slice(tt_i * 128, (tt_i + 1) * 128)
            hb_i = tt_i % 2
            norm_tile(tt_i, None, h2f[hb_i][:], ("h2f", hb_i))
            cp("act", h2b[hb_i][:], h2f[hb_i][:], r=[("h2f", hb_i)], w=[("h2b", hb_i)])
            dma("st", h2_d[rows, :], h2b[hb_i][:], r=[("h2b", hb_i)], w=[("h2", tt_i)], slot="ev%d" % hb_i)
            for q in range(4):
                for j in range(4):
                    kc = q * 4 + j
                    tr(psA[q][:, j * 128:(j + 1) * 128], h2f[hb_i][:, kc * 128:(kc + 1) * 128], cst[:, IDN, :], r=[("h2f", hb_i), "cst"], w=[PA[q]])
                cp("act" if q % 2 else "dve", h2T[:, q * 4:(q + 1) * 4, :], psA[q][:, :].rearrange("p (k t) -> p k t", t=128), r=[PA[q]], w=["h2T"])
            for kc in range(KC):
                mm(psA[4][:, 0:16], h2T[:, kc, :], wr[:, kc, :], kc == 0, kc == KC - 1, r=["h2T", "wr"], w=[PA[4]])
            S.op("dve", lambda e: e.reduce_max(out=sm[:, 0:1], in_=psA[4][:, 0:16], axis=AX.X), r=[PA[4]], w=["smg"])
            ts("dve", sm[:, 1:2], sm[:, 0:1], -1.0, None, ALU.mult, None, r=["smg"], w=["smg"])
            S.op("dve", lambda e: e.memset(sm[:, 2:3], 0.0), r=["smg"], w=["smg"])
            act(sm[:, 16:32], psA[4][:, 0:16], AF.Exp, r=[PA[4], "smg"], w=["smg"], bias=sm[:, 1:2], accum_out=sm[:, 2:3])
            S.op("dve", lambda e: e.reciprocal(out=sm[:, 3:4], in_=sm[:, 2:3]), r=["smg"], w=["smg"])
            ts("dve", sm[:, 32:48], sm[:, 16:32], sm[:, 3:4], None, ALU.mult, None, r=["smg"], w=["smg"])
            tr(psA[5][0:16, 0:128], sm[:, 32:48], cst[:, IDN, :], r=["smg", "cst"], w=[PA[5]])
            cp("dve", affT[:, tt_i * 128:(tt_i + 1) * 128], psA[5][0:16, 0:128], r=[PA[5]], w=["affT"])
        for si, (toff, n, cap) in enumerate(sets):
            cp("dve", work[:, 0:n], affT[:, toff:toff + n], r=["affT"], w=["work"])
            for r8 in range(cap // 8):
                sl = slice(r8 * 8, (r8 + 1) * 8)
                S.op("dve", lambda e, sl=sl, n=n: e.max(out=vals[:, sl], in_=work[:, 0:n]), r=["work"], w=["vals"])
                S.op("dve", lambda e, sl=sl, n=n: e.max_index(out=idxu[:, sl], in_max=vals[:, sl], in_values=work[:, 0:n]), r=["work", "vals"], w=["idxu"])
                S.op("dve", lambda e, sl=sl, n=n: e.match_replace(out=work[:, 0:n], in_to_replace=vals[:, sl], in_values=work[:, 0:n], imm_value=-1.0),
                     r=["work", "vals", "idxu"], w=["work"])
            cp("dve", idxf[:, 0:cap], idxu[:, 0:cap], r=["idxu"], w=["idxf"])
            ts("dve", idxf[:, 0:cap], idxf[:, 0:cap], float(toff), None, ALU.add, None, r=["idxf"], w=["idxf"])
            for sti, (sj, s0, ns, c0) in enumerate(slot_tiles):
                if sj != si:
                    continue
                tr(psA[0][0:ns, 0:16], idxf[:, s0:s0 + ns], cst[0:16, IDN, 0:16], r=["idxf", "cst"], w=[PA[0]])
                cp("dve", slotf[0:ns, :], psA[0][0:ns, 0:16], r=[PA[0]], w=["slotf"])
                cp("dve", slotidx[0:ns, sti, :], slotf[0:ns, :], r=["slotf"], w=["slotidx"])
                tr(psA[1][0:ns, 0:16], vals[:, s0:s0 + ns], cst[0:16, IDN, 0:16], r=["vals", "cst"], w=[PA[1]])
                cp("dve", gvs[0:ns, sti, :], psA[1][0:ns, 0:16], r=[PA[1]], w=["gvs"])
        if debug:
            dbg_aff = nc.dram_tensor("dbg_aff", [16, TA], F32, kind="ExternalOutput").ap()
            dbg_idx = nc.dram_tensor("dbg_idx", [128, NSL, 16], U32, kind="ExternalOutput").ap()
            dbg_gv = nc.dram_tensor("dbg_gv", [128, NSL, 16], F32, kind="ExternalOutput").ap()
            dma("sp", dbg_aff, affT[:], r=["affT"], w=["dbg_aff"], slot="ld0")
            dma("sp", dbg_idx, slotidx[:], r=["slotidx"], w=["dbg_idx"], slot="ld1")
            dma("sp", dbg_gv, gvs[:], r=["gvs"], w=["dbg_gv"], slot="ld2")
        S.barrier()
        ar["off"] = markH
        xgT = sb("xgT", [128, KC, NS_TOT], BF)
        hTe = sb("hTe", [128, 8, NS_TOT], BF)
        wg = [sb("wg%d" % i, [128, KC, 512], BF) for i in range(2)]
        wu = [sb("wu%d" % i, [128, KC, 512], BF) for i in range(2)]
        wd = [sb("wd%d" % i, [128, 8, 1024], BF) for i in range(2)]
        xg = [sb("xg%d" % i, [128, 2048], BF) for i in range(NSL)]
        ysb = [sb("ysbm%d" % i, [128, 2048]) for i in range(2)]
        sgt = [sb("sgt%d" % i, [128, 512]) for i in range(2)]
        cchunks = []
        c = 0
        for si, (_, _, cap) in enumerate(sets):
            for a in range(0, cap, 512):
                w_ = min(512, cap - a)
                cchunks.append((c + a, w_))
            c += cap
        def gathers(e_i):
            for sti, (sj, s0, ns, c0) in enumerate(slot_tiles):
                S.op("pool", lambda e, ns=ns, sti=sti, e_i=e_i: e.indirect_dma_start(
                    out=xg[sti][0:ns, :], out_offset=None, in_=h2_d[:, :],
                    in_offset=bass.IndirectOffsetOnAxis(ap=slotidx[0:ns, sti, e_i:e_i + 1], axis=0)),
                    r=K("h2", range(NTT)) + ["slotidx"], w=[("xg", sti)], slot="xg%d" % sti)

        def transposes(e_i):
            for sti, (sj, s0, ns, c0) in enumerate(slot_tiles):
                for half in range(2):
                    pt = rr(2)
                    for j in range(8):
                        kc = half * 8 + j
                        tr(psT[pt][:, j * 128:j * 128 + ns], xg[sti][0:ns, kc * 128:(kc + 1) * 128], cstb[0:ns, IDN, 0:ns],
                           r=[("xg", sti), "cstb"], w=[PT[pt]])
                    cp("act" if half else "dve", xgT[:, half * 8:(half + 1) * 8, c0:c0 + ns],
                       psT[pt][:, :].rearrange("p (k t) -> p k t", t=128)[:, :, 0:ns], r=[PT[pt]], w=["xgT"])

        def load_up(e_i, f4):
            load_w(wg[f4], w_eg[l, e_i, :, f4 * 512:(f4 + 1) * 512], KC, ("wg", f4), "wg%d" % f4)
            load_w(wu[f4], w_eu[l, e_i, :, f4 * 512:(f4 + 1) * 512], KC, ("wu", f4), "wu%d" % f4)

        def load_down(e_i):
            for half in range(2):
                load_w(wd[half], w_ed[l, e_i, :, half * 1024:(half + 1) * 1024], 8, ("wd", half), "wd%d" % half)

        def up(e_i, f4):
            b = f4
            for fc in range(4):
                for (cc0, cw_) in cchunks:
                    pg, pu = (0, 1) if rr(2) else (2, 3)
                    for kc in range(KC):
                        mm(psA[pg][:, 0:cw_], wg[b][:, kc, fc * 128:(fc + 1) * 128], xgT[:, kc, cc0:cc0 + cw_], kc == 0, kc == KC - 1,
                           r=[("wg", b), "xgT"], w=[PA[pg]])
                    for kc in range(KC):
                        mm(psA[pu][:, 0:cw_], wu[b][:, kc, fc * 128:(fc + 1) * 128], xgT[:, kc, cc0:cc0 + cw_], kc == 0, kc == KC - 1,
                           r=[("wu", b), "xgT"], w=[PA[pu]])
                    sgi = rr(2)
                    act(sgt[sgi][:, 0:cw_], psA[pg][:, 0:cw_], AF.Silu, r=[PA[pg]], w=[("sgt", sgi)])
                    tt("dve", hTe[:, f4 * 4 + fc, cc0:cc0 + cw_], sgt[sgi][:, 0:cw_], psA[pu][:, 0:cw_], ALU.mult,
                       r=[("sgt", sgi), PA[pu]], w=["hTe"])

        def down(e_i):
            for sti, (sj, s0, ns, c0) in enumerate(slot_tiles):
                yb = rr(2)
                for cg in range(4):
                    b = cg // 2
                    pb = 4 + (cg % 2)
                    for fc in range(8):
                        mm(psA[pb][0:ns, :], hTe[:, fc, c0:c0 + ns], wd[b][:, fc, (cg % 2) * 512:(cg % 2 + 1) * 512], fc == 0, fc == 7,
                           r=["hTe", ("wd", b)], w=[PA[pb]])
                    ts("dve", ysb[yb][0:ns, cg * 512:(cg + 1) * 512], psA[pb][0:ns, :], gvs[0:ns, sti, e_i:e_i + 1], None,
                       ALU.mult, None, r=[PA[pb], "gvs"], w=[("ysbm", yb)])
                S.op("pool", lambda e, yb=yb, ns=ns, sti=sti, e_i=e_i: e.indirect_dma_start(
                    out=moe_d[:, :], out_offset=bass.IndirectOffsetOnAxis(ap=slotidx[0:ns, sti, e_i:e_i + 1], axis=0),
                    in_=ysb[yb][0:ns, :], in_offset=None, compute_op=ALU.add),
                    r=[("ysbm", yb), "slotidx"], w=K("moe", range(NTT)), slot="sc%d" % yb)

        gathers(0)
        load_up(0, 0)
        for e_i in range(NE):
            transposes(e_i)
            load_up(e_i, 1)
            up(e_i, 0)
            load_down(e_i)
            up(e_i, 1)
            if e_i + 1 < NE:
                gathers(e_i + 1)
                load_up(e_i + 1, 0)
            down(e_i)
        S.barrier()
        ar["off"] = markH
        xo = [sb("xo%d" % i, [128, 2048]) for i in range(2)]
        mo = [sb("mo%d" % i, [128, 2048]) for i in range(2)]
        for tt_i in range(NTT):
            b = tt_i % 2
            st = 1 if tt_i < CTX // 128 else 0
            rows = slice(tt_i * 128, (tt_i + 1) * 128)
            dma("sp", xo[b][:], xres[rows, :], r=[("xres", tt_i)], w=[("xo", b)], slot="xo%d" % b)
            dma("sp", mo[b][:], moe_d[rows, :], r=[("moe", tt_i)], w=[("mo", b)], slot="mo%d" % b)
            tt("dve", mo[b][:], mo[b][:], gate[:, st, :], ALU.mult, r=[("mo", b), "gate"], w=[("mo", b)])
            tt("pool", xo[b][:], xo[b][:], mo[b][:], ALU.add, r=[("xo", b), ("mo", b)], w=[("xo", b)])
            dma("st", xres[rows, :], xo[b][:], r=[("xo", b)], w=[("xres", tt_i)], slot="ev%d" % b)

    zb = sb("zb", [128, 2048], BF)
    S.op("pool", lambda e: e.memset(zb[:], 0.0), w=["zb"])
    ar["perm"] = ar["off"]
    for j4 in range(4):
        for c0 in range(0, TA, 2048):
            n = min(2048, TA - c0)
            dma("sp", mixT[1024 + j4 * 128:1024 + (j4 + 1) * 128, c0:c0 + n], zb[:, 0:n], r=["zb"],
                w=[("mixT", 8 + j4, q) for q in range(c0 // 128, (c0 + n) // 128)], slot="cp%d" % (j4 % 4))
    phase0()
    for l in range(L):
        if upto < 1:
            continue
        phase_AB(l)
        if upto < 2:
            continue
        phase_E(l)
        if upto < 3:
            continue
        phase_C(l)
        if upto < 4:
            continue
        phase_D(l)
        if upto < 5:
            continue
        phase_F(l)
        if upto < 6:
            continue
        phase_GH(l)
    phase_begin()
    for tt_i in range(T // 128):
        rows = slice(tt_i * 128, (tt_i + 1) * 128)
        dma("sp", out_d[rows, :], xres[CTX + tt_i * 128:CTX + (tt_i + 1) * 128, :], r=[("xres", CTX // 128 + tt_i)],
            w=[("out", tt_i)], slot="cp%d" % (tt_i % 4))
    stats = S.emit(nc)
    es.close()
    return nc, stats


def host_inputs(inputs, T, L):
    f = np.float32
    r = {}
    p = np.arange(128)[:, None]
    j = np.arange(128)[None, :]
    consts = np.stack([(p == j), (p <= j), (p >= j), (p > j), (p < j)], axis=1).astype(f)
    r["consts"] = np.ascontiguousarray(consts)
    rows = T // 64
    row = np.repeat(np.arange(rows), 64).astype(f)
    col = np.tile(np.arange(64), rows).astype(f)
    inv = (10000.0 ** (-np.arange(32, dtype=f) / 32)).astype(f)
    ar = row[:, None] * inv
    ac = col[:, None] * inv
    cos = np.concatenate([np.cos(ar), np.cos(ar), np.cos(ac), np.cos(ac)], axis=1).astype(f)
    sin = np.concatenate([-np.sin(ar), np.sin(ar), -np.sin(ac), np.sin(ac)], axis=1).astype(f)
    r["ropec"] = np.ascontiguousarray(np.tile(cos, (1, 8)))
    r["ropes"] = np.ascontiguousarray(np.tile(sin, (1, 8)))
    for k in ("w_mod", "b_mod", "w_in", "w_out", "w_router", "w_expert_gate", "w_expert_up", "w_expert_down",
              "norm1_g", "norm2_g", "q_norm_g", "k_norm_g", "attn_sink", "ssd_d", "ssd_norm_g"):
        r[k] = np.ascontiguousarray(np.asarray(inputs[k], dtype=f)[:L])
    r["ssd_dt_bias"] = np.ascontiguousarray(np.asarray(inputs["ssd_dt_bias"], f)[:L].reshape(L, 16))
    r["ssd_a_log"] = np.ascontiguousarray(np.asarray(inputs["ssd_a_log"], f)[:L].reshape(L, 16))
    cw = np.asarray(inputs["ssd_conv_w"], f)[:L]
    r["convw"] = np.ascontiguousarray(cw.reshape(L, 3, 8, 128).transpose(0, 3, 2, 1))
    r["convb"] = np.ascontiguousarray(np.asarray(inputs["ssd_conv_b"], f)[:L].reshape(L, 8, 128).transpose(0, 2, 1))
    sw = np.asarray(inputs["sc_conv_w"], f)[:L]
    r["scw"] = np.ascontiguousarray(sw.reshape(L, 3, 4, 128).transpose(0, 3, 2, 1))
    x = np.asarray(inputs["x"], f)
    ctx = np.asarray(inputs["ctx"], f)
    c = np.asarray(inputs["c"], f)
    cc = np.asarray(inputs["c_ctx"], f)
    maps = []
    for b in range(x.shape[0]):
        m = dict(r)
        m["xall"] = np.ascontiguousarray(np.concatenate([ctx[b], x[b, :T]], axis=0))
        cs = np.stack([c[b], cc], axis=0)
        m["cT"] = np.ascontiguousarray(cs.reshape(2, KC, 128).transpose(2, 1, 0))
        maps.append(m)
    return maps


_CACHE = {}


def kernel(**inputs):
    T = inputs["x"].shape[1]
    L = inputs["w_mod"].shape[0]
    B = inputs["x"].shape[0]
    key = (T, L)
    if key not in _CACHE:
        _CACHE[key] = build(T, L)[0]
    nc = _CACHE[key]
    maps = host_inputs(inputs, T, L)
    res = run_bass_kernel_spmd(nc, maps, core_ids=list(range(B)))
    return np.stack([np.asarray(res.results[b]["out"]) for b in range(B)], axis=0).astype(np.float32)
```

```python
import math
from contextlib import ExitStack

import numpy as np
import concourse.bass as bass
import concourse.mybir as mybir
from concourse.bass_utils import run_bass_kernel_spmd

F32 = mybir.dt.float32
BF = mybir.dt.bfloat16
U32 = mybir.dt.uint32
AF = mybir.ActivationFunctionType
ALU = mybir.AluOpType
AX = mybir.AxisListType

D = 2048
KC = 16
CTX = 256
NE = 16
FF = 1024
EPS = 1e-6
C_Q, C_K, C_V, C_Z, C_XBC, C_DT, C_SB, C_SC, C_SH, C_END = 0, 1024, 1280, 1536, 2048, 3072, 3088, 3600, 4112, 4624

SAME_ENGINE_SYNC = True
EPOCH = 30000


class Op:
    __slots__ = ("eng", "fn", "slot", "deps", "needs_inc", "val", "idx", "ep")

    def __init__(self, eng, fn, slot):
        self.eng, self.fn, self.slot = eng, fn, slot
        self.deps = set()
        self.needs_inc = False
        self.val = 0


class Sch:
    ENGS = ("pe", "act", "dve", "pool", "sp")

    def __init__(self):
        self.ops = []
        self.last_w = {}
        self.readers = {}
        self.slot_last = {}

    def barrier(self):
        last = {}
        for o in self.ops:
            if o.fn is None:
                continue
            last[o.slot if o.slot is not None else ("eng", o.eng)] = o
        for e in self.ENGS:
            b = Op(e, None, None)
            b.idx = len(self.ops)
            b.deps = set(last.values())
            self.ops.append(b)

    def op(self, eng, fn, r=(), w=(), slot=None):
        o = Op(eng, fn, slot)
        o.idx = len(self.ops)
        deps = o.deps
        for k in r:
            lw = self.last_w.get(k)
            if lw is not None:
                deps.add(lw)
        for k in w:
            lw = self.last_w.get(k)
            if lw is not None:
                deps.add(lw)
            for rd in self.readers.get(k, ()):
                deps.add(rd)
        if slot is not None:
            p = self.slot_last.get(slot)
            if p is not None:
                deps.add(p)
            self.slot_last[slot] = o
        deps.discard(o)
        for k in r:
            self.readers.setdefault(k, []).append(o)
        for k in w:
            self.last_w[k] = o
            self.readers[k] = []
        self.ops.append(o)
        return o

    def emit(self, nc):
        for o in self.ops:
            keep = set()
            for d in o.deps:
                if o.fn is not None and d.slot is None and d.eng == o.eng and (o.eng == "pe" or not SAME_ENGINE_SYNC):
                    continue
                keep.add(d)
            o.deps = keep
            for d in keep:
                d.needs_inc = True
        eng_cnt = {e: 0 for e in self.ENGS}
        slot_cnt = {}
        slots = []
        for o in self.ops:
            if o.slot is not None:
                if o.slot not in slot_cnt:
                    slot_cnt[o.slot] = 0
                    slots.append(o.slot)
                slot_cnt[o.slot] += 16
                o.val = slot_cnt[o.slot]
                o.needs_inc = True
            elif o.needs_inc:
                o.ep = eng_cnt[o.eng] // EPOCH
                o.val = eng_cnt[o.eng] % EPOCH + 1
                eng_cnt[o.eng] += 1
        with ExitStack() as es:
            esem = {(e, k): es.enter_context(nc.semaphore("e_%s%d" % (e, k)))
                    for e in self.ENGS if e != "sp" for k in range(eng_cnt[e] // EPOCH + 1)}
            ssem = {s: es.enter_context(nc.semaphore("s_%d" % i)) for i, s in enumerate(slots)}
            block = es.enter_context(nc.Block())
            engobj = {"pe": "tensor", "act": "scalar", "dve": "vector", "pool": "gpsimd", "sp": "sync"}

            def semof(o):
                return ssem[o.slot] if o.slot is not None else esem[(o.eng, o.ep)]

            def run(e):
                def body(eng):
                    waited = {}
                    for o in self.ops:
                        if o.eng != e:
                            continue
                        need = {}
                        for d in o.deps:
                            s = semof(d)
                            key = id(s)
                            if need.get(key, (0, None))[0] < d.val:
                                need[key] = (d.val, s)
                        for key, (val, s) in need.items():
                            if waited.get(key, 0) >= val:
                                continue
                            eng.wait_ge(s, val)
                            waited[key] = val
                        if o.fn is None:
                            continue
                        ins = o.fn(eng)
                        if o.needs_inc:
                            ins.then_inc(semof(o), 16 if o.slot is not None else 1)
                    if e == "sp":
                        for s in slots:
                            eng.wait_ge(ssem[s], slot_cnt[s])
                return body

            for e in self.ENGS:
                getattr(block, engobj[e])(run(e))
        return eng_cnt, slot_cnt


def build(T, L, debug=False, upto=99):
    TA = CTX + T
    NTT = TA // 128
    NCH = NTT
    cap_l = 2 * T // NE
    cap_c = 2 * CTX // NE
    assert cap_l % 128 == 0 or cap_l <= 128
    nc = bass.Bass("TRN2", target_bir_lowering=False)
    S = Sch()
    es = ExitStack()

    def din(name, shape, dt=F32):
        return nc.dram_tensor(name, list(shape), dt, kind="ExternalInput").ap()

    def dscr(name, shape, dt=F32):
        return nc.dram_tensor(name, list(shape), dt, kind="ExternalOutput" if debug else "Internal").ap()

    ARENA_N = 52224
    arena = es.enter_context(nc.sbuf_tensor("arena", [128, ARENA_N], F32))
    ar = {"off": 0}

    def sb(name, shape, dt=F32):
        shape = list(shape)
        nelem = 1
        for x in shape[1:]:
            nelem *= x
        size = 4 if dt in (F32, U32) else 2
        n32 = (nelem * size + 3) // 4
        n32 = (n32 + 7) // 8 * 8
        off = ar["off"]
        assert off + n32 <= ARENA_N, ("arena overflow", name, off, n32)
        ar["off"] = off + n32
        ap = arena[0:shape[0], off:off + n32]
        if dt != F32:
            ap = ap.bitcast(dt)
        ap = ap[:, 0:nelem]
        if len(shape) == 3:
            ap = ap.rearrange("p (a b) -> p a b", b=shape[2])
        elif len(shape) == 4:
            ap = ap.rearrange("p (a b c) -> p a b c", b=shape[2], c=shape[3])
        return ap

    def phase_begin():
        S.barrier()
        ar["off"] = ar["perm"]

    xall = din("xall", [TA, D])
    cT_d = din("cT", [128, KC, 2])
    ropec_d = din("ropec", [T, 1024])
    ropes_d = din("ropes", [T, 1024])
    consts_d = din("consts", [128, 5, 128])
    w_mod = din("w_mod", [L, D, 6 * D])
    b_mod = din("b_mod", [L, 6 * D])
    w_in = din("w_in", [L, D, C_END])
    w_out = din("w_out", [L, D, D])
    w_r = din("w_router", [L, D, NE])
    w_eg = din("w_expert_gate", [L, NE, D, FF])
    w_eu = din("w_expert_up", [L, NE, D, FF])
    w_ed = din("w_expert_down", [L, NE, FF, D])
    norm1_g = din("norm1_g", [L, D])
    norm2_g = din("norm2_g", [L, D])
    qng = din("q_norm_g", [L, 128])
    kng = din("k_norm_g", [L, 128])
    sink_d = din("attn_sink", [L, 8])
    convw_d = din("convw", [L, 128, 8, 3])
    convb_d = din("convb", [L, 128, 8])
    dtb_d = din("ssd_dt_bias", [L, 16])
    alog_d = din("ssd_a_log", [L, 16])
    dsk_d = din("ssd_d", [L, 8])
    sng_d = din("ssd_norm_g", [L, 512])
    scw_d = din("scw", [L, 128, 4, 3])
    out_d = nc.dram_tensor("out", [T, D], F32, kind="ExternalOutput").ap()

    xres = dscr("xres", [TA, D])
    modd = dscr("modd", [L, 2, 6 * D])
    gsd = dscr("gsd", [L, 2, 6, D])
    q_raw = dscr("q_raw", [TA, 1024])
    k_raw = dscr("k_raw", [TA, 256])
    v_tok = dscr("v_tok", [TA, 256], BF)
    z_tok = dscr("z_tok", [TA, 512])
    dt_raw = dscr("dt_raw", [TA, 16])
    xbcT = dscr("xbcT", [1024, TA])
    scT = dscr("scT", [1536, TA])
    qT = dscr("qT", [8, 128, TA], BF)
    kT = dscr("kT", [2, 128, TA], BF)
    mixT = dscr("mixT", [D, TA], BF)
    convT = dscr("convT", [1024, TA], BF)
    xbc_tok = dscr("xbc_tok", [TA, 768], BF)
    y_f = dscr("y_f", [TA, 512])
    h2_d = dscr("h2", [TA, D], BF)
    moe_d = dscr("moe_out", [TA, D])

    cst = sb("cst", [128, 5, 128])
    cstb = sb("cstb", [128, 5, 128], BF)
    onesb = sb("onesb", [128, 128], BF)
    ones32 = sb("ones32", [128, 128])
    zero32 = sb("zero32", [128, 2048])
    IDN, LE, GE, GT, LT = 0, 1, 2, 3, 4
    S.op("sp", lambda e: e.dma_start(out=cst[:], in_=consts_d), w=["cst"], slot="ld0")
    S.op("dve", lambda e: e.tensor_copy(out=cstb[:], in_=cst[:]), r=["cst"], w=["cstb"])
    S.op("dve", lambda e: e.memset(onesb[:], 1.0), w=["onesb"])
    S.op("dve", lambda e: e.memset(ones32[:], 1.0), w=["ones32"])
    S.op("pool", lambda e: e.memset(zero32[:], 0.0), w=["zero32"])

    psA = [es.enter_context(nc.psum_tensor("psA%d" % i, [128, 512], F32)) for i in range(6)]
    psT = [es.enter_context(nc.psum_tensor("psT%d" % i, [128, 1024], BF)) for i in range(2)]
    PA = ["psA%d" % i for i in range(6)]
    PT = ["psT0", "psT1"]

    cnt = {"i": 0}

    def rr(n):
        cnt["i"] += 1
        return cnt["i"] % n

    for tt in range(NTT):
        rows = slice(tt * 128, (tt + 1) * 128)
        S.op("sp", lambda e, rows=rows: e.dma_start(out=xres[rows, :], in_=xall[rows, :]),
             w=[("xres", tt)], slot="cp%d" % (tt % 4))

    def mm(out, lhsT, rhs, start, stop, r, w):
        import os
        if os.environ.get("NOMM"):
            return
        S.op("pe", lambda e: e.matmul(out, lhsT=lhsT, rhs=rhs, start=start, stop=stop), r=r, w=w)

    def tr(out, in_, ident, r, w):
        S.op("pe", lambda e: e.transpose(out=out, in_=in_, identity=ident), r=r, w=w)

    def act(out, in_, func, r, w, **kw):
        S.op("act", lambda e: e.activation(out=out, in_=in_, func=func, **kw), r=r, w=w)

    def tt(eng, out, in0, in1, op, r, w):
        S.op(eng, lambda e: e.tensor_tensor(out=out, in0=in0, in1=in1, op=op), r=r, w=w)

    def ts(eng, out, in0, s1, s2, op0, op1, r, w):
        if s2 is None:
            S.op(eng, lambda e: e.tensor_scalar(out=out, in0=in0, scalar1=s1, scalar2=None, op0=op0), r=r, w=w)
        else:
            S.op(eng, lambda e: e.tensor_scalar(out=out, in0=in0, scalar1=s1, scalar2=s2, op0=op0, op1=op1), r=r, w=w)

    def stt_(eng, out, in0, scalar, in1, op0, op1, r, w):
        S.op(eng, lambda e: e.scalar_tensor_tensor(out=out, in0=in0, scalar=scalar, in1=in1, op0=op0, op1=op1), r=r, w=w)

    def cp(eng, out, in_, r, w):
        if eng == "act":
            S.op(eng, lambda e: e.copy(out=out, in_=in_), r=r, w=w)
        else:
            S.op(eng, lambda e: e.tensor_copy(out=out, in_=in_), r=r, w=w)

    def dma(eng, out, in_, r, w, slot, **kw):
        import os
        if os.environ.get("NOST") and slot.startswith("ev"):
            return
        if eng == "st":
            eng = "act"
        S.op(eng, lambda e: e.dma_start(out=out, in_=in_, **kw), r=r, w=w, slot=slot)

    def bc_rows(ap, P=128):
        pat = [list(x) for x in ap.ap]
        while len(pat) > 1 and pat[0][1] == 1:
            pat = pat[1:]
        return bass.AP(ap.tensor, ap.offset, [[0, P]] + pat)

    def bc_last(ap, n):
        pat = [list(x) for x in ap.ap]
        return bass.AP(ap.tensor, ap.offset, pat + [[0, n]])

    def bc_mid(ap, n):
        pat = [list(x) for x in ap.ap]
        return bass.AP(ap.tensor, ap.offset, [pat[0], [0, n]] + pat[1:])

    wslot = {"i": 0}
    NWS = 8

    def load_w(dst, src, nk, key, slot):
        for k0 in range(0, nk, 4):
            m = min(4, nk - k0)
            wslot["i"] += 1
            dma("pool", dst[:, k0:k0 + m, :], src[k0 * 128:(k0 + m) * 128, :].rearrange("(k p) n -> p k n", p=128),
                r=[], w=[key], slot="W%d" % (wslot["i"] % NWS), max_dma_last_dim=8192)

    def evac_eng():
        return "act" if rr(2) else "dve"

    def K(name, rng):
        return [(name, i) for i in rng]

    def rstd_from_ss(ssap, outap, n, tmpap, keys):
        ts("dve", tmpap, ssap, 1.0 / n, EPS, ALU.mult, ALU.add, r=keys, w=keys)
        act(tmpap, tmpap, AF.Sqrt, r=keys, w=keys)
        S.op("dve", lambda e: e.reciprocal(out=outap, in_=tmpap), r=keys, w=keys)

    ar["perm"] = ar["off"]

    def phase0():
        phase_begin()
        cTs = sb("cTs", [128, KC, 2])
        cTb = sb("cTb", [128, KC, 2], BF)
        mblk = sb("mblk", [2, 2048])
        bblk = sb("bblk", [2, 2048])
        gblk = sb("gblk", [2, 2048])
        oblk = sb("oblk", [2, 2048])
        wm = [sb("wm%d" % i, [128, 2048], BF) for i in range(4)]
        dma("sp", cTs[:], cT_d, r=[], w=["cTs"], slot="ld0")
        act(cTb[:], cTs[:], AF.Silu, r=["cTs"], w=["cTb"])
        idxmap = {0: 1, 1: 0, 2: 2, 3: 4, 4: 3, 5: 5}
        for l in range(L):
            for cb in range(6):
                for kc in range(KC):
                    b = rr(4)
                    dma("pool", wm[b][:], w_mod[l, kc * 128:(kc + 1) * 128, cb * 2048:(cb + 1) * 2048],
                        r=[], w=[("wm", b)], slot="W%d" % (rr(NWS)), max_dma_last_dim=8192)
                    for j in range(4):
                        mm(psA[j][0:2, :], cTb[:, kc, :], wm[b][:, j * 512:(j + 1) * 512], kc == 0, kc == KC - 1,
                           r=["cTb", ("wm", b)], w=[PA[j]])
                for p in range(2):
                    dma("sp", bblk[p:p + 1, :], b_mod[l:l + 1, cb * 2048:(cb + 1) * 2048], r=[], w=["bblk"], slot="ld%d" % p)
                    if cb in (1, 4):
                        ng = norm1_g if cb == 1 else norm2_g
                        dma("sp", gblk[p:p + 1, :], ng[l:l + 1, :], r=[], w=["gblk"], slot="ld%d" % (2 + p))
                for j in range(4):
                    tt("dve", mblk[:, j * 512:(j + 1) * 512], psA[j][0:2, :], bblk[:, j * 512:(j + 1) * 512], ALU.add,
                       r=[PA[j], "bblk"], w=["mblk"])
                if cb in (1, 4):
                    stt_("dve", oblk[:], mblk[:], 1.0, gblk[:], ALU.add, ALU.mult, r=["mblk", "gblk"], w=["oblk"])
                else:
                    cp("dve", oblk[:], mblk[:], r=["mblk"], w=["oblk"])
                dma("sp", gsd[l, :, idxmap[cb], :], oblk[:], r=["oblk"], w=[("gsd", l)], slot="st0")

    blocks = [(0, CTX)]
    BLK = min(2048, T)
    for i in range(T // BLK):
        blocks.append((CTX + i * BLK, BLK))

    xt = h32 = junk = GS = stt = gate = None

    def load_GS(l, which, with_gate):
        for st in range(2):
            for j in range(2):
                dma("sp", GS[:, st, j, :], bc_rows(gsd[l, st, 3 * which + j:3 * which + j + 1, :]),
                    r=[("gsd", l)], w=["GS"], slot="ld%d" % (st * 2 + j))
            if with_gate:
                dma("sp", gate[:, st, :], bc_rows(gsd[l, st, 3 * which + 2:3 * which + 3, :]),
                    r=[("gsd", l)], w=["gate"], slot="ld%d" % (4 + st))

    def norm_tile(tt_i, xsrc_key, out_ap, out_key):
        b = tt_i % 2
        st = 1 if tt_i < CTX // 128 else 0
        sq_, h_, s_ = junk[b], h32[b], stt[b]
        S.op("pool", lambda e, a=s_: e.memset(a[:], 0.0), w=[("stt", b)])
        act(sq_[:], xt[b][:], AF.Square, r=[("xt", b)], w=[("junk", b), ("stt", b)], accum_out=s_[:, 0:1])
        rstd_from_ss(s_[:, 0:1], s_[:, 2:3], D, s_[:, 1:2], [("stt", b)])
        stt_("dve", h_[:], xt[b][:], s_[:, 2:3], GS[:, st, 0, :], ALU.mult, ALU.mult, r=[("xt", b), ("stt", b), "GS"], w=[("h32", b)])
        tt("pool", out_ap, h_[:], GS[:, st, 1, :], ALU.add, r=[("h32", b), "GS"], w=[out_key])

    def load_x(tt_i):
        b = tt_i % 2
        dma("sp", xt[b][:], xres[tt_i * 128:(tt_i + 1) * 128, :], r=[("xres", tt_i)], w=[("xt", b)], slot="xt%d" % b)

    def alloc_norm():
        g = {}
        g["xt"] = [sb("xt%d" % i, [128, 2048]) for i in range(2)]
        g["h32"] = [sb("h32_%d" % i, [128, 2048]) for i in range(2)]
        g["junk"] = [sb("junk%d" % i, [128, 2048], BF) for i in range(2)]
        g["GS"] = sb("GS", [128, 2, 2, 2048])
        g["stt"] = [sb("stt%d" % i, [128, 16]) for i in range(2)]
        return g

    def phase_AB(l):
        phase_begin()
        nonlocal xt, h32, junk, GS, stt, gate
        g = alloc_norm()
        xt, h32, junk, GS, stt = g["xt"], g["h32"], g["junk"], g["GS"], g["stt"]
        hT = sb("hT", [128, KC, 2048], BF)
        wbuf = [sb("wbuf%d" % i, [128, KC, 512], BF) for i in range(2)]
        hb = [sb("hb%d" % i, [128, 2048], BF) for i in range(2)]
        ev = [sb("ev%d" % i, [128, 512]) for i in range(2)]
        evb = [sb("evb%d" % i, [128, 512], BF) for i in range(2)]
        wdt = sb("wdt", [128, KC, 16], BF)
        load_GS(l, 0, False)
        for (t0, n) in blocks:
            nt = n // 128
            tt0 = t0 // 128
            def a_norm(ti):
                hbi = (tt0 + ti) % 2
                norm_tile(tt0 + ti, None, hb[hbi][:], ("hb", hbi))

            def a_trans(ti):
                hbi = (tt0 + ti) % 2
                for half in range(2):
                    for j in range(8):
                        kc = half * 8 + j
                        tr(psT[half][:, j * 128:(j + 1) * 128], hb[hbi][:, kc * 128:(kc + 1) * 128], cstb[:, IDN, :],
                           r=[("hb", hbi), "cstb"], w=[PT[half]])
                    cp("act" if half else "dve", hT[:, half * 8:(half + 1) * 8, ti * 128:(ti + 1) * 128],
                       psT[half][:].rearrange("p (k t) -> p k t", k=8), r=[PT[half]], w=[("hT", ti)])

            load_x(tt0)
            if nt > 1:
                load_x(tt0 + 1)
            a_norm(0)
            for ti in range(nt):
                if ti + 1 < nt:
                    a_norm(ti + 1)
                if ti + 2 < nt:
                    load_x(tt0 + ti + 2)
                a_trans(ti)
            import os
            if os.environ.get("STOP") == "A":
                continue
            groups = [(C_Q, "q0"), (C_Q + 512, "q1"), (C_K, "kv"), (C_Z, "z")]
            if os.environ.get("GRP"):
                groups = [g for g in groups if g[1] in os.environ["GRP"].split(",")]
            for (c0, gname) in groups:
                b = rr(2)
                load_w(wbuf[b], w_in[l, :, c0:c0 + 512], KC, ("wbuf", b), "wb%d" % b)
                for ti in range(nt):
                    pb = rr(6)
                    rows = slice(t0 + ti * 128, t0 + (ti + 1) * 128)
                    for kc in range(KC):
                        mm(psA[pb][:, :], hT[:, kc, ti * 128:(ti + 1) * 128], wbuf[b][:, kc, :], kc == 0, kc == KC - 1,
                           r=[("hT", ti), ("wbuf", b)], w=[PA[pb]])
                    eb = rr(2)
                    eng = evac_eng()
                    tkey = tt0 + ti
                    if gname == "kv":
                        cp(eng, ev[eb][:], psA[pb][:], r=[PA[pb]], w=[("ev", eb)])
                        cp("pool", evb[eb][:, 0:256], ev[eb][:, 256:512], r=[("ev", eb)], w=[("evb", eb)])
                        dma("st", k_raw[rows, :], ev[eb][:, 0:256], r=[("ev", eb)], w=[("k_raw", tkey)], slot="ev%d" % eb)
                        dma("st", v_tok[rows, :], evb[eb][:, 0:256], r=[("evb", eb)], w=[("v_tok", tkey)], slot="evb%d" % eb)
                    else:
                        cp(eng, ev[eb][:], psA[pb][:], r=[PA[pb]], w=[("ev", eb)])
                        if gname == "q0":
                            dma("st", q_raw[rows, 0:512], ev[eb][:], r=[("ev", eb)], w=[("q_raw0", tkey)], slot="ev%d" % eb)
                        elif gname == "q1":
                            dma("st", q_raw[rows, 512:1024], ev[eb][:], r=[("ev", eb)], w=[("q_raw1", tkey)], slot="ev%d" % eb)
                        else:
                            dma("st", z_tok[rows, :], ev[eb][:], r=[("ev", eb)], w=[("z_tok", tkey)], slot="ev%d" % eb)
            if os.environ.get("STOP") == "B1":
                continue
            load_w(wdt, w_in[l, :, C_DT:C_DT + 16], KC, "wdt", "wdt")
            for ti in range(nt):
                pb = rr(6)
                rows = slice(t0 + ti * 128, t0 + (ti + 1) * 128)
                for kc in range(KC):
                    mm(psA[pb][:, 0:16], hT[:, kc, ti * 128:(ti + 1) * 128], wdt[:, kc, :], kc == 0, kc == KC - 1,
                       r=[("hT", ti), "wdt"], w=[PA[pb]])
                eb = rr(2)
                cp(evac_eng(), ev[eb][:, 0:16], psA[pb][:, 0:16], r=[PA[pb]], w=[("ev", eb)])
                dma("st", dt_raw[rows, :], ev[eb][:, 0:16], r=[("ev", eb)], w=[("dt_raw", tt0 + ti)], slot="ev%d" % eb)
            fgroups = [(C_XBC + i * 512, xbcT, i * 512, "xbcT") for i in range(2)] + \
                      [(C_SB + i * 512, scT, i * 512, "scT") for i in range(3)]
            cw = min(512, n)
            for (c0, dst, row0, dname) in fgroups:
                b = rr(2)
                load_w(wbuf[b], w_in[l, :, c0:c0 + 512], KC, ("wbuf", b), "wb%d" % b)
                for j in range(4):
                    for ci in range(n // cw):
                        pb = rr(6)
                        for kc in range(KC):
                            mm(psA[pb][:, 0:cw], wbuf[b][:, kc, j * 128:(j + 1) * 128], hT[:, kc, ci * cw:(ci + 1) * cw],
                               kc == 0, kc == KC - 1,
                               r=K("hT", range(ci * cw // 128, (ci + 1) * cw // 128)) + [("wbuf", b)], w=[PA[pb]])
                        eb = rr(2)
                        cp(evac_eng(), ev[eb][:, 0:cw], psA[pb][:, 0:cw], r=[PA[pb]], w=[("ev", eb)])
                        ch0 = row0 + j * 128
                        dma("st", dst[ch0:ch0 + 128, t0 + ci * cw:t0 + (ci + 1) * cw], ev[eb][:, 0:cw], r=[("ev", eb)],
                            w=[(dname, ch0 // 128, (t0 + ci * cw) // 128 + q) for q in range(cw // 128)], slot="ev%d" % eb)

    def conv_pieces():
        P = [(0, CTX, 0, CTX)]
        step = min(T, 2048)
        for a in range(0, T, step):
            P.append((CTX + a, step, CTX, CTX + T))
        return P

    def load_halo(dst, src_rows, t0, n, lo, hi, key, slot, rkeys):
        a = max(t0 - 1, lo)
        b = min(t0 + n + 1, hi)
        if a > t0 - 1:
            S.op("pool", lambda e: e.memset(dst[:, 0:1], 0.0), w=[key])
        if b < t0 + n + 1:
            S.op("pool", lambda e: e.memset(dst[:, n + 1:n + 2], 0.0), w=[key])
        dma("sp", dst[:, a - (t0 - 1):b - (t0 - 1)], src_rows[:, a:b], r=rkeys, w=[key], slot=slot)

    def phase_E(l):
        phase_begin()
        NP = min(T, 2048) + 2
        cin = sb("cin", [128, NP])
        hin = sb("hin", [128, NP])
        bin_ = sb("bin", [128, NP])
        acc = sb("acc", [128, NP])
        ob = sb("ob", [128, NP], BF)
        scw = sb("scw_s", [128, 4, 3])
        dma("sp", scw[:], scw_d[l], r=[], w=["scw"], slot="ld0")
        for j in range(4):
            for (t0, n, lo, hi) in conv_pieces():
                tts = range(t0 // 128, (t0 + n) // 128)
                tth = range(max(t0 - 128, lo) // 128, min(t0 + n + 128, hi) // 128)
                load_halo(cin, scT[512 + j * 128:512 + (j + 1) * 128, :], t0, n, lo, hi, "cin", "ld1",
                          [("scT", 4 + j, q) for q in tth])
                load_halo(hin, scT[1024 + j * 128:1024 + (j + 1) * 128, :], t0, n, lo, hi, "hin", "ld2",
                          [("scT", 8 + j, q) for q in tth])
                dma("sp", bin_[:, 0:n], scT[j * 128:(j + 1) * 128, t0:t0 + n], r=[("scT", j, q) for q in tts], w=["bin"], slot="ld3")
                tt("dve", cin[:, 0:n + 2], cin[:, 0:n + 2], hin[:, 0:n + 2], ALU.mult, r=["cin", "hin"], w=["cin"])
                ts("pool", acc[:, 0:n], cin[:, 1:n + 1], scw[:, j, 1:2], None, ALU.mult, None, r=["cin", "scw"], w=["acc"])
                stt_("dve", acc[:, 0:n], cin[:, 0:n], scw[:, j, 0:1], acc[:, 0:n], ALU.mult, ALU.add, r=["cin", "scw", "acc"], w=["acc"])
                stt_("dve", acc[:, 0:n], cin[:, 2:n + 2], scw[:, j, 2:3], acc[:, 0:n], ALU.mult, ALU.add, r=["cin", "scw", "acc"], w=["acc"])
                tt("pool", ob[:, 0:n], acc[:, 0:n], bin_[:, 0:n], ALU.mult, r=["acc", "bin"], w=["ob"])
                dma("st", mixT[1536 + j * 128:1536 + (j + 1) * 128, t0:t0 + n], ob[:, 0:n], r=["ob"],
                    w=[("mixT", 12 + j, q) for q in tts], slot="ev0")

    def phase_F(l):
        phase_begin()
        nonlocal gate
        hT = sb("hT", [128, KC, 2048], BF)
        wbuf = [sb("wbuf%d" % i, [128, KC, 512], BF) for i in range(2)]
        gate = sb("gate", [128, 2, 2048])
        xo = [sb("xo%d" % i, [128, 512]) for i in range(2)]
        t1 = [sb("t1%d" % i, [128, 512]) for i in range(2)]
        for st in range(2):
            dma("sp", gate[:, st, :], bc_rows(gsd[l, st, 2:3, :]), r=[("gsd", l)], w=["gate"], slot="ld%d" % (4 + st))
        for (t0, n) in blocks:
            nt = n // 128
            tt0 = t0 // 128
            st = 1 if t0 < CTX else 0
            for kc in range(KC):
                dma("sp", hT[:, kc, 0:n], mixT[kc * 128:(kc + 1) * 128, t0:t0 + n],
                    r=[("mixT", kc, q) for q in range(tt0, tt0 + nt)], w=[("hT", q) for q in range(nt)], slot="ld%d" % (kc % 4))
            for cg in range(4):
                b = rr(2)
                load_w(wbuf[b], w_out[l, :, cg * 512:(cg + 1) * 512], KC, ("wbuf", b), "wb%d" % b)
                for ti in range(nt):
                    pb = rr(6)
                    eb = rr(2)
                    rows = slice(t0 + ti * 128, t0 + (ti + 1) * 128)
                    dma("sp", xo[eb][:], xres[rows, cg * 512:(cg + 1) * 512], r=[("xres", tt0 + ti)], w=[("xo", eb)], slot="xo%d" % eb)
                    for kc in range(KC):
                        mm(psA[pb][:, :], hT[:, kc, ti * 128:(ti + 1) * 128], wbuf[b][:, kc, :], kc == 0, kc == KC - 1,
                           r=[("hT", ti), ("wbuf", b)], w=[PA[pb]])
                    tt("dve", t1[eb][:], psA[pb][:], gate[:, st, cg * 512:(cg + 1) * 512], ALU.mult, r=[PA[pb], "gate"], w=[("t1", eb)])
                    tt("pool", t1[eb][:], t1[eb][:], xo[eb][:], ALU.add, r=[("t1", eb), ("xo", eb)], w=[("t1", eb)])
                    dma("st", xres[rows, cg * 512:(cg + 1) * 512], t1[eb][:], r=[("t1", eb)], w=[("xres", tt0 + ti)], slot="ev%d" % eb)

    def phase_C(l):
        phase_begin()
        NL = T // 128
        gq = sb("gq", [128, 128])
        gk = sb("gk", [128, 128])
        esink = sb("esink", [128, 8])
        dma("sp", gq[:], bc_rows(qng[l:l + 1, :]), r=[], w=["gq"], slot="ld0")
        dma("sp", gk[:], bc_rows(kng[l:l + 1, :]), r=[], w=["gk"], slot="ld1")
        dma("sp", esink[:], bc_rows(sink_d[l:l + 1, :]), r=[], w=["esink"], slot="ld2")
        act(esink[:], esink[:], AF.Exp, r=["esink"], w=["esink"])
        mark = ar["off"]
        src = [sb("src%d" % i, [128, 1024]) for i in range(2)]
        sq2 = [sb("sq%d" % i, [128, 1024]) for i in range(2)]
        xn2 = [sb("xn%d" % i, [128, 1024]) for i in range(2)]
        t12 = [sb("t1_%d" % i, [128, 1024]) for i in range(2)]
        t22 = [sb("t2_%d" % i, [128, 1024]) for i in range(2)]
        cosb = [sb("cos%d" % i, [128, 1024]) for i in range(2)]
        sinb = [sb("sin%d" % i, [128, 1024]) for i in range(2)]
        ob2 = [sb("ob%d" % i, [128, 1024], BF) for i in range(2)]
        stg = [sb("stg%d" % i, [128, 8, 128], BF) for i in range(2)]
        st82 = [sb("st8_%d" % i, [128, 32]) for i in range(2)]
        pc = {"i": 0}

        def prep(tt_i, H, srcd, colkey, gt, dstT, dkey):
            pc["i"] += 1
            pb_ = pc["i"] % 2
            sq, xn, t1, t2, ob, st8 = sq2[pb_], xn2[pb_], t12[pb_], t22[pb_], ob2[pb_], st82[pb_]
            kq = lambda n_: (n_, pb_)
            b = pc["i"] % 2
            lat = tt_i >= CTX // 128
            rows = slice(tt_i * 128, (tt_i + 1) * 128)
            W = H * 128
            dma("sp", src[b][:, 0:W], srcd[rows, :], r=[(k_, tt_i) for k_ in colkey], w=[("src", b)], slot="ld%d" % (3 + b))
            if lat:
                lr = slice((tt_i - 2) * 128, (tt_i - 1) * 128)
                dma("sp", cosb[b][:, 0:W], ropec_d[lr, 0:W], r=[], w=[("cos", b)], slot="ld%d" % (5 + b))
                dma("sp", sinb[b][:, 0:W], ropes_d[lr, 0:W], r=[], w=[("sin", b)], slot="ld%d" % (7 + b))
            tt("pool", sq[:, 0:W], src[b][:, 0:W], src[b][:, 0:W], ALU.mult, r=[("src", b)], w=[kq("sq")])
            S.op("dve", lambda e: e.reduce_sum(out=st8[:, 0:H], in_=sq[:, 0:W].rearrange("p (h d) -> p h d", d=128), axis=AX.X),
                 r=[kq("sq")], w=[kq("st8")])
            rstd_from_ss(st8[:, 0:H], st8[:, 16:16 + H], 128, st8[:, 8:8 + H], [kq("st8")])
            v3 = lambda a: a[:, 0:W].rearrange("p (h d) -> p h d", d=128)
            tt("dve", v3(xn), v3(src[b]), bc_last(st8[:, 16:16 + H], 128), ALU.mult, r=[("src", b), kq("st8")], w=[kq("xn")])
            tt("pool", v3(xn), v3(xn), bc_mid(gt[:], H), ALU.mult, r=[kq("xn"), "gq", "gk"], w=[kq("xn")])
            if lat:
                tt("dve", t1[:, 0:W], xn[:, 0:W], cosb[b][:, 0:W], ALU.mult, r=[kq("xn"), ("cos", b)], w=[kq("t1")])
                v4 = lambda a: a[:, 0:W].rearrange("p (a two c) -> p a two c", two=2, c=32)
                tt("pool", v4(t2)[:, :, 0, :], v4(xn)[:, :, 1, :], v4(sinb[b])[:, :, 0, :], ALU.mult, r=[kq("xn"), ("sin", b)], w=[kq("t2")])
                tt("pool", v4(t2)[:, :, 1, :], v4(xn)[:, :, 0, :], v4(sinb[b])[:, :, 1, :], ALU.mult, r=[kq("xn"), ("sin", b)], w=[kq("t2")])
                tt("dve", ob[:, 0:W], t1[:, 0:W], t2[:, 0:W], ALU.add, r=[kq("t1"), kq("t2")], w=[kq("ob")])
            else:
                cp("dve", ob[:, 0:W], xn[:, 0:W], r=[kq("xn")], w=[kq("ob")])
            pt = rr(2)
            for h in range(H):
                tr(psT[pt][:, h * 128:(h + 1) * 128], ob[:, h * 128:(h + 1) * 128], cstb[:, IDN, :], r=[kq("ob"), "cstb"], w=[PT[pt]])
            sg = rr(2)
            cp("act", stg[sg][:, 0:H, :], psT[pt][:, 0:W].rearrange("p (h t) -> p h t", t=128), r=[PT[pt]], w=[("stg", sg)])
            dma("st", dstT[:, :, rows].rearrange("h d t -> d h t"), stg[sg][:, 0:H, :], r=[("stg", sg)], w=[(dkey, tt_i)], slot="ev%d" % sg)

        for tt_i in range(NTT):
            prep(tt_i, 2, k_raw, ["k_raw"], gk, kT, "kT")
            prep(tt_i, 8, q_raw, ["q_raw0", "q_raw1"], gq, qT, "qT")

        S.barrier()
        ar["off"] = mark
        kTg = sb("kTg", [128, TA], BF)
        vg = sb("vg", [128, NTT, 128], BF)
        q4 = [sb("q4%d" % i, [128, 4, 128], BF) for i in range(2)]
        pTb = [sb("pT%d" % i, [128, 512], BF) for i in range(3)]
        den = sb("den", [128, 512])
        osb = [sb("osb%d" % i, [128, 4, 128], BF) for i in range(2)]
        sc = 1.0 / math.sqrt(128.0)
        for g in range(2):
            dma("sp", kTg[:], kT[g], r=K("kT", range(NTT)), w=["kTg"], slot="ld0")
            dma("sp", vg[:], v_tok[:, g * 128:(g + 1) * 128].rearrange("(c p) d -> p c d", p=128),
                r=K("v_tok", range(NTT)), w=["vg"], slot="ld1")
            for qt in range(NTT):
                b = qt % 2
                rows = slice(qt * 128, (qt + 1) * 128)
                dma("sp", q4[b][:], qT[g * 4:(g + 1) * 4, :, rows].rearrange("h d t -> d h t"), r=[("qT", qt)], w=[("q4", b)], slot="ld%d" % (2 + b))
                chunks = [(0, None), (1, None)]
                if qt >= 2:
                    i = qt - 2
                    if i > 0:
                        chunks.append((qt - 1, GE))
                    chunks.append((qt, None))
                    if i < NL - 1:
                        chunks.append((qt + 1, LE))
                ops, dps = psA[2 + b], psA[4 + b]
                opk, dpk = PA[2 + b], PA[4 + b]
                q4f = q4[b][:].rearrange("p h t -> p (h t)")
                nch = len(chunks)
                sbank = {}

                def score(ci):
                    kt = chunks[ci][0]
                    sb_i = ci % 2
                    sbank[ci] = sb_i
                    mm(psA[sb_i][:, :], kTg[:, kt * 128:(kt + 1) * 128], q4f, True, True, r=["kTg", ("q4", b)], w=[PA[sb_i]])

                score(0)
                for ci, (kt, mk) in enumerate(chunks):
                    if ci + 1 < nch:
                        score(ci + 1)
                    sb_i = sbank[ci]
                    pi = ci % 3
                    act(pTb[pi][:], psA[sb_i][:, :], AF.Exp, r=[PA[sb_i]], w=[("pT", pi)], scale=sc)
                    if mk is not None:
                        pv = pTb[pi][:].rearrange("p (h t) -> p h t", t=128)
                        tt("pool", pv, pv, bc_mid(cstb[:, mk, :], 4), ALU.mult, r=[("pT", pi), "cstb"], w=[("pT", pi)])
                    mm(ops[:, :], vg[:, kt, :], pTb[pi][:], ci == 0, ci == nch - 1, r=["vg", ("pT", pi)], w=[opk])
                    mm(dps[:, :], onesb[:], pTb[pi][:], ci == 0, ci == nch - 1, r=["onesb", ("pT", pi)], w=[dpk])
                d3 = den[:].rearrange("p (h t) -> p h t", t=128)
                tt("dve", d3, dps[:, :].rearrange("p (h t) -> p h t", t=128), bc_last(esink[:, g * 4:(g + 1) * 4], 128), ALU.add,
                   r=[dpk, "esink"], w=["den"])
                S.op("dve", lambda e: e.reciprocal(out=den[:], in_=den[:]), r=["den"], w=["den"])
                tt("dve", osb[b][:], ops[:, :].rearrange("p (h t) -> p h t", t=128), d3, ALU.mult, r=[opk, "den"], w=[("osb", b)])
                dma("st", mixT[g * 512:(g + 1) * 512, rows].rearrange("(h d) t -> d h t", d=128), osb[b][:], r=[("osb", b)],
                    w=[("mixT", g * 4 + h, qt) for h in range(4)], slot="ev%d" % b)

    def phase_D(l):
        phase_begin()
        NP = min(T, 2048) + 2
        xin = sb("xin", [128, NP])
        acc = sb("acc", [128, NP])
        cvo = sb("cvo", [128, NP], BF)
        cw = sb("cw", [128, 8, 3])
        cbias = sb("cbias", [128, 8])
        stg = [sb("stgd%d" % i, [128, 8, 128], BF) for i in range(2)]
        dma("sp", cw[:], convw_d[l], r=[], w=["cw"], slot="ld0")
        dma("sp", cbias[:], convb_d[l], r=[], w=["cbias"], slot="ld1")
        for j in range(8):
            for (t0, n, lo, hi) in conv_pieces():
                tts = range(t0 // 128, (t0 + n) // 128)
                tth = range(max(t0 - 128, lo) // 128, min(t0 + n + 128, hi) // 128)
                load_halo(xin, xbcT[j * 128:(j + 1) * 128, :], t0, n, lo, hi, "xin", "ld2", [("xbcT", j, q) for q in tth])
                ts("dve", acc[:, 0:n], xin[:, 1:n + 1], cw[:, j, 1:2], cbias[:, j:j + 1], ALU.mult, ALU.add, r=["xin", "cw", "cbias"], w=["acc"])
                stt_("dve", acc[:, 0:n], xin[:, 0:n], cw[:, j, 0:1], acc[:, 0:n], ALU.mult, ALU.add, r=["xin", "cw", "acc"], w=["acc"])
                stt_("dve", acc[:, 0:n], xin[:, 2:n + 2], cw[:, j, 2:3], acc[:, 0:n], ALU.mult, ALU.add, r=["xin", "cw", "acc"], w=["acc"])
                act(cvo[:, 0:n], acc[:, 0:n], AF.Silu, r=["acc"], w=["cvo"])
                dma("st", convT[j * 128:(j + 1) * 128, t0:t0 + n], cvo[:, 0:n], r=["cvo"], w=[("convT", j, q) for q in tts], slot="ev0")
                if j < 6:
                    for g8 in range(0, n // 128, 8):
                        m = min(8, n // 128 - g8)
                        pt = rr(2)
                        sg = rr(2)
                        for q in range(m):
                            tr(psT[pt][:, q * 128:(q + 1) * 128], cvo[:, (g8 + q) * 128:(g8 + q + 1) * 128], cstb[:, IDN, :],
                               r=["cvo", "cstb"], w=[PT[pt]])
                        cp("act", stg[sg][:, 0:m, :], psT[pt][:, 0:m * 128].rearrange("p (c d) -> p c d", d=128), r=[PT[pt]], w=[("stgd", sg)])
                        r0 = t0 + g8 * 128
                        dma("st", xbc_tok[r0:r0 + m * 128, j * 128:(j + 1) * 128].rearrange("(c p) d -> p c d", p=128), stg[sg][:, 0:m, :],
                            r=[("stgd", sg)], w=[("xbc_tok", j, r0 // 128 + q) for q in range(m)], slot="ev%d" % (1 + sg))
        phase_begin()
        dtb = sb("dtb", [128, 16])
        A16 = sb("A16", [128, 16])
        Dsk = sb("Dsk", [128, 8])
        gn = sb("gn", [128, 512])
        dma("sp", dtb[:], bc_rows(dtb_d[l:l + 1, :]), r=[], w=["dtb"], slot="ld0")
        dma("sp", A16[:], bc_rows(alog_d[l:l + 1, :]), r=[], w=["A16"], slot="ld1")
        dma("sp", Dsk[:], bc_rows(dsk_d[l:l + 1, :]), r=[], w=["Dsk"], slot="ld2")
        dma("sp", gn[:], bc_rows(sng_d[l:l + 1, :]), r=[], w=["gn"], slot="ld3")
        act(A16[:], A16[:], AF.Exp, r=["A16"], w=["A16"])
        ts("dve", A16[:], A16[:], -1.0, None, ALU.mult, None, r=["A16"], w=["A16"])
        NLB = 3
        dtr = [sb("dtr%d" % i, [128, 16]) for i in range(NLB)]
        BT = [sb("BT%d" % i, [128, 2, 128], BF) for i in range(NLB)]
        CT = [sb("CT%d" % i, [128, 2, 128], BF) for i in range(NLB)]
        xsb = [sb("xsb%d" % i, [128, 512], BF) for i in range(NLB)]
        Btk = [sb("Btk%d" % i, [128, 256], BF) for i in range(NLB)]
        sm = sb("sm", [128, 64])
        sm2 = sb("sm2", [128, 16])
        ex = [sb("ex%d" % i, [128, 24]) for i in range(2)]
        lt = sb("lt", [128, 8, 128])
        dec = sb("dec", [128, 8, 128])
        cbm = sb("cbm", [128, 2, 128])
        MT = [sb("MT%d" % i, [128, 8, 128], BF) for i in range(2)]
        xdt = [sb("xdt%d" % i, [128, 512], BF) for i in range(2)]
        xw = [sb("xw%d" % i, [128, 512], BF) for i in range(2)]
        t1 = sb("t1d", [128, 512])
        ysb = [sb("ysb%d" % i, [128, 512]) for i in range(2)]
        hst = sb("hst", [128, 512])
        hTb = sb("hTb", [128, 512], BF)
        yfl = sb("yfl", [128, 512])
        zl = sb("zl", [128, 512])
        u = sb("u", [128, 512])
        ub = sb("ub", [128, 512], BF)
        stg2 = [sb("stg2%d" % i, [128, 4, 128], BF) for i in range(2)]
        v8 = lambda a: a.rearrange("p (h d) -> p h d", d=64)
        p0, pS0, pS1, pY, pO, pS2 = psA
        k0, kS0, kS1, kY, kO, kS2 = PA

        def loads(c, b):
            rows = slice(c * 128, (c + 1) * 128)
            dma("sp", dtr[b][:], dt_raw[rows, :], r=[("dt_raw", c)], w=[("dtr", b)], slot="ld%d" % (4 + b))
            dma("sp", BT[b][:], convT[512:768, rows].rearrange("(g n) t -> n g t", n=128), r=[("convT", 4, c), ("convT", 5, c)], w=[("BT", b)], slot="ld%d" % (7 + b))
            dma("sp", CT[b][:], convT[768:1024, rows].rearrange("(g n) t -> n g t", n=128), r=[("convT", 6, c), ("convT", 7, c)], w=[("CT", b)], slot="ld%d" % (10 + b))
            dma("sp", xsb[b][:], xbc_tok[rows, 0:512], r=[("xbc_tok", j, c) for j in range(4)], w=[("xsb", b)], slot="ld%d" % (13 + b))
            dma("sp", Btk[b][:], xbc_tok[rows, 512:768], r=[("xbc_tok", j, c) for j in (4, 5)], w=[("Btk", b)], slot="ld%d" % (16 + b))

        for d in range(2):
            Ud, Ld, Md = (LE, GT, LE) if d == 0 else (GE, LT, GE)
            order = list(range(NTT)) if d == 0 else [1, 0] + list(range(NTT - 1, 1, -1))
            S.op("pool", lambda e: e.memset(hst[:], 0.0), w=["hst"])
            S.op("pool", lambda e: e.memset(hTb[:], 0.0), w=["hTb"])

            def stageA(oi):
                b = oi % NLB
                ab = oi % 2
                dt8, dta8, dtw = sm[:, 0:8], sm[:, 8:16], sm[:, 16:24]
                tt("dve", sm[:, 24:32], dtr[b][:, d * 8:(d + 1) * 8], dtb[:, d * 8:(d + 1) * 8], ALU.add, r=[("dtr", b), "dtb"], w=["sm"])
                act(sm[:, 24:32], sm[:, 24:32], AF.Exp, r=["sm"], w=["sm"])
                ts("dve", sm[:, 24:32], sm[:, 24:32], 1.0, None, ALU.add, None, r=["sm"], w=["sm"])
                act(dt8, sm[:, 24:32], AF.Ln, r=["sm"], w=["sm"])
                tt("dve", dta8, dt8, A16[:, d * 8:(d + 1) * 8], ALU.mult, r=["sm", "A16"], w=["sm"])
                mm(p0[:, 0:8], cst[:, Ud, :], dta8, True, True, r=["cst", "sm"], w=[k0])
                mm(p0[:, 8:16], cst[:, Ld, :], dta8, True, True, r=["cst", "sm"], w=[k0])
                mm(p0[:, 16:24], ones32[:], dta8, True, True, r=["ones32", "sm"], w=[k0])
                for g in range(2):
                    mm(p0[:, 128 + g * 128:256 + g * 128], BT[b][:, g, :], CT[b][:, g, :], True, True, r=[("BT", b), ("CT", b)], w=[k0])
                act(ex[ab][:], p0[:, 0:24], AF.Exp, r=[k0], w=[("ex", ab)])
                tt("dve", cbm[:], p0[:, 128:384].rearrange("p (g t) -> p g t", t=128), bc_mid(cst[:, Md, :], 2), ALU.mult,
                   r=[k0, "cst", ("ex", ab)], w=["cbm"])
                tt("pool", lt[:], bc_mid(cst[:, Ld, :], 8), bc_last(dta8, 128), ALU.mult, r=["cst", "sm"], w=["lt"])
                for h in range(8):
                    ps_, pk_ = (pS0, kS0) if h < 4 else (pS1, kS1)
                    mm(ps_[:, (h % 4) * 128:(h % 4 + 1) * 128], lt[:, h, :], cst[:, Ud, :], True, True, r=["lt", "cst"], w=[pk_])
                act(dec[:, 0:4, :], pS0[:, :].rearrange("p (h t) -> p h t", t=128), AF.Exp, r=[kS0], w=["dec0"])
                act(dec[:, 4:8, :], pS1[:, :].rearrange("p (h t) -> p h t", t=128), AF.Exp, r=[kS1], w=["dec1"])
                tt("dve", MT[ab][:, 0:4, :], dec[:, 0:4, :], bc_mid(cbm[:, 0, :], 4), ALU.mult, r=["dec0", "cbm"], w=[("MT0", ab)])
                tt("pool", MT[ab][:, 4:8, :], dec[:, 4:8, :], bc_mid(cbm[:, 1, :], 4), ALU.mult, r=["dec1", "cbm"], w=[("MT1", ab)])
                tt("dve", v8(xdt[ab][:]), v8(xsb[b][:]), bc_last(dt8, 64), ALU.mult, r=[("xsb", b), "sm"], w=[("xdt", ab)])
                tt("dve", dtw, dt8, ex[ab][:, 8:16], ALU.mult, r=["sm", ("ex", ab)], w=["sm"])
                tt("pool", v8(xw[ab][:]), v8(xsb[b][:]), bc_last(dtw, 64), ALU.mult, r=[("xsb", b), "sm"], w=[("xw", ab)])

            def stageB(oi, c):
                b = oi % NLB
                ab = oi % 2
                rows = slice(c * 128, (c + 1) * 128)
                for h in range(8):
                    mm(pY[:, h * 64:(h + 1) * 64], MT[ab][:, h, :], xdt[ab][:, h * 64:(h + 1) * 64], True, True,
                       r=[("MT0", ab) if h < 4 else ("MT1", ab), ("xdt", ab)], w=[kY])
                for g in range(2):
                    mm(pO[:, g * 256:(g + 1) * 256], CT[b][:, g, :], hTb[:, g * 256:(g + 1) * 256], True, True, r=[("CT", b), "hTb"], w=[kO])
                for g in range(2):
                    mm(pS2[:, g * 256:(g + 1) * 256], Btk[b][:, g * 128:(g + 1) * 128], xw[ab][:, g * 256:(g + 1) * 256], True, True,
                       r=[("Btk", b), ("xw", ab)], w=[kS2])
                tt("dve", v8(t1[:]), v8(pO[:, :]), bc_last(ex[ab][:, 0:8], 64), ALU.mult, r=[kO, ("ex", ab)], w=["t1d"])
                yb = ysb[oi % 2]
                tt("dve", yb[:], pY[:, :], t1[:], ALU.add, r=[kY, "t1d"], w=[("ysb", oi % 2)])
                tt("pool", v8(hst[:]), v8(hst[:]), bc_last(ex[ab][:, 16:24], 64), ALU.mult, r=["hst", ("ex", ab)], w=["hst"])
                tt("dve", hst[:], hst[:], pS2[:, :], ALU.add, r=["hst", kS2], w=["hst"])
                cp("act", hTb[:], hst[:], r=["hst"], w=["hTb"])
                if d == 0:
                    dma("st", y_f[rows, :], yb[:], r=[("ysb", oi % 2)], w=[("y_f", c)], slot="ev%d" % (oi % 2))
                else:
                    dma("sp", yfl[:], y_f[rows, :], r=[("y_f", c)], w=["yfl"], slot="ld19")
                    dma("sp", zl[:], z_tok[rows, :], r=[("z_tok", c)], w=["zl"], slot="ld20")
                    tt("pool", yb[:], yb[:], yfl[:], ALU.add, r=[("ysb", oi % 2), "yfl"], w=[("ysb", oi % 2)])
                    tt("dve", v8(u[:]), v8(xsb[b][:]), bc_last(Dsk[:], 64), ALU.mult, r=[("xsb", b), "Dsk"], w=["u"])
                    tt("dve", yb[:], yb[:], u[:], ALU.add, r=[("ysb", oi % 2), "u"], w=[("ysb", oi % 2)])
                    act(zl[:], zl[:], AF.Silu, r=["zl"], w=["zl"])
                    tt("dve", u[:], yb[:], zl[:], ALU.mult, r=[("ysb", oi % 2), "zl"], w=["u"])
                    tt("pool", t1[:], u[:], u[:], ALU.mult, r=["u"], w=["t1d"])
                    S.op("dve", lambda e: e.reduce_sum(out=sm2[:, 0:2], in_=t1[:].rearrange("p (g c) -> p g c", c=256), axis=AX.X),
                         r=["t1d"], w=["sm2"])
                    rstd_from_ss(sm2[:, 0:2], sm2[:, 4:6], 256, sm2[:, 2:4], ["sm2"])
                    tt("dve", u[:].rearrange("p (g c) -> p g c", c=256), u[:].rearrange("p (g c) -> p g c", c=256),
                       bc_last(sm2[:, 4:6], 256), ALU.mult, r=["u", "sm2"], w=["u"])
                    tt("pool", ub[:], u[:], gn[:], ALU.mult, r=["u", "gn"], w=["ub"])
                    pt = rr(2)
                    sg = rr(2)
                    for j in range(4):
                        tr(psT[pt][:, j * 128:(j + 1) * 128], ub[:, j * 128:(j + 1) * 128], cstb[:, IDN, :], r=["ub", "cstb"], w=[PT[pt]])
                    cp("act", stg2[sg][:], psT[pt][:, 0:512].rearrange("p (j t) -> p j t", t=128), r=[PT[pt]], w=[("stg2", sg)])
                    dma("st", mixT[1024:1536, rows].rearrange("(j c) t -> c j t", c=128), stg2[sg][:], r=[("stg2", sg)],
                        w=[("mixT", 8 + j, c) for j in range(4)], slot="ev%d" % (2 + sg))

            n_o = len(order)
            loads(order[0], 0)
            if n_o > 1:
                loads(order[1], 1)
            stageA(0)
            for oi, c in enumerate(order):
                if oi + 2 < n_o:
                    loads(order[oi + 2], (oi + 2) % NLB)
                if oi + 1 < n_o:
                    stageA(oi + 1)
                stageB(oi, c)

    sets = [(0, CTX, cap_c)] + [(CTX, T, cap_l)]
    slot_tiles = []
    col = 0
    for si, (_, _, cap) in enumerate(sets):
        for s0 in range(0, cap, 128):
            ns = min(128, cap - s0)
            slot_tiles.append((si, s0, ns, col))
            col += ns
    NS_TOT = col
    NSL = len(slot_tiles)

    def phase_GH(l):
        phase_begin()
        nonlocal xt, h32, junk, GS, stt, gate
        slotidx = sb("slotidx", [128, NSL, 16], U32)
        gvs = sb("gvs", [128, NSL, 16])
        gate = sb("gate", [128, 2, 2048])
        markH = ar["off"]
        g = alloc_norm()
        xt, h32, junk, GS, stt = g["xt"], g["h32"], g["junk"], g["GS"], g["stt"]
        h2f = [sb("h2f%d" % i, [128, 2048]) for i in range(2)]
        h2b = [sb("h2b%d" % i, [128, 2048], BF) for i in range(2)]
        h2T = sb("h2T", [128, KC, 128])
        wr = sb("wr", [128, KC, 16])
        sm = sb("smg", [128, 64])
        affT = sb("affT", [16, TA])
        work = sb("work", [16, max(T, CTX)])
        maxcap = max(cap_l, cap_c)
        vals = sb("vals", [16, maxcap])
        idxu = sb("idxu", [16, maxcap], U32)
        idxf = sb("idxf", [16, maxcap])
        slotf = sb("slotf", [128, 16])
        load_GS(l, 1, True)
        dma("sp", wr[:], w_r[l].rearrange("(kc p) e -> p kc e", p=128), r=[], w=["wr"], slot="ld6")
        for tt_i in range(NTT):
            dma("sp", moe_d[tt_i * 128:(tt_i + 1) * 128, :], zero32[:], r=["zero32"], w=[("moe", tt_i)], slot="cp%d" % (tt_i % 4))
        load_x(0)
        for tt_i in range(NTT):
            if tt_i + 1 < NTT:
                load_x(tt_i + 1)
            rows = slice(tt_i * 128, (tt_i + 1) * 128)
            hb_i = tt_i % 2
            norm_tile(tt_i, None, h2f[hb_i][:], ("h2f", hb_i))
            cp("act", h2b[hb_i][:], h2f[hb_i][:], r=[("h2f", hb_i)], w=[("h2b", hb_i)])
            dma("st", h2_d[rows, :], h2b[hb_i][:], r=[("h2b", hb_i)], w=[("h2", tt_i)], slot="ev%d" % hb_i)
            for q in range(4):
                for j in range(4):
                    kc = q * 4 + j
                    tr(psA[q][:, j * 128:(j + 1) * 128], h2f[hb_i][:, kc * 128:(kc + 1) * 128], cst[:, IDN, :], r=[("h2f", hb_i), "cst"], w=[PA[q]])
                cp("act" if q % 2 else "dve", h2T[:, q * 4:(q + 1) * 4, :], psA[q][:, :].rearrange("p (k t) -> p k t", t=128), r=[PA[q]], w=["h2T"])
            for kc in range(KC):
                mm(psA[4][:, 0:16], h2T[:, kc, :], wr[:, kc, :], kc == 0, kc == KC - 1, r=["h2T", "wr"], w=[PA[4]])
            S.op("dve", lambda e: e.reduce_max(out=sm[:, 0:1], in_=psA[4][:, 0:16], axis=AX.X), r=[PA[4]], w=["smg"])
            ts("dve", sm[:, 1:2], sm[:, 0:1], -1.0, None, ALU.mult, None, r=["smg"], w=["smg"])
            S.op("dve", lambda e: e.memset(sm[:, 2:3], 0.0), r=["smg"], w=["smg"])
            act(sm[:, 16:32], psA[4][:, 0:16], AF.Exp, r=[PA[4], "smg"], w=["smg"], bias=sm[:, 1:2], accum_out=sm[:, 2:3])
            S.op("dve", lambda e: e.reciprocal(out=sm[:, 3:4], in_=sm[:, 2:3]), r=["smg"], w=["smg"])
            ts("dve", sm[:, 32:48], sm[:, 16:32], sm[:, 3:4], None, ALU.mult, None, r=["smg"], w=["smg"])
            tr(psA[5][0:16, 0:128], sm[:, 32:48], cst[:, IDN, :], r=["smg", "cst"], w=[PA[5]])
            cp("dve", affT[:, tt_i * 128:(tt_i + 1) * 128], psA[5][0:16, 0:128], r=[PA[5]], w=["affT"])
        for si, (toff, n, cap) in enumerate(sets):
            cp("dve", work[:, 0:n], affT[:, toff:toff + n], r=["affT"], w=["work"])
            for r8 in range(cap // 8):
                sl = slice(r8 * 8, (r8 + 1) * 8)
                S.op("dve", lambda e, sl=sl, n=n: e.max(out=vals[:, sl], in_=work[:, 0:n]), r=["work"], w=["vals"])
                S.op("dve", lambda e, sl=sl, n=n: e.max_index(out=idxu[:, sl], in_max=vals[:, sl], in_values=work[:, 0:n]), r=["work", "vals"], w=["idxu"])
                S.op("dve", lambda e, sl=sl, n=n: e.match_replace(out=work[:, 0:n], in_to_replace=vals[:, sl], in_values=work[:, 0:n], imm_value=-1.0),
                     r=["work", "vals", "idxu"], w=["work"])
            cp("dve", idxf[:, 0:cap], idxu[:, 0:cap], r=["idxu"], w=["idxf"])
            ts("dve", idxf[:, 0:cap], idxf[:, 0:cap], float(toff), None, ALU.add, None, r=["idxf"], w=["idxf"])
            for sti, (sj, s0, ns, c0) in enumerate(slot_tiles):
                if sj != si:
                    continue
                tr(psA[0][0:ns, 0:16], idxf[:, s0:s0 + ns], cst[0:16, IDN, 0:16], r=["idxf", "cst"], w=[PA[0]])
                cp("dve", slotf[0:ns, :], psA[0][0:ns, 0:16], r=[PA[0]], w=["slotf"])
                cp("dve", slotidx[0:ns, sti, :], slotf[0:ns, :], r=["slotf"], w=["slotidx"])
                tr(psA[1][0:ns, 0:16], vals[:, s0:s0 + ns], cst[0:16, IDN, 0:16], r=["vals", "cst"], w=[PA[1]])
                cp("dve", gvs[0:ns, sti, :], psA[1][0:ns, 0:16], r=[PA[1]], w=["gvs"])
        if debug:
            dbg_aff = nc.dram_tensor("dbg_aff", [16, TA], F32, kind="ExternalOutput").ap()
            dbg_idx = nc.dram_tensor("dbg_idx", [128, NSL, 16], U32, kind="ExternalOutput").ap()
            dbg_gv = nc.dram_tensor("dbg_gv", [128, NSL, 16], F32, kind="ExternalOutput").ap()
            dma("sp", dbg_aff, affT[:], r=["affT"], w=["dbg_aff"], slot="ld0")
            dma("sp", dbg_idx, slotidx[:], r=["slotidx"], w=["dbg_idx"], slot="ld1")
            dma("sp", dbg_gv, gvs[:], r=["gvs"], w=["dbg_gv"], slot="ld2")
        S.barrier()
        ar["off"] = markH
        xgT = sb("xgT", [128, KC, NS_TOT], BF)
        hTe = sb("hTe", [128, 8, NS_TOT], BF)
        wg = [sb("wg%d" % i, [128, KC, 512], BF) for i in range(2)]
        wu = [sb("wu%d" % i, [128, KC, 512], BF) for i in range(2)]
        wd = [sb("wd%d" % i, [128, 8, 1024], BF) for i in range(2)]
        xg = [sb("xg%d" % i, [128, 2048], BF) for i in range(NSL)]
        ysb = [sb("ysbm%d" % i, [128, 2048]) for i in range(2)]
        sgt = [sb("sgt%d" % i, [128, 512]) for i in range(2)]
        cchunks = []
        c = 0
        for si, (_, _, cap) in enumerate(sets):
            for a in range(0, cap, 512):
                w_ = min(512, cap - a)
                cchunks.append((c + a, w_))
            c += cap
        def gathers(e_i):
            for sti, (sj, s0, ns, c0) in enumerate(slot_tiles):
                S.op("pool", lambda e, ns=ns, sti=sti, e_i=e_i: e.indirect_dma_start(
                    out=xg[sti][0:ns, :], out_offset=None, in_=h2_d[:, :],
                    in_offset=bass.IndirectOffsetOnAxis(ap=slotidx[0:ns, sti, e_i:e_i + 1], axis=0)),
                    r=K("h2", range(NTT)) + ["slotidx"], w=[("xg", sti)], slot="xg%d" % sti)

        def transposes(e_i):
            for sti, (sj, s0, ns, c0) in enumerate(slot_tiles):
                for half in range(2):
                    pt = rr(2)
                    for j in range(8):
                        kc = half * 8 + j
                        tr(psT[pt][:, j * 128:j * 128 + ns], xg[sti][0:ns, kc * 128:(kc + 1) * 128], cstb[0:ns, IDN, 0:ns],
                           r=[("xg", sti), "cstb"], w=[PT[pt]])
                    cp("act" if half else "dve", xgT[:, half * 8:(half + 1) * 8, c0:c0 + ns],
                       psT[pt][:, :].rearrange("p (k t) -> p k t", t=128)[:, :, 0:ns], r=[PT[pt]], w=["xgT"])

        def load_up(e_i, f4):
            load_w(wg[f4], w_eg[l, e_i, :, f4 * 512:(f4 + 1) * 512], KC, ("wg", f4), "wg%d" % f4)
            load_w(wu[f4], w_eu[l, e_i, :, f4 * 512:(f4 + 1) * 512], KC, ("wu", f4), "wu%d" % f4)

        def load_down(e_i):
            for half in range(2):
                load_w(wd[half], w_ed[l, e_i, :, half * 1024:(half + 1) * 1024], 8, ("wd", half), "wd%d" % half)

        def up(e_i, f4):
            b = f4
            for fc in range(4):
                for (cc0, cw_) in cchunks:
                    pg, pu = (0, 1) if rr(2) else (2, 3)
                    for kc in range(KC):
                        mm(psA[pg][:, 0:cw_], wg[b][:, kc, fc * 128:(fc + 1) * 128], xgT[:, kc, cc0:cc0 + cw_], kc == 0, kc == KC - 1,
                           r=[("wg", b), "xgT"], w=[PA[pg]])
                    for kc in range(KC):
                        mm(psA[pu][:, 0:cw_], wu[b][:, kc, fc * 128:(fc + 1) * 128], xgT[:, kc, cc0:cc0 + cw_], kc == 0, kc == KC - 1,
                           r=[("wu", b), "xgT"], w=[PA[pu]])
                    sgi = rr(2)
                    act(sgt[sgi][:, 0:cw_], psA[pg][:, 0:cw_], AF.Silu, r=[PA[pg]], w=[("sgt", sgi)])
                    tt("dve", hTe[:, f4 * 4 + fc, cc0:cc0 + cw_], sgt[sgi][:, 0:cw_], psA[pu][:, 0:cw_], ALU.mult,
                       r=[("sgt", sgi), PA[pu]], w=["hTe"])

        def down(e_i):
            for sti, (sj, s0, ns, c0) in enumerate(slot_tiles):
                yb = rr(2)
                for cg in range(4):
                    b = cg // 2
                    pb = 4 + (cg % 2)
                    for fc in range(8):
                        mm(psA[pb][0:ns, :], hTe[:, fc, c0:c0 + ns], wd[b][:, fc, (cg % 2) * 512:(cg % 2 + 1) * 512], fc == 0, fc == 7,
                           r=["hTe", ("wd", b)], w=[PA[pb]])
                    ts("dve", ysb[yb][0:ns, cg * 512:(cg + 1) * 512], psA[pb][0:ns, :], gvs[0:ns, sti, e_i:e_i + 1], None,
                       ALU.mult, None, r=[PA[pb], "gvs"], w=[("ysbm", yb)])
                S.op("pool", lambda e, yb=yb, ns=ns, sti=sti, e_i=e_i: e.indirect_dma_start(
                    out=moe_d[:, :], out_offset=bass.IndirectOffsetOnAxis(ap=slotidx[0:ns, sti, e_i:e_i + 1], axis=0),
                    in_=ysb[yb][0:ns, :], in_offset=None, compute_op=ALU.add),
                    r=[("ysbm", yb), "slotidx"], w=K("moe", range(NTT)), slot="sc%d" % yb)

        gathers(0)
        load_up(0, 0)
        for e_i in range(NE):
            transposes(e_i)
            load_up(e_i, 1)
            up(e_i, 0)
            load_down(e_i)
            up(e_i, 1)
            if e_i + 1 < NE:
                gathers(e_i + 1)
                load_up(e_i + 1, 0)
            down(e_i)
        S.barrier()
        ar["off"] = markH
        xo = [sb("xo%d" % i, [128, 2048]) for i in range(2)]
        mo = [sb("mo%d" % i, [128, 2048]) for i in range(2)]
        for tt_i in range(NTT):
            b = tt_i % 2
            st = 1 if tt_i < CTX // 128 else 0
            rows = slice(tt_i * 128, (tt_i + 1) * 128)
            dma("sp", xo[b][:], xres[rows, :], r=[("xres", tt_i)], w=[("xo", b)], slot="xo%d" % b)
            dma("sp", mo[b][:], moe_d[rows, :], r=[("moe", tt_i)], w=[("mo", b)], slot="mo%d" % b)
            tt("dve", mo[b][:], mo[b][:], gate[:, st, :], ALU.mult, r=[("mo", b), "gate"], w=[("mo", b)])
            tt("pool", xo[b][:], xo[b][:], mo[b][:], ALU.add, r=[("xo", b), ("mo", b)], w=[("xo", b)])
            dma("st", xres[rows, :], xo[b][:], r=[("xo", b)], w=[("xres", tt_i)], slot="ev%d" % b)

    zb = sb("zb", [128, 2048], BF)
    S.op("pool", lambda e: e.memset(zb[:], 0.0), w=["zb"])
    ar["perm"] = ar["off"]
    for j4 in range(4):
        for c0 in range(0, TA, 2048):
            n = min(2048, TA - c0)
            dma("sp", mixT[1024 + j4 * 128:1024 + (j4 + 1) * 128, c0:c0 + n], zb[:, 0:n], r=["zb"],
                w=[("mixT", 8 + j4, q) for q in range(c0 // 128, (c0 + n) // 128)], slot="cp%d" % (j4 % 4))
    phase0()
    for l in range(L):
        if upto < 1:
            continue
        phase_AB(l)
        if upto < 2:
            continue
        phase_E(l)
        if upto < 3:
            continue
        phase_C(l)
        if upto < 4:
            continue
        phase_D(l)
        if upto < 5:
            continue
        phase_F(l)
        if upto < 6:
            continue
        phase_GH(l)
    phase_begin()
    for tt_i in range(T // 128):
        rows = slice(tt_i * 128, (tt_i + 1) * 128)
        dma("sp", out_d[rows, :], xres[CTX + tt_i * 128:CTX + (tt_i + 1) * 128, :], r=[("xres", CTX // 128 + tt_i)],
            w=[("out", tt_i)], slot="cp%d" % (tt_i % 4))
    stats = S.emit(nc)
    es.close()
    return nc, stats


def host_inputs(inputs, T, L):
    f = np.float32
    r = {}
    p = np.arange(128)[:, None]
    j = np.arange(128)[None, :]
    consts = np.stack([(p == j), (p <= j), (p >= j), (p > j), (p < j)], axis=1).astype(f)
    r["consts"] = np.ascontiguousarray(consts)
    rows = T // 64
    row = np.repeat(np.arange(rows), 64).astype(f)
    col = np.tile(np.arange(64), rows).astype(f)
    inv = (10000.0 ** (-np.arange(32, dtype=f) / 32)).astype(f)
    ar = row[:, None] * inv
    ac = col[:, None] * inv
    cos = np.concatenate([np.cos(ar), np.cos(ar), np.cos(ac), np.cos(ac)], axis=1).astype(f)
    sin = np.concatenate([-np.sin(ar), np.sin(ar), -np.sin(ac), np.sin(ac)], axis=1).astype(f)
    r["ropec"] = np.ascontiguousarray(np.tile(cos, (1, 8)))
    r["ropes"] = np.ascontiguousarray(np.tile(sin, (1, 8)))
    for k in ("w_mod", "b_mod", "w_in", "w_out", "w_router", "w_expert_gate", "w_expert_up", "w_expert_down",
              "norm1_g", "norm2_g", "q_norm_g", "k_norm_g", "attn_sink", "ssd_d", "ssd_norm_g"):
        r[k] = np.ascontiguousarray(np.asarray(inputs[k], dtype=f)[:L])
    r["ssd_dt_bias"] = np.ascontiguousarray(np.asarray(inputs["ssd_dt_bias"], f)[:L].reshape(L, 16))
    r["ssd_a_log"] = np.ascontiguousarray(np.asarray(inputs["ssd_a_log"], f)[:L].reshape(L, 16))
    cw = np.asarray(inputs["ssd_conv_w"], f)[:L]
    r["convw"] = np.ascontiguousarray(cw.reshape(L, 3, 8, 128).transpose(0, 3, 2, 1))
    r["convb"] = np.ascontiguousarray(np.asarray(inputs["ssd_conv_b"], f)[:L].reshape(L, 8, 128).transpose(0, 2, 1))
    sw = np.asarray(inputs["sc_conv_w"], f)[:L]
    r["scw"] = np.ascontiguousarray(sw.reshape(L, 3, 4, 128).transpose(0, 3, 2, 1))
    x = np.asarray(inputs["x"], f)
    ctx = np.asarray(inputs["ctx"], f)
    c = np.asarray(inputs["c"], f)
    cc = np.asarray(inputs["c_ctx"], f)
    maps = []
    for b in range(x.shape[0]):
        m = dict(r)
        m["xall"] = np.ascontiguousarray(np.concatenate([ctx[b], x[b, :T]], axis=0))
        cs = np.stack([c[b], cc], axis=0)
        m["cT"] = np.ascontiguousarray(cs.reshape(2, KC, 128).transpose(2, 1, 0))
        maps.append(m)
    return maps


_CACHE = {}


def kernel(**inputs):
    T = inputs["x"].shape[1]
    L = inputs["w_mod"].shape[0]
    B = inputs["x"].shape[0]
    key = (T, L)
    if key not in _CACHE:
        _CACHE[key] = build(T, L)[0]
    nc = _CACHE[key]
    maps = host_inputs(inputs, T, L)
    res = run_bass_kernel_spmd(nc, maps, core_ids=list(range(B)))
    return np.stack([np.asarray(res.results[b]["out"]) for b in range(B)], axis=0).astype(np.float32)
```
